# Optimizing a Trainium2 kernel written in Bass

```python
import math
import jax
import jax.numpy as jnp
from jax import lax
import numpy as np

D_MODEL = 1024
BATCH = 16
SEQ = 2048
DEPTH = 1

CTX_LEN = 256
GRID_W = 64
ROPE_BASE = 10000.0
NORM_EPS = 1e-6

DA_HEADS = 4
DA_QK_DIM = 64
DA_V_DIM = 2 * DA_QK_DIM
DA_WIDTH = DA_HEADS * DA_V_DIM
Q_BLOCK = 128

GLA_HEADS = 4
GLA_K_DIM = 64
GLA_V_DIM = 128
GLA_WIDTH = GLA_HEADS * GLA_V_DIM
GLA_GATE_RANK = 16
GLA_GATE_TAU = 16.0
GLA_CHUNK = 64

MIX_WIDTH = DA_WIDTH + GLA_WIDTH
IN_PROJ_SIZES = (DA_HEADS * 2 * DA_QK_DIM, DA_HEADS * 2 * DA_QK_DIM, DA_WIDTH,
                 GLA_HEADS * GLA_K_DIM, GLA_HEADS * GLA_K_DIM, GLA_WIDTH, GLA_WIDTH,
                 GLA_GATE_RANK, GLA_GATE_RANK)
IN_PROJ_DIM = sum(IN_PROJ_SIZES)

N_GROUPS = 4
EXPERTS_PER_GROUP = 8
N_EXPERTS = N_GROUPS * EXPERTS_PER_GROUP
TOP_K = 2
EXPERT_HIDDEN = 512
DISPATCH_BLOCK = 128

kernel_name = "hymba_diffattn_gla_hiermoe_dit_block"


def rms_norm(x, gain):
    xf = x.astype(jnp.float32)
    y = xf * lax.rsqrt(jnp.mean(xf * xf, axis=-1, keepdims=True) + NORM_EPS)
    return (y * gain.astype(jnp.float32)).astype(x.dtype)


def modulate(h, shift, scale):
    return h * (1 + scale) + shift


def split_in_proj(p):
    parts, start = [], 0
    for size in IN_PROJ_SIZES:
        parts.append(p[..., start:start + size])
        start += size
    return parts


def axial_rope_tables(rows, rot_dim):
    n_freq = rot_dim // 4
    r, col = jnp.meshgrid(jnp.arange(rows, dtype=jnp.float32),
                          jnp.arange(GRID_W, dtype=jnp.float32), indexing="ij")
    pos = jnp.stack([r.reshape(-1), col.reshape(-1)], axis=-1)
    inv_freq = jnp.power(ROPE_BASE, -jnp.arange(n_freq, dtype=jnp.float32) / n_freq)
    ang = pos[:, :, None] * inv_freq
    return jnp.cos(ang), jnp.sin(ang)


def apply_axial_rope(x, cos, sin):
    n_freq = cos.shape[-1]
    xr = x.reshape(x.shape[:-1] + (2, 2, n_freq))
    x1, x2 = xr[..., 0, :], xr[..., 1, :]
    cs, sn = cos.astype(x.dtype), sin.astype(x.dtype)
    out = jnp.stack([x1 * cs - x2 * sn, x2 * cs + x1 * sn], axis=-2)
    return out.reshape(x.shape)


def diff_attend(q, k, v, lam):
    s = jnp.einsum("bqhmd,bkhmd->bhmqk", q, k).astype(jnp.float32) * (DA_QK_DIM ** -0.5)
    p = jax.nn.softmax(s, axis=-1)
    a = p[:, :, 0] - lam * p[:, :, 1]
    return jnp.einsum("bhqk,bkhe->bqhe", a.astype(v.dtype), v)


def gla_chunked(q, k, v, log_a, s0):
    B, T, H, dk = q.shape
    dv = v.shape[-1]
    n_chunks = T // GLA_CHUNK

    def chunks(t):
        return jnp.moveaxis(t.reshape(B, n_chunks, GLA_CHUNK, H, t.shape[-1]), 1, 0)

    mask = jnp.tril(jnp.ones((GLA_CHUNK, GLA_CHUNK), dtype=bool))

    def step(state, inp):
        qi, ki, vi, gi = inp
        b = jnp.cumsum(gi.astype(jnp.float32), axis=1)
        b_last = b[:, -1]
        q_dec = qi * jnp.exp(b)
        k_inv = ki * jnp.exp(-b)
        scores = jnp.einsum("bihk,bjhk->bhij", q_dec, k_inv)
        scores = jnp.where(mask, scores, 0.0)
        o = (jnp.einsum("bhij,bjhv->bihv", scores, vi)
             + jnp.einsum("bihk,bhkv->bihv", q_dec, state))
        k_to_end = ki * jnp.exp(b_last[:, None] - b)
        new_state = (state * jnp.exp(b_last)[..., None]
                     + jnp.einsum("bjhk,bjhv->bhkv", k_to_end, vi))
        return new_state, o

    s_final, o = lax.scan(step, s0, (chunks(q), chunks(k), chunks(v), chunks(log_a)))
    o = jnp.moveaxis(o, 0, 1).reshape(B, T, H, dv)
    return o, s_final


def log_gate(lowrank, w2, b):
    return jax.nn.log_sigmoid((lowrank @ w2 + b).astype(jnp.float32)) / GLA_GATE_TAU


def token_mixer(h_lat, h_ctx, cos, sin, lambda_init, w_in, da_lambda, da_subln_g,
                gla_gate_w2, gla_gate_b, gla_norm_g, w_out, with_ctx_out):
    B, S, _ = h_lat.shape
    L = h_ctx.shape[1]
    dt = h_lat.dtype
    dq_l, dk_l, dv_l, gq_l, gk_l, gv_l, gr_l, lrf_l, lrb_l = split_in_proj(h_lat @ w_in)
    dq_c, dk_c, dv_c, gq_c, gk_c, gv_c, gr_c, lrf_c, lrb_c = split_in_proj(h_ctx @ w_in)

    cos_b, sin_b = cos[:, None, None], sin[:, None, None]
    q_l = apply_axial_rope(dq_l.reshape(B, S, DA_HEADS, 2, DA_QK_DIM), cos_b, sin_b)
    k_l = apply_axial_rope(dk_l.reshape(B, S, DA_HEADS, 2, DA_QK_DIM), cos_b, sin_b)
    v_l = dv_l.reshape(B, S, DA_HEADS, DA_V_DIM)
    k_c = dk_c.reshape(B, L, DA_HEADS, 2, DA_QK_DIM)
    v_c = dv_c.reshape(B, L, DA_HEADS, DA_V_DIM)
    lam_p = da_lambda.astype(jnp.float32)
    lam = (jnp.exp(jnp.sum(lam_p[0] * lam_p[1])) - jnp.exp(jnp.sum(lam_p[2] * lam_p[3]))
           + lambda_init)
    k_all = jnp.concatenate([k_c, k_l], axis=1)
    v_all = jnp.concatenate([v_c, v_l], axis=1)
    qb = jnp.moveaxis(q_l.reshape(B, S // Q_BLOCK, Q_BLOCK, DA_HEADS, 2, DA_QK_DIM), 1, 0)
    a_l = lax.map(lambda qi: diff_attend(qi, k_all, v_all, lam), qb)
    a_l = jnp.moveaxis(a_l, 0, 1).reshape(B, S, DA_HEADS, DA_V_DIM)
    a_l = rms_norm(a_l, da_subln_g) * (1.0 - lambda_init)

    def gla_inputs(gq, gk, gv, lrf, lrb, n):
        q = gq.reshape(B, n, GLA_HEADS, GLA_K_DIM) * (GLA_K_DIM ** -0.5)
        k = gk.reshape(B, n, GLA_HEADS, GLA_K_DIM)
        v = gv.reshape(B, n, GLA_HEADS, GLA_V_DIM)
        g_f = log_gate(lrf, gla_gate_w2[0], gla_gate_b[0]).reshape(B, n, GLA_HEADS, GLA_K_DIM)
        g_b = log_gate(lrb, gla_gate_w2[1], gla_gate_b[1]).reshape(B, n, GLA_HEADS, GLA_K_DIM)
        return q, k, v, g_f, g_b

    gql, gkl, gvl, gfl, gbl = gla_inputs(gq_l, gk_l, gv_l, lrf_l, lrb_l, S)
    gqc, gkc, gvc, gfc, gbc = gla_inputs(gq_c, gk_c, gv_c, lrf_c, lrb_c, L)
    flip = lambda t: jnp.flip(t, axis=1)
    s0 = jnp.zeros((B, GLA_HEADS, GLA_K_DIM, GLA_V_DIM), jnp.float32)
    o_cf, s_cf = gla_chunked(gqc, gkc, gvc, gfc, s0)
    o_cb, s_cb = gla_chunked(flip(gqc), flip(gkc), flip(gvc), flip(gbc), s0)
    o_lf, _ = gla_chunked(gql, gkl, gvl, gfl, s_cf)
    o_lb, _ = gla_chunked(flip(gql), flip(gkl), flip(gvl), flip(gbl), s_cb)
    o_l = (o_lf + flip(o_lb)).astype(dt)
    g_l = rms_norm(o_l, gla_norm_g) * jax.nn.silu(gr_l.reshape(B, S, GLA_HEADS, GLA_V_DIM))

    y_lat = jnp.concatenate([a_l.reshape(B, S, DA_WIDTH), g_l.reshape(B, S, GLA_WIDTH)], -1) @ w_out
    if not with_ctx_out:
        return y_lat, None

    q_c = dq_c.reshape(B, L, DA_HEADS, 2, DA_QK_DIM)
    a_c = rms_norm(diff_attend(q_c, k_c, v_c, lam), da_subln_g) * (1.0 - lambda_init)
    o_c = (o_cf + flip(o_cb)).astype(dt)
    g_c = rms_norm(o_c, gla_norm_g) * jax.nn.silu(gr_c.reshape(B, L, GLA_HEADS, GLA_V_DIM))
    y_ctx = jnp.concatenate([a_c.reshape(B, L, DA_WIDTH), g_c.reshape(B, L, GLA_WIDTH)], -1) @ w_out
    return y_lat, y_ctx


def hier_moe(h, w_rg, b_rg, w_re, b_re, w_gate, w_up, w_down):
    T, D = h.shape
    hf = h.astype(jnp.float32)
    g_logits = hf @ w_rg.astype(jnp.float32) + b_rg.astype(jnp.float32)
    g_prob = jax.nn.softmax(g_logits, axis=-1)
    g_sel = jnp.argmax(g_logits, axis=-1)
    g_w = jnp.take_along_axis(g_prob, g_sel[:, None], axis=-1)
    e_logits = (hf @ w_re.astype(jnp.float32) + b_re.astype(jnp.float32)).reshape(
        T, N_GROUPS, EXPERTS_PER_GROUP)
    e_in_group = jnp.take_along_axis(e_logits, g_sel[:, None, None], axis=1)[:, 0]
    top_v, top_i = lax.top_k(e_in_group, TOP_K)
    weights = jax.nn.softmax(top_v, axis=-1) * g_w
    expert_id = (g_sel[:, None] * EXPERTS_PER_GROUP + top_i).astype(jnp.int32)

    tk = T * TOP_K
    n_blocks = -(-(tk + N_EXPERTS * (DISPATCH_BLOCK - 1)) // DISPATCH_BLOCK)
    n_rows = n_blocks * DISPATCH_BLOCK
    flat_e = expert_id.reshape(-1)
    flat_t = jnp.repeat(jnp.arange(T, dtype=jnp.int32), TOP_K)
    flat_w = weights.reshape(-1)
    order = jnp.argsort(flat_e)
    e_sorted = flat_e[order]
    counts = jnp.bincount(flat_e, length=N_EXPERTS)
    starts = jnp.cumsum(counts) - counts
    padded = ((counts + DISPATCH_BLOCK - 1) // DISPATCH_BLOCK) * DISPATCH_BLOCK
    pad_ends = jnp.cumsum(padded)
    pad_starts = pad_ends - padded
    dest = pad_starts[e_sorted] + (jnp.arange(tk) - starts[e_sorted])
    row_tok = jnp.full((n_rows,), T, jnp.int32).at[dest].set(flat_t[order])
    row_w = jnp.zeros((n_rows,), jnp.float32).at[dest].set(flat_w[order])
    block_start = jnp.arange(n_blocks, dtype=jnp.int32) * DISPATCH_BLOCK
    block_e = jnp.minimum(jnp.searchsorted(pad_ends, block_start, side="right"), N_EXPERTS - 1)
    h_pad = jnp.concatenate([h, jnp.zeros((1, D), h.dtype)], axis=0)
    xb = h_pad[row_tok].reshape(n_blocks, DISPATCH_BLOCK, D)

    def expert_block(args):
        xi, e = args
        return (jax.nn.silu(xi @ w_gate[e]) * (xi @ w_up[e])) @ w_down[e]

    yb = lax.map(expert_block, (xb, block_e)).reshape(n_rows, D)
    y = jax.ops.segment_sum(yb * row_w[:, None].astype(yb.dtype), row_tok, num_segments=T + 1)
    return y[:T]


def setup_inputs(seed: int = 0) -> dict:
    key = jax.random.key(seed)
    ks = jax.random.split(key, 24)
    f32 = jnp.float32
    D = D_MODEL
    nrm = lambda k, shape, s: jax.random.normal(k, shape, f32) * s
    gain = lambda k, shape: 1.0 + 0.02 * jax.random.normal(k, shape, f32)
    return {
        "x": nrm(ks[0], (BATCH, SEQ, D), 1.0),
        "c": nrm(ks[1], (BATCH, D), 1.0),
        "ctx": nrm(ks[2], (BATCH, CTX_LEN, D), 1.0),
        "c_ctx": nrm(ks[3], (D,), 1.0),
        "w_ada": nrm(ks[4], (DEPTH, D, 6 * D), 0.5 * D ** -0.5),
        "b_ada": nrm(ks[5], (DEPTH, 6 * D), 0.02),
        "norm_mix_g": gain(ks[6], (DEPTH, D)),
        "norm_ffn_g": gain(ks[7], (DEPTH, D)),
        "w_in": nrm(ks[8], (DEPTH, D, IN_PROJ_DIM), D ** -0.5),
        "da_lambda": nrm(ks[9], (DEPTH, 4, DA_QK_DIM), 0.1),
        "da_subln_g": gain(ks[10], (DEPTH, DA_V_DIM)),
        "gla_gate_w2": nrm(ks[11], (DEPTH, 2, GLA_GATE_RANK, GLA_HEADS * GLA_K_DIM), GLA_GATE_RANK ** -0.5),
        "gla_gate_b": nrm(ks[12], (DEPTH, 2, GLA_HEADS * GLA_K_DIM), 0.1),
        "gla_norm_g": gain(ks[13], (DEPTH, GLA_V_DIM)),
        "w_out": nrm(ks[14], (DEPTH, MIX_WIDTH, D), MIX_WIDTH ** -0.5),
        "router_group_w": nrm(ks[15], (DEPTH, D, N_GROUPS), D ** -0.5),
        "router_group_b": nrm(ks[16], (DEPTH, N_GROUPS), 0.01),
        "router_expert_w": nrm(ks[17], (DEPTH, D, N_EXPERTS), D ** -0.5),
        "router_expert_b": nrm(ks[18], (DEPTH, N_EXPERTS), 0.01),
        "expert_w_gate": nrm(ks[19], (DEPTH, N_EXPERTS, D, EXPERT_HIDDEN), D ** -0.5),
        "expert_w_up": nrm(ks[20], (DEPTH, N_EXPERTS, D, EXPERT_HIDDEN), D ** -0.5),
        "expert_w_down": nrm(ks[21], (DEPTH, N_EXPERTS, EXPERT_HIDDEN, D), EXPERT_HIDDEN ** -0.5),
        "final_norm_g": gain(ks[22], (D,)),
    }


def reference(x, c, ctx, c_ctx, w_ada, b_ada, norm_mix_g, norm_ffn_g, w_in, da_lambda,
              da_subln_g, gla_gate_w2, gla_gate_b, gla_norm_g, w_out, router_group_w,
              router_group_b, router_expert_w, router_expert_b, expert_w_gate, expert_w_up,
              expert_w_down, final_norm_g):
    B, S, D = x.shape
    L = ctx.shape[1]
    rows = S // GRID_W
    cos, sin = axial_rope_tables(rows, DA_QK_DIM)
    for layer in range(DEPTH):
        last = layer == DEPTH - 1
        lambda_init = 0.8 - 0.6 * math.exp(-0.3 * layer)
        mod_lat = (jax.nn.silu(c) @ w_ada[layer] + b_ada[layer])[:, None, :]
        mod_ctx = jax.nn.silu(c_ctx) @ w_ada[layer] + b_ada[layer]
        sh1, sc1, gt1, sh2, sc2, gt2 = jnp.split(mod_lat, 6, axis=-1)
        csh1, csc1, cgt1, csh2, csc2, cgt2 = jnp.split(mod_ctx, 6, axis=-1)

        h_lat = modulate(rms_norm(x, norm_mix_g[layer]), sh1, sc1)
        h_ctx = modulate(rms_norm(ctx, norm_mix_g[layer]), csh1, csc1)
        y_lat, y_ctx = token_mixer(h_lat, h_ctx, cos, sin, lambda_init, w_in[layer],
                                   da_lambda[layer], da_subln_g[layer], gla_gate_w2[layer],
                                   gla_gate_b[layer], gla_norm_g[layer], w_out[layer],
                                   with_ctx_out=not last)
        x = x + gt1 * y_lat

        moe_args = (router_group_w[layer], router_group_b[layer], router_expert_w[layer],
                    router_expert_b[layer], expert_w_gate[layer], expert_w_up[layer],
                    expert_w_down[layer])
        h2 = modulate(rms_norm(x, norm_ffn_g[layer]), sh2, sc2)
        x = x + gt2 * hier_moe(h2.reshape(B * S, D), *moe_args).reshape(B, S, D)
        if not last:
            ctx = ctx + cgt1 * y_ctx
            hc2 = modulate(rms_norm(ctx, norm_ffn_g[layer]), csh2, csc2)
            ctx = ctx + cgt2 * hier_moe(hc2.reshape(B * L, D), *moe_args).reshape(B, L, D)
    return rms_norm(x, final_norm_g)
```

```python
from contextlib import ExitStack
import os
import math
import numpy as np
import concourse.bass as bass
import concourse.mybir as mybir
from concourse.bass_utils import run_bass_kernel_spmd

F32 = mybir.dt.float32
BF16 = mybir.dt.bfloat16
AF = mybir.ActivationFunctionType
ALU = mybir.AluOpType
AX = mybir.AxisListType

ENGS = ("pe", "act", "dve", "pool", "sp")
NB = 2
NT = 18
TL = 16
D = 1024
EPS = 1e-6
NE = 32


class Buf:
    __slots__ = ("name", "last_writer", "readers")

    def __init__(self, name):
        self.name = name
        self.last_writer = None
        self.readers = []


class Op:
    __slots__ = ("eng", "fn", "deps", "is_dma", "signal", "semval", "key", "dma_val")

    def __init__(self, eng, fn):
        self.eng = eng
        self.fn = fn
        self.deps = set()
        self.is_dma = False
        self.signal = False
        self.semval = 0
        self.key = None
        self.dma_val = 0


class Sched:
    def __init__(self, nc):
        self.nc = nc
        self.ops = {e: [] for e in ENGS}
        self.dma_keys = {}
        self.bufs = {}
        self.last = {e: None for e in ENGS}

    def buf(self, name):
        b = self.bufs.get(name)
        if b is None:
            b = Buf(name)
            self.bufs[name] = b
        return b

    def op(self, eng, fn, reads=(), writes=(), dma_key=None, extra_deps=()):
        o = Op(eng, fn)
        o.deps.update(extra_deps)
        reads = [self.buf(r) for r in reads]
        writes = [self.buf(w) for w in writes]
        for r in reads:
            if r.last_writer is not None:
                o.deps.add(r.last_writer)
        for w in writes:
            if w.last_writer is not None:
                o.deps.add(w.last_writer)
            o.deps.update(w.readers)
        for r in reads:
            r.readers.append(o)
        for w in writes:
            w.last_writer = o
            w.readers = []
        if dma_key is not None:
            o.is_dma = True
            o.key = dma_key
            st = self.dma_keys.setdefault(dma_key, [0, None])
            if st[1] is not None:
                o.deps.add(st[1])
            st[0] += 16
            st[1] = o
            o.dma_val = st[0]
        o.deps.discard(o)
        if eng == "pe":
            o.deps = {d for d in o.deps if d.is_dma or d.eng != "pe"}
        for d in o.deps:
            d.signal = True
        self.ops[eng].append(o)
        if not o.is_dma and fn is not None:
            self.last[eng] = o
        return o

    def barrier(self):
        deps = [o for o in self.last.values() if o is not None]
        deps += [st[1] for st in self.dma_keys.values() if st[1] is not None]
        for e in ENGS:
            self.op(e, None, extra_deps=deps)

    def emit(self):
        nc = self.nc
        with ExitStack() as es:
            esem = {e: es.enter_context(nc.semaphore("s_" + e)) for e in ENGS}
            dsem = {k: es.enter_context(nc.semaphore("d%d" % i)) for i, k in enumerate(self.dma_keys)}
            for e in ENGS:
                c = 0
                for o in self.ops[e]:
                    if o.signal and not o.is_dma and o.fn is not None:
                        c += 1
                        o.semval = c
            block = es.enter_context(nc.Block())

            self.icount = {}

            def run(ename, eng):
                known = {}
                ic = 0
                for o in self.ops[ename]:
                    self.icount[ename] = ic
                    need = {}
                    for d in o.deps:
                        if d.is_dma:
                            s, v = dsem[d.key], d.dma_val
                        else:
                            s, v = esem[d.eng], d.semval
                        if v > need.get(s, (0, None))[0]:
                            need[s] = (v, s)
                    for v, s in need.values():
                        if known.get(s, 0) < v:
                            eng.wait_ge(s, v)
                            known[s] = v
                            ic += 1
                    if o.fn is None:
                        continue
                    ins = o.fn(eng)
                    ic += 1
                    if o.is_dma:
                        ins.then_inc(dsem[o.key], 16)
                    elif o.signal:
                        ins.then_inc(esem[ename], 1)

            block.tensor(lambda eng: run("pe", eng))
            block.scalar(lambda eng: run("act", eng))
            block.vector(lambda eng: run("dve", eng))
            block.gpsimd(lambda eng: run("pool", eng))
            block.sync(lambda eng: run("sp", eng))


class TV:
    def __init__(self, ap, name):
        self.ap = ap
        self.name = name

    def b(self, i=None):
        return self.name if i is None else "%s#%s" % (self.name, i)


class Arena:
    def __init__(self, nc, nbytes):
        self.cap = nbytes
        self.t = nc.alloc_sbuf_tensor("arena", [128, nbytes // 4], F32)
        self.top = 0
        self.cnt = 0
        self.peak = 0

    def alloc(self, name, shape, dtype):
        esz = 4 if dtype == F32 else 2
        free = 1
        for s in shape[1:]:
            free *= s
        nbytes = (free * esz + 63) // 64 * 64
        off = self.top
        self.top += nbytes
        self.peak = max(self.peak, self.top)
        assert self.top <= self.cap, ("SBUF arena overflow", name, self.top)
        ap = self.t[0:shape[0], off // 4:(off + nbytes) // 4]
        if esz == 2:
            ap = ap.bitcast(BF16)
        ap = ap[:, 0:free]
        if len(shape) == 3:
            ap = ap.rearrange("p (a b) -> p a b", a=shape[1], b=shape[2])
        elif len(shape) == 4:
            ap = ap.rearrange("p (a b c) -> p a b c", a=shape[1], b=shape[2], c=shape[3])
        self.cnt += 1
        return TV(ap, "%s@%d" % (name, self.cnt))


def build_program(stop=None):
    nc = bass.Bass("TRN2", target_bir_lowering=False)
    S = Sched(nc)

    def din(name, shape):
        return nc.dram_tensor(name, list(shape), F32, kind="ExternalInput").ap()

    xin = din("xin", [NB, NT * 128, D])
    cT_d = din("cT", [128, 8, 3])
    wada_d = din("w_ada", [128, 8, 6 * D])
    bada_d = din("b_ada", [128, 48])
    g1_d = din("g1", [128, 8])
    g2_d = din("g2", [128, 8])
    win_d = din("w_in", [128, 8, 3104])
    lam_d = din("da_lambda", [1, 256])
    subln_d = din("subln_g", [1, 128])
    glang_d = din("gla_norm_g", [1, 128])
    fng_d = din("final_g", [1, D])
    w2_d = din("gate_w2", [32, 512])
    gb_d = din("gate_b", [1, 512])
    wout_d = din("w_out", [128, 8, D])
    wr_d = din("w_router", [128, 8, 36])
    br_d = din("b_router", [1, 36])
    ewg_d = din("ewg", [NE, 128, 8, 512])
    ewu_d = din("ewu", [NE, 128, 8, 512])
    ewd_d = din("ewd", [NE, 128, 4, D])
    cos_d = din("rope_cos", [128, 2048])
    sin_d = din("rope_sin", [128, 2048])
    perm_d = din("rope_perm", [128, 128])
    tri_d = din("tri", [128, 4, 128])
    out_d = nc.dram_tensor("out", [NB, TL * 128, D], F32, kind="ExternalOutput").ap()
    mods_d = nc.dram_tensor("mods_scr", [NB, 48, 128], F32, kind="Internal").ap()
    x1s_d = nc.dram_tensor("x1_scr", [NB, TL * 128, D], F32, kind="Internal").ap()

    A = Arena(nc, 200 * 1024)
    ps = [nc.alloc_psum_tensor("ps%d" % i, [128, 512], F32) for i in range(8)]

    def PB(i):
        return "psb%d" % i

    def MM(out, lhsT, rhs, start, stop, r, w, sgc=False):
        S.op("pe", lambda e: e.matmul(out, lhsT=lhsT, rhs=rhs, start=start, stop=stop, skip_group_check=sgc), r, w)

    def TR(out, in_, ident, r, w):
        S.op("pe", lambda e: e.transpose(out, in_, ident), r, w)

    def ACT(out, in_, func, r, w, bias=None, scale=None, accum=None):
        kw = {}
        if bias is not None:
            kw["bias"] = bias
        if scale is not None:
            kw["scale"] = scale
        if accum is not None:
            kw["accum_out"] = accum
        S.op("act", lambda e: e.activation(out=out, in_=in_, func=func, **kw), r, w)

    def TT(eng, out, in0, in1, op, r, w):
        S.op(eng, lambda e: e.tensor_tensor(out=out, in0=in0, in1=in1, op=op), r, w)

    def TS(eng, out, in0, s1, s2, op0, op1, r, w):
        if op1 is None:
            S.op(eng, lambda e: e.tensor_scalar(out=out, in0=in0, scalar1=s1, scalar2=None, op0=op0), r, w)
        else:
            S.op(eng, lambda e: e.tensor_scalar(out=out, in0=in0, scalar1=s1, scalar2=s2, op0=op0, op1=op1), r, w)

    def STT(eng, out, in0, sc, in1, op0, op1, r, w):
        S.op(eng, lambda e: e.scalar_tensor_tensor(out=out, in0=in0, scalar=sc, in1=in1, op0=op0, op1=op1), r, w)

    def CP(eng, out, in_, r, w):
        if eng == "act":
            S.op("act", lambda e: e.activation(out=out, in_=in_, func=AF.Identity), r, w)
        else:
            S.op(eng, lambda e: e.tensor_copy(out=out, in_=in_), r, w)

    def RECIP(out, in_, r, w):
        S.op("dve", lambda e: e.reciprocal(out=out, in_=in_), r, w)

    def RMAX(out, in_, r, w):
        S.op("dve", lambda e: e.reduce_max(out=out, in_=in_, axis=AX.X), r, w)

    def RSUM(out, in_, r, w):
        S.op("dve", lambda e: e.reduce_sum(out=out, in_=in_, axis=AX.X), r, w)

    def MSET(eng, ap, val, w):
        S.op(eng, lambda e: e.memset(ap, val), (), w)

    def DMA(q, out, in_, r, w, key):
        return S.op(q, lambda e: e.dma_start(out=out, in_=in_), r, w, dma_key=key)

    def finish(dumps):
        outs = []
        for i, (name, ap, bufs) in enumerate(dumps):
            d = nc.dram_tensor("dbg_" + name, list(ap.shape), ap.dtype, kind="ExternalOutput").ap()
            outs.append(DMA("sp", d, ap, bufs, (), "dbg%d" % i))
        S.op("sp", None, extra_deps=outs)
        S.emit()
        print("ICOUNT", S.icount, {e: max([o.semval for o in S.ops[e]] + [0]) for e in ENGS}, {k: v[0] for k, v in S.dma_keys.items()})
        return nc

    identb = A.alloc("identb", [128, 128], BF16)
    identf = A.alloc("identf", [128, 128], F32)
    permb = A.alloc("permb", [128, 128], BF16)
    tri = A.alloc("tri", [128, 4, 128], F32)
    epsb = A.alloc("epsb", [128, 1], F32)
    onesf = A.alloc("onesf", [128, 1], F32)
    onesrow = A.alloc("onesrow", [1, 128], BF16)
    g1 = A.alloc("g1", [128, 8], F32)
    g2 = A.alloc("g2", [128, 8], F32)
    bada = A.alloc("bada", [128, 48], F32)
    cT = A.alloc("cT", [128, 8, 3], F32)
    scT = A.alloc("scT", [128, 8, 3], BF16)
    modT = A.alloc("modT", [128, 48, 3], F32)
    A1 = A.alloc("A1", [128, 3, 8], F32)
    A2 = A.alloc("A2", [128, 3, 8], F32)
    wr32 = A.alloc("wr32", [128, 8, 36], F32)
    brb = A.alloc("brb", [128, 36], F32)
    w2blk = A.alloc("w2blk", [32, 512], BF16)
    gbrow = A.alloc("gbrow", [1, 512], BF16)
    sublnb = A.alloc("sublnb", [128, 128], F32)
    glangb = A.alloc("glangb", [128, 128], F32)
    lamb = A.alloc("lamb", [128, 256], F32)
    lamt = A.alloc("lamt", [128, 8], F32)
    Wd = A.alloc("Wd", [128, TL, NE], F32)
    hT = A.alloc("hT", [128, 8, NT * 128], BF16)

    for tv, val in ((identb, 1.0), (identf, 1.0)):
        MSET("pool", tv.ap, val, [tv.b()])
        S.op("pool", (lambda ap: lambda e: e.affine_select(out=ap, in_=ap, pattern=[[-1, 128]],
                                                             compare_op=ALU.is_equal, fill=0.0, base=0,
                                                             channel_multiplier=1))(tv.ap),
             [tv.b()], [tv.b()])
    MSET("pool", epsb.ap, EPS, [epsb.b()])
    MSET("pool", onesf.ap, 1.0, [onesf.b()])
    MSET("pool", onesrow.ap, 1.0, [onesrow.b()])
    DMA("pool", permb.ap, perm_d, (), [permb.b()], "permb")
    DMA("sp", tri.ap, tri_d, (), [tri.b()], "tri")
    DMA("sp", g1.ap, g1_d, (), [g1.b()], "g1")
    DMA("sp", g2.ap, g2_d, (), [g2.b()], "g2")
    DMA("sp", bada.ap, bada_d, (), [bada.b()], "bada")
    DMA("sp", cT.ap, cT_d, (), [cT.b()], "cT")
    DMA("sp", wr32.ap, wr_d, (), [wr32.b()], "wr32")
    DMA("sp", brb.ap, br_d.partition_broadcast(128), (), [brb.b()], "brb")
    DMA("pool", w2blk.ap, w2_d, (), [w2blk.b()], "w2blk")
    DMA("pool", gbrow.ap, gb_d, (), [gbrow.b()], "gbrow")
    DMA("sp", sublnb.ap, subln_d.partition_broadcast(128), (), [sublnb.b()], "sublnb")
    DMA("sp", glangb.ap, glang_d.partition_broadcast(128), (), [glangb.b()], "glangb")
    DMA("sp", lamb.ap, lam_d.partition_broadcast(128), (), [lamb.b()], "lamb")
    lambda_init = 0.8 - 0.6 * math.exp(0.0)
    TS("dve", sublnb.ap, sublnb.ap, 1.0 - lambda_init, None, ALU.mult, None, [sublnb.b()], [sublnb.b()])
    lt = lamt.ap
    lb = lamb.ap
    TT("dve", lb[:, 0:64], lb[:, 0:64], lb[:, 64:128], ALU.mult, [lamb.b()], [lamb.b()])
    TT("dve", lb[:, 128:192], lb[:, 128:192], lb[:, 192:256], ALU.mult, [lamb.b()], [lamb.b()])
    RSUM(lt[:, 0:1], lb[:, 0:64], [lamb.b()], [lamt.b()])
    RSUM(lt[:, 1:2], lb[:, 128:192], [lamb.b()], [lamt.b()])
    ACT(lt[:, 2:4], lt[:, 0:2], AF.Exp, [lamt.b()], [lamt.b()])
    TT("dve", lt[:, 4:5], lt[:, 3:4], lt[:, 2:3], ALU.subtract, [lamt.b()], [lamt.b()])
    TS("dve", lt[:, 7:8], lt[:, 4:5], -lambda_init, None, ALU.add, None, [lamt.b()], [lamt.b()])
    nlam = lt[:, 7:8]

    ACT(scT.ap, cT.ap, AF.Silu, [cT.b()], [scT.b()])
    m0 = A.top
    wb = [A.alloc("wbuf%d" % i, [128, 8, 512], BF16) for i in range(2)]
    psA = ps[0][:, 0:144]
    for grp in range(12):
        w = wb[grp % 2]
        DMA("pool", w.ap, wada_d[:, :, grp * 512:(grp + 1) * 512], (), [w.b()], "wbuf%d" % (grp % 2))
        for c in range(4):
            fo = grp * 4 + c
            for kc in range(8):
                MM(psA[:, fo * 3:fo * 3 + 3], w.ap[:, kc, c * 128:(c + 1) * 128], scT.ap[:, kc, :],
                   kc == 0, kc == 7, [w.b(), scT.b()], [PB(0)])
    psA3 = psA.rearrange("p (a b) -> p a b", a=48, b=3)
    for j in range(3):
        TT("dve", modT.ap[:, :, j], psA3[:, :, j], bada.ap, ALU.add, [PB(0), bada.b()], [modT.b()])
        STT("dve", A1.ap[:, j, :], modT.ap[:, 8:16, j], 1.0, g1.ap, ALU.add, ALU.mult, [modT.b(), g1.b()], [A1.b()])
        STT("dve", A2.ap[:, j, :], modT.ap[:, 32:40, j], 1.0, g2.ap, ALU.add, ALU.mult, [modT.b(), g2.b()], [A2.b()])
    mtmp = A.alloc("mtmp", [128, 48], F32)
    mrow = A.alloc("mrow", [48, 128], F32)
    for j in range(NB):
        CP("dve", mtmp.ap, modT.ap[:, :, j], [modT.b()], [mtmp.b()])
        TR(ps[1][0:48, 0:128], mtmp.ap, identf.ap, [mtmp.b(), identf.b()], [PB(1)])
        CP("dve", mrow.ap, ps[1][0:48, 0:128], [PB(1)], [mrow.b()])
        DMA("sp", mods_d[j], mrow.ap, [mrow.b()], ["mods"], "mods_w")
    S.barrier()
    if stop == 0:
        g1t = A.alloc("g1t", [128, D], F32)
        DMA("sp", g1t.ap, mods_d[0, 16:24, :].rearrange("(o a) b -> o (a b)", o=1).partition_broadcast(128), ["mods"], [g1t.b()], "gt1b")
        return finish([("modT", modT.ap, [modT.b()]), ("A1", A1.ap, [A1.b()]), ("A2", A2.ap, [A2.b()]),
                       ("lamt", lamt.ap, [lamt.b()]), ("gt1b", g1t.ap, [g1t.b()]), ("subln", sublnb.ap, [sublnb.b()]),
                       ("identb", identb.ap, [identb.b()])])
    A.top = m0
    base_top = A.top
    if os.environ.get("PAD"):
        pd_ = A.alloc("pad", [128, 8], F32)
        base_top = A.top
        for i in range(int(os.environ["PAD"])):
            MSET("dve", pd_.ap, float(i), [pd_.b()])

    def rms_stats(src_ap, src_b, junk, st, n):
        MSET("pool", st.ap[:, 0:1], 0.0, [st.b()])
        ACT(junk.ap, src_ap, AF.Square, src_b, [junk.b(), st.b()], accum=st.ap[:, 0:1])
        ACT(st.ap[:, 1:2], st.ap[:, 0:1], AF.Sqrt, [st.b(), epsb.b()], [st.b()], bias=epsb.ap, scale=1.0 / n)
        RECIP(st.ap[:, 1:2], st.ap[:, 1:2], [st.b()], [st.b()])

    out_stores = []
    TOKB = [(0, 512), (512, 512), (1024, 512), (1536, 512), (2048, 256)]
    for b in range(NB):
        A.top = base_top
        mixer_mark = A.top
        mix = A.alloc("mix", [128, TL, D], BF16)
        att_mark = A.top
        cosb = A.alloc("cosb", [128, 2048], BF16)
        sinb = A.alloc("sinb", [128, 2048], BF16)
        QT = A.alloc("QT", [128, 4, 2048], BF16)
        KT = A.alloc("KT", [128, 4, NT * 128], BF16)
        V = A.alloc("V", [128, NT, 4, 130], BF16)
        DMA("pool", cosb.ap, cos_d, (), [cosb.b()], "cosb")
        DMA("pool", sinb.ap, sin_d, (), [sinb.b()], "sinb")
        MSET("pool", V.ap[:, :, :, 128:130], 1.0, [V.b("ones")])
        pa_mark = A.top
        xt = [A.alloc("xt%d" % i, [128, D], F32) for i in range(2)]
        xs = [A.alloc("xs%d" % i, [128, D], BF16) for i in range(2)]
        junk = A.alloc("junk", [128, D], BF16)
        st = [A.alloc("st%d" % i, [128, 2], F32) for i in range(2)]
        for t in range(NT):
            j = 2 if t < 2 else b
            x_, s_, t_ = xt[t % 2], xs[t % 2], st[t % 2]
            DMA("sp", x_.ap, xin[b, t * 128:(t + 1) * 128, :], (), [x_.b()], "xt%d" % (t % 2))
            rms_stats(x_.ap, [x_.b()], junk, t_, D)
            S.op("dve", (lambda o, i, s: lambda e: e.tensor_scalar_mul(out=o, in0=i, scalar1=s))(s_.ap, x_.ap, t_.ap[:, 1:2]),
                 [x_.b(), t_.b()], [s_.b()])
            pb = 2 + (t % 2)
            pT = ps[pb][:].bitcast(BF16)
            for kc in range(8):
                TR(pT[:, kc * 128:(kc + 1) * 128], s_.ap[:, kc * 128:(kc + 1) * 128], identb.ap,
                   [s_.b(), identb.b()], [PB(pb)])
            for kc in range(8):
                o_ = hT.ap[:, kc, t * 128:(t + 1) * 128]
                i_ = pT[:, kc * 128:(kc + 1) * 128]
                if kc % 2 == 0:
                    ACT(o_, i_, AF.Identity, [PB(pb), A1.b(), modT.b()], [hT.b(t)],
                        bias=modT.ap[:, kc, j:j + 1], scale=A1.ap[:, j, kc:kc + 1])
                else:
                    TS("dve", o_, i_, A1.ap[:, j, kc:kc + 1], modT.ap[:, kc, j:j + 1], ALU.mult, ALU.add,
                       [PB(pb), A1.b(), modT.b()], [hT.b(t)])
        hT_all = [hT.b(t) for t in range(NT)]
        if stop == 1:
            S.barrier()
            return finish([("hT", hT.ap, hT_all)])

        wb = [A.alloc("wbuf%d" % i, [128, 8, 512], BF16) for i in range(2)]
        qraw = [A.alloc("qraw%d" % i, [128, 512], BF16) for i in range(2)]
        rt1 = [A.alloc("rt1_%d" % i, [128, 512], F32) for i in range(2)]
        rt2 = [A.alloc("rt2_%d" % i, [128, 512], F32) for i in range(2)]
        cnt = 0
        for gi, (dst, col0) in enumerate(((QT, 0), (KT, 512))):
            w = wb[gi % 2]
            DMA("pool", w.ap, win_d[:, :, col0:col0 + 512], (), [w.b()], "wbuf%d" % (gi % 2))
            for h in range(4):
                if dst is KT:
                    pb = cnt % 2
                    for kc in range(8):
                        MM(ps[pb][:, 0:256], w.ap[:, kc, h * 128:(h + 1) * 128], hT.ap[:, kc, 0:256], kc == 0, kc == 7,
                           [w.b(), hT.b(0), hT.b(1)], [PB(pb)])
                    CP("act", KT.ap[:, h, 0:256], ps[pb][:, 0:256], [PB(pb)], [KT.b("%d_c" % h)])
                    cnt += 1
                for tb_i in range(4):
                    tb = 0 if os.environ.get('TB0') else tb_i
                    pz_ = 0 if os.environ.get("PAR0") else cnt % 2
                    pb = pz_ if not os.environ.get("PARB") else cnt % 2
                    pw = 2 + pz_
                    qr, r1, r2 = qraw[pz_], rt1[pz_], rt2[pz_]
                    tok = 256 + tb * 512
                    hr = [hT.b(2 + tb * 4 + i) for i in range(4)]
                    for kc in range(8):
                        MM(ps[pb][:], w.ap[:, kc, h * 128:(h + 1) * 128], hT.ap[:, kc, tok:tok + 512], kc == 0, kc == 7,
                           [w.b()] + hr, [PB(pb)])
                    CP("act", qr.ap, ps[pb][:], [PB(pb)], [qr.b()])
                    MM(ps[pw][:], permb.ap, qr.ap, True, True, [permb.b(), qr.b()], [PB(pw)])
                    TT("dve", r1.ap, ps[pb][:], cosb.ap[:, tb * 512:(tb + 1) * 512], ALU.mult, [PB(pb), cosb.b(), qr.b()], [r1.b()])
                    TT("dve", r2.ap, ps[pw][:], sinb.ap[:, tb * 512:(tb + 1) * 512], ALU.mult, [PB(pw), sinb.b()], [r2.b()])
                    if dst is QT:
                        o_ = QT.ap[:, h, tb * 512:(tb + 1) * 512]
                        ob = QT.b("%d_%d" % (h, tb))
                    else:
                        o_ = KT.ap[:, h, tok:tok + 512]
                        ob = KT.b("%d_%d" % (h, tb))
                    TT("dve" if os.environ.get("POOLDVE") else "pool", o_, r1.ap, r2.ap, ALU.add, [r1.b(), r2.b()], [ob])
                    if cnt + 1 == int(os.environ.get("CUTN", "0")):
                        S.barrier()
                        return finish([("QT", QT.ap, []), ("KT", KT.ap, [])])
                    cnt += 1
        w = wb[0]
        DMA("pool", w.ap, win_d[:, :, 1024:1536], (), [w.b()], "wbuf0")
        for t in range(NT):
            pb = 4 + t % 2
            for kc in range(8):
                MM(ps[pb][:], hT.ap[:, kc, t * 128:(t + 1) * 128], w.ap[:, kc, :], kc == 0, kc == 7,
                   [w.b(), hT.b(t)], [PB(pb)])
            CP("act" if t % 2 == 0 else "dve", V.ap[:, t, :, 0:128],
               ps[pb][:].rearrange("p (a b) -> p a b", a=4, b=128), [PB(pb)], [V.b(t)])
        S.barrier()
        if stop == 2:
            return finish([("QT", QT.ap, []), ("KT", KT.ap, []), ("V", V.ap, [])])
        A.top = pa_mark

        ET = [A.alloc("ET%d" % i, [128, 512], BF16) for i in range(3)]
        fin = [A.alloc("fin%d" % i, [128, 8], F32) for i in range(2)]
        ta = [A.alloc("ta%d" % i, [128, 128], F32) for i in range(2)]
        tb_ = [A.alloc("tb%d" % i, [128, 128], F32) for i in range(2)]
        junk = A.alloc("junk", [128, 128], BF16)
        Vall = [V.b(t) for t in range(NT)] + [V.b("ones")]
        cnt = 0
        fcnt = 0
        for h in range(4):
            Kall = [KT.b("%d_c" % h)] + [KT.b("%d_%d" % (h, i)) for i in range(4)]
            for qb in range(4):
                par = (h * 4 + qb) % 2

                def acc(m, qs):
                    r = m * 4 + qs
                    return ps[2 + par * 3 + r // 3][:, (r % 3) * 129:(r % 3) * 129 + 129], "acc%d_%d" % (par, r)
                started = set()
                for m in range(2):
                    for kt in range(NT):
                        pb = cnt % 2
                        e_ = ET[cnt % 3]
                        MM(ps[pb][:], KT.ap[m * 64:(m + 1) * 64, h, kt * 128:(kt + 1) * 128],
                           QT.ap[m * 64:(m + 1) * 64, h, qb * 512:(qb + 1) * 512], True, True,
                           Kall + [QT.b("%d_%d" % (h, qb))], [PB(pb)])
                        ACT(e_.ap, ps[pb][:], AF.Exp, [PB(pb)], [e_.b()], scale=0.125)
                        for qs in range(4):
                            ap_, bn = acc(m, qs)
                            bank = (m * 4 + qs) // 3
                            st_ = kt == 0 and bank not in started
                            started.add(bank)
                            MM(ap_, e_.ap[:, qs * 128:(qs + 1) * 128], V.ap[:, kt, h, 0:129], st_, kt == NT - 1,
                               [e_.b()] + Vall, [bn], sgc=True)
                        cnt += 1
                for qs in range(4):
                    a1, b1 = acc(0, qs)
                    a2, b2 = acc(1, qs)
                    f, x_, y_ = fin[fcnt % 2], ta[fcnt % 2], tb_[fcnt % 2]
                    fcnt += 1
                    tq = qb * 4 + qs
                    RECIP(f.ap[:, 0:1], a1[:, 128:129], [b1], [f.b()])
                    RECIP(f.ap[:, 1:2], a2[:, 128:129], [b2], [f.b()])
                    TT("dve", f.ap[:, 2:3], f.ap[:, 1:2], nlam, ALU.mult, [f.b(), lamt.b()], [f.b()])
                    TS("dve", x_.ap, a1[:, 0:128], f.ap[:, 0:1], None, ALU.mult, None, [b1, f.b()], [x_.b()])
                    STT("dve", y_.ap, a2[:, 0:128], f.ap[:, 2:3], x_.ap, ALU.mult, ALU.add, [b2, f.b(), x_.b()], [y_.b()])
                    MSET("pool", f.ap[:, 3:4], 0.0, [f.b()])
                    ACT(junk.ap, y_.ap, AF.Square, [y_.b()], [junk.b(), f.b()], accum=f.ap[:, 3:4])
                    ACT(f.ap[:, 4:5], f.ap[:, 3:4], AF.Sqrt, [f.b(), epsb.b()], [f.b()], bias=epsb.ap, scale=1.0 / 128)
                    RECIP(f.ap[:, 4:5], f.ap[:, 4:5], [f.b()], [f.b()])
                    STT("dve", mix.ap[:, tq, h * 128:(h + 1) * 128], y_.ap, f.ap[:, 4:5], sublnb.ap, ALU.mult, ALU.mult,
                        [y_.b(), f.b(), sublnb.b()], [mix.b(tq)])
        S.barrier()
        if stop == 3:
            return finish([("mix", mix.ap, [])])
        A.top = att_mark

        gqk = A.alloc("gqk", [128, 4, NT * 128], BF16)
        gkt = A.alloc("gkt", [128, NT, 256], BF16)
        gv = A.alloc("gv", [128, NT, 512], BF16)
        lrT = A.alloc("lrT", [32, NT * 128], BF16)
        Sb = A.alloc("Sb", [128, 4, TL, 128], BF16)
        gla_mark = A.top
        wb = [A.alloc("wbuf%d" % i, [128, 8, 512], BF16) for i in range(2)]
        w = wb[0]
        DMA("pool", w.ap, win_d[:, :, 1536:2048], (), [w.b()], "wbuf0")
        cnt = 0
        for c in range(4):
            for (tok, n) in TOKB:
                pb = cnt % 2
                hr = [hT.b(tok // 128 + i) for i in range(n // 128)]
                for kc in range(8):
                    MM(ps[pb][:, 0:n], w.ap[:, kc, c * 128:(c + 1) * 128], hT.ap[:, kc, tok:tok + n], kc == 0, kc == 7,
                       [w.b()] + hr, [PB(pb)])
                CP("act" if cnt % 2 == 0 else "dve", gqk.ap[:, c, tok:tok + n], ps[pb][:, 0:n], [PB(pb)], [gqk.b()])
                cnt += 1
        for t in range(NT):
            pb = 2 + t % 2
            for kc in range(8):
                MM(ps[pb][:, 0:256], hT.ap[:, kc, t * 128:(t + 1) * 128], w.ap[:, kc, 256:512], kc == 0, kc == 7,
                   [w.b(), hT.b(t)], [PB(pb)])
            CP("act" if t % 2 == 0 else "dve", gkt.ap[:, t, :], ps[pb][:, 0:256], [PB(pb)], [gkt.b()])
        w = wb[1]
        DMA("pool", w.ap, win_d[:, :, 2048:2560], (), [w.b()], "wbuf1")
        for t in range(NT):
            pb = 4 + t % 2
            for kc in range(8):
                MM(ps[pb][:], hT.ap[:, kc, t * 128:(t + 1) * 128], w.ap[:, kc, :], kc == 0, kc == 7,
                   [w.b(), hT.b(t)], [PB(pb)])
            CP("act" if t % 2 == 0 else "dve", gv.ap[:, t, :], ps[pb][:], [PB(pb)], [gv.b()])
        w = wb[0]
        DMA("pool", w.ap[:, :, 0:32], win_d[:, :, 3072:3104], (), [w.b()], "wbuf0")
        for (tok, n) in TOKB:
            pb = 6 + cnt % 2
            cnt += 1
            hr = [hT.b(tok // 128 + i) for i in range(n // 128)]
            for kc in range(8):
                MM(ps[pb][0:32, 0:n], w.ap[:, kc, 0:32], hT.ap[:, kc, tok:tok + n], kc == 0, kc == 7, [w.b()] + hr, [PB(pb)])
            CP("act", lrT.ap[:, tok:tok + n], ps[pb][0:32, 0:n], [PB(pb)], [lrT.b()])
        S.barrier()
        if stop == 4:
            return finish([("gqk", gqk.ap, []), ("gkt", gkt.ap, []), ("gv", gv.ap, []), ("lrT", lrT.ap, [])])
        A.top = gla_mark

        St = A.alloc("St", [128, 4, 128], F32)
        MSET("dve", St.ap, 0.0, [St.b(i) for i in range(4)])
        G_ = [A.alloc("G%d" % i, [128, 256], F32) for i in range(2)]
        ER = [A.alloc("ER%d" % i, [128, 256], F32) for i in range(2)]
        k2e = [A.alloc("k2e%d" % i, [128, 256], BF16) for i in range(2)]
        dec = [A.alloc("dec%d" % i, [128, 2], F32) for i in range(2)]
        orders = (list(range(NT)), [1, 0] + list(range(NT - 1, 1, -1)))
        cnt = 0
        for s in range(NT):
            for d in range(2):
                t = orders[d][s]
                g_, er, ke, dc = G_[cnt % 2], ER[cnt % 2], k2e[cnt % 2], dec[cnt % 2]
                pz = cnt % 2
                pu = 2 + cnt % 2
                pd = 4 + cnt % 2
                cnt += 1
                MM(ps[pz][:, 0:256], lrT.ap[:, t * 128:(t + 1) * 128], w2blk.ap[:, d * 256:(d + 1) * 256], True, False,
                   [lrT.b(), w2blk.b()], [PB(pz)])
                MM(ps[pz][:, 0:256], onesrow.ap, gbrow.ap[:, d * 256:(d + 1) * 256], False, True,
                   [onesrow.b(), gbrow.b()], [PB(pz)])
                ACT(g_.ap, ps[pz][:, 0:256], AF.Exp, [PB(pz)], [g_.b()], scale=-1.0)
                ACT(g_.ap, g_.ap, AF.Ln, [g_.b()], [g_.b()], bias=1.0)
                MM(ps[pz][:, 256:512], tri.ap[:, 2 + d, :], g_.ap, True, True, [tri.b(), g_.b()], [PB(pz)])
                ACT(er.ap, ps[pz][:, 256:512], AF.Exp, [PB(pz)], [er.b()], scale=-1.0 / 16)
                TT("dve", ke.ap, gkt.ap[:, t, :], er.ap, ALU.mult, [gkt.b(), er.b()], [ke.b()])
                for p in range(2):
                    MM(ps[pd][:, p:p + 1], g_.ap[:, p * 128:(p + 1) * 128], onesf.ap, True, True, [g_.b(), onesf.b()], [PB(pd)])
                    MM(ps[pu][:, p * 256:(p + 1) * 256], ke.ap[:, p * 128:(p + 1) * 128], gv.ap[:, t, p * 256:(p + 1) * 256],
                       True, True, [ke.b(), gv.b()], [PB(pu)])
                ACT(dc.ap, ps[pd][:, 0:2], AF.Exp, [PB(pd)], [dc.b()], scale=-1.0 / 16)
                for p in range(2):
                    i = d * 2 + p
                    if t >= 2:
                        CP("pool", Sb.ap[:, i, t - 2, :], St.ap[:, i, :], [St.b(i)], [Sb.b()])
                    for hh in range(2):
                        r0, r1_ = hh * 64, hh * 64 + 64
                        STT("dve", St.ap[r0:r1_, i, :], St.ap[r0:r1_, i, :], dc.ap[r0:r1_, p:p + 1],
                            ps[pu][r0:r1_, p * 256 + hh * 128:p * 256 + hh * 128 + 128], ALU.mult, ALU.add,
                            [St.b(i), dc.b(), PB(pu)], [St.b(i)])
        S.barrier()
        if stop == 5:
            return finish([("Sb", Sb.ap, []), ("St", St.ap, [])])
        A.top = gla_mark

        G2 = [A.alloc("G2_%d" % i, [128, 512], F32) for i in range(2)]
        eb = [A.alloc("eb%d" % i, [128, 512], F32) for i in range(2)]
        ei = [A.alloc("ei%d" % i, [128, 512], F32) for i in range(2)]
        qd = [A.alloc("qd%d" % i, [128, 2, 2, 128], BF16) for i in range(2)]
        ki = [A.alloc("ki%d" % i, [128, 2, 2, 128], BF16) for i in range(2)]
        qz = [A.alloc("qz%d" % i, [128, 2, 4, 128], BF16) for i in range(2)]
        for i in range(2):
            MSET("pool", qz[i].ap, 0.0, [qz[i].b()])
        sTm = [A.alloc("sTm%d" % i, [128, 2, 4, 128], BF16) for i in range(2)]
        osq = [A.alloc("osq%d" % i, [128, 4, 128], F32) for i in range(2)]
        fs = [A.alloc("fs%d" % i, [128, 8], F32) for i in range(2)]
        for t in range(2, NT):
            k = t % 2
            g_, eb_, ei_, qd_, ki_, sm, oq, f = G2[k], eb[k], ei[k], qz[k], ki[k], sTm[k], osq[k], fs[k]
            pz, pbk, ps0, ps1, po = 0 + k, 2 + k, 4, 5, 6 + k
            MM(ps[pz][:], lrT.ap[:, t * 128:(t + 1) * 128], w2blk.ap, True, False, [lrT.b(), w2blk.b()], [PB(pz)])
            MM(ps[pz][:], onesrow.ap, gbrow.ap, False, True, [onesrow.b(), gbrow.b()], [PB(pz)])
            ACT(g_.ap, ps[pz][:], AF.Exp, [PB(pz)], [g_.b()], scale=-1.0)
            ACT(g_.ap, g_.ap, AF.Ln, [g_.b()], [g_.b()], bias=1.0)
            for d in range(2):
                for p in range(2):
                    c0 = (d * 2 + p) * 128
                    MM(ps[pbk][:, c0:c0 + 128], g_.ap[:, d * 256 + p * 128:d * 256 + p * 128 + 128], tri.ap[:, d, :], True, True,
                       [g_.b(), tri.b()], [PB(pbk)])
            ACT(eb_.ap, ps[pbk][:], AF.Exp, [PB(pbk)], [eb_.b()], scale=-1.0 / 16)
            ACT(ei_.ap, ps[pbk][:], AF.Exp, [PB(pbk)], [ei_.b()], scale=1.0 / 16)
            if os.environ.get("P2CUT") == "2":
                S.barrier()
                return finish([("f", f.ap, [])])

            for d in range(2):
                for p in range(2):
                    c0 = (d * 2 + p) * 128
                    for hh in range(2):
                        q0 = hh * 64
                        STT("dve", qd_.ap[q0:q0 + 64, d, 2 * p + hh, :], gqk.ap[q0:q0 + 64, p, t * 128:(t + 1) * 128], 0.125,
                            eb_.ap[q0:q0 + 64, c0:c0 + 128], ALU.mult, ALU.mult, [gqk.b(), eb_.b()], [qd_.b()])
                    TT("dve", ki_.ap[:, d, p, :], gqk.ap[:, 2 + p, t * 128:(t + 1) * 128], ei_.ap[:, c0:c0 + 128], ALU.mult,
                       [gqk.b(), ei_.b()], [ki_.b()])
            if os.environ.get("P2CUT") == "3":
                S.barrier()
                return finish([("f", f.ap, [])])

            for d in range(2):
                psd = ps0 if d == 0 else ps1
                for h in range(4):
                    r0 = (h % 2) * 64
                    MM(ps[psd][:, h * 128:(h + 1) * 128], ki_.ap[:, d, h // 2, :], qd_.ap[:, d, h, :],
                       True, True, [ki_.b(), qd_.b()], [PB(psd)])
                if os.environ.get("P2CUT") == "41":
                    S.barrier()
                    return finish([("f", f.ap, [])])
                for h in range(4):
                    TT("dve", sm.ap[:, d, h, :], ps[psd][:, h * 128:(h + 1) * 128], tri.ap[:, d, :], ALU.mult,
                       [PB(psd), tri.b()], [sm.b()])
                if os.environ.get("P2CUT") == "42":
                    S.barrier()
                    return finish([("f", f.ap, [])])
            if os.environ.get("P2CUT") == "4":
                S.barrier()
                return finish([("f", f.ap, [])])

            for h in range(4):
                r0 = (h % 2) * 64
                o_ = ps[po][:, h * 128:(h + 1) * 128]
                MM(o_, sm.ap[:, 0, h, :], gv.ap[:, t, h * 128:(h + 1) * 128], True, False, [sm.b(), gv.b()], [PB(po)])
                MM(o_, sm.ap[:, 1, h, :], gv.ap[:, t, h * 128:(h + 1) * 128], False, True, [sm.b(), gv.b()], [PB(po)])
            if os.environ.get("P2CUT") == "5":
                S.barrier()
                return finish([("f", f.ap, [])])

            for h in range(4):
                r0 = (h % 2) * 64
                o_ = ps[pz][:, h * 128:(h + 1) * 128]
                MM(o_, qd_.ap[:, 0, h, :], Sb.ap[:, 0 + h // 2, t - 2, :], True, False, [qd_.b(), Sb.b()], [PB(pz)])
                MM(o_, qd_.ap[:, 1, h, :], Sb.ap[:, 2 + h // 2, t - 2, :], False, True, [qd_.b(), Sb.b()], [PB(pz)])
            osb = oq
            ACT(osb.ap.rearrange("p a b -> p (a b)"), ps[pz][:], AF.Identity, [PB(pz)], [oq.b()])
            TT("dve", osb.ap.rearrange("p a b -> p (a b)"), ps[po][:], osb.ap.rearrange("p a b -> p (a b)"), ALU.add,
               [PB(po), oq.b()], [oq.b()])
            if os.environ.get("P2CUT") == "6":
                S.barrier()
                return finish([("f", f.ap, [])])

            o3 = osb.ap
            sq_ = eb_.ap.rearrange("p (a b) -> p a b", a=4, b=128)
            MSET("pool", f.ap[:, 0:4], 0.0, [f.b()])
            for h in range(4):
                ACT(sq_[:, h, :], o3[:, h, :], AF.Square, [oq.b()], [eb_.b(), f.b()], accum=f.ap[:, h:h + 1])
            ACT(f.ap[:, 4:8], f.ap[:, 0:4], AF.Sqrt, [f.b(), epsb.b()], [f.b()], bias=epsb.ap, scale=1.0 / 128)
            RECIP(f.ap[:, 4:8], f.ap[:, 4:8], [f.b()], [f.b()])
            for h in range(4):
                STT("dve", mix.ap[:, t - 2, 512 + h * 128:512 + (h + 1) * 128], o3[:, h, :], f.ap[:, 4 + h:5 + h], glangb.ap,
                    ALU.mult, ALU.mult, [oq.b(), f.b(), glangb.b()], [mix.b(t - 2)])
            if os.environ.get("P2CUT") == "7":
                S.barrier()
                return finish([("f", f.ap, [])])

        S.barrier()
        if stop == 6:
            return finish([("mix", mix.ap, [])])
        A.top = att_mark

        wb = [A.alloc("wbuf%d" % i, [128, 8, 512], BF16) for i in range(1)]
        sg = [A.alloc("sg%d" % i, [128, 512], F32) for i in range(2)]
        w = wb[0]
        DMA("pool", w.ap, win_d[:, :, 2560:3072], (), [w.b()], "wbuf0")
        for t in range(TL):
            pb = t % 2
            s_ = sg[t % 2]
            for kc in range(8):
                MM(ps[pb][:], hT.ap[:, kc, (t + 2) * 128:(t + 3) * 128], w.ap[:, kc, :], kc == 0, kc == 7,
                   [w.b(), hT.b(t + 2)], [PB(pb)])
            ACT(s_.ap, ps[pb][:], AF.Silu, [PB(pb)], [s_.b()])
            TT("dve" if t % 2 == 0 else "pool", mix.ap[:, t, 512:1024], mix.ap[:, t, 512:1024], s_.ap, ALU.mult,
               [mix.b(t), s_.b()], [mix.b(t)])
        S.barrier()
        if stop == 7:
            return finish([("mix", mix.ap, [])])
        A.top = att_mark

        wo = A.alloc("wo", [128, 8, D], BF16)
        gt1b = A.alloc("gt1b", [128, D], F32)
        DMA("pool", wo.ap, wout_d, (), [wo.b()], "wo")
        DMA("sp", gt1b.ap, mods_d[b, 16:24, :].rearrange("(o a) b -> o (a b)", o=1).partition_broadcast(128), ["mods"], [gt1b.b()], "gt1b")
        mixT = [A.alloc("mixT%d" % i, [128, 8, 128], BF16) for i in range(2)]
        xt = [A.alloc("xt%d" % i, [128, D], F32) for i in range(2)]
        x1 = [A.alloc("x1_%d" % i, [128, D], F32) for i in range(2)]
        ytmp = A.alloc("ytmp", [128, D], F32)
        xs2 = A.alloc("xs2", [128, D], F32)
        h32 = A.alloc("h32", [128, 8, 128], F32)
        junk = A.alloc("junk", [128, D], BF16)
        st = [A.alloc("st%d" % i, [128, 2], F32) for i in range(2)]
        rt = [A.alloc("rt%d" % i, [128, 160], F32) for i in range(2)]
        h2T = hT
        for t in range(TL):
            k = t % 2
            mT, x_, x1_, s_, r_ = mixT[k], xt[k], x1[k], st[k], rt[k]
            DMA("sp", x_.ap, xin[b, (t + 2) * 128:(t + 3) * 128, :], (), [x_.b()], "xt%d" % k)
            pT = ps[0][:].bitcast(BF16)
            for kc in range(8):
                TR(pT[:, kc * 128:(kc + 1) * 128], mix.ap[:, t, kc * 128:(kc + 1) * 128], identb.ap, [mix.b(t), identb.b()], [PB(0)])
            CP("act", mT.ap, pT.rearrange("p (a b) -> p a b", a=8, b=128), [PB(0)], [mT.b()])
            for half in range(2):
                pb = 1 + half
                for kc in range(8):
                    MM(ps[pb][:], mT.ap[:, kc, :], wo.ap[:, kc, half * 512:(half + 1) * 512], kc == 0, kc == 7,
                       [mT.b(), wo.b()], [PB(pb)])
                sl = slice(half * 512, (half + 1) * 512)
                TT("dve", ytmp.ap[:, sl], ps[pb][:], gt1b.ap[:, sl], ALU.mult, [PB(pb), gt1b.b()], [ytmp.b(half)])
                TT("pool", x1_.ap[:, sl], ytmp.ap[:, sl], x_.ap[:, sl], ALU.add, [ytmp.b(half), x_.b()], [x1_.b()])
            DMA("sp", x1s_d[b, t * 128:(t + 1) * 128, :], x1_.ap, [x1_.b()], ["x1s"], "x1_%d" % k)
            rms_stats(x1_.ap, [x1_.b()], junk, s_, D)
            S.op("dve", (lambda o, i, s: lambda e: e.tensor_scalar_mul(out=o, in0=i, scalar1=s))(xs2.ap, x1_.ap, s_.ap[:, 1:2]),
                 [x1_.b(), s_.b()], [xs2.b()])
            for kc in range(8):
                pb = 3 + kc // 4
                TR(ps[pb][:, (kc % 4) * 128:(kc % 4 + 1) * 128], xs2.ap[:, kc * 128:(kc + 1) * 128], identf.ap,
                   [xs2.b(), identf.b()], [PB(pb)])
            for kc in range(8):
                pb = 3 + kc // 4
                i_ = ps[pb][:, (kc % 4) * 128:(kc % 4 + 1) * 128]
                if kc % 2 == 0:
                    ACT(h32.ap[:, kc, :], i_, AF.Identity, [PB(pb), A2.b(), modT.b()], [h32.b()],
                        bias=modT.ap[:, 24 + kc, b:b + 1], scale=A2.ap[:, b, kc:kc + 1])
                else:
                    TS("dve", h32.ap[:, kc, :], i_, A2.ap[:, b, kc:kc + 1], modT.ap[:, 24 + kc, b:b + 1], ALU.mult, ALU.add,
                       [PB(pb), A2.b(), modT.b()], [h32.b()])
            CP("pool", h2T.ap[:, :, t * 128:(t + 1) * 128], h32.ap, [h32.b()], [h2T.b("m%d" % t)])
            for kc in range(8):
                MM(ps[5][:, 0:36], h32.ap[:, kc, :], wr32.ap[:, kc, :], kc == 0, kc == 7, [h32.b(), wr32.b()], [PB(5)])
            R = r_.ap
            rb = [r_.b()]
            lg, els, k1, k2, tmp = R[:, 0:36], R[:, 36:68], R[:, 68:100], R[:, 100:132], R[:, 132:140]
            sc = R[:, 140:160]
            TT("dve", lg, ps[5][:, 0:36], brb.ap, ALU.add, [PB(5), brb.b()], rb)
            RMAX(sc[:, 0:1], lg[:, 0:4], rb, rb)
            TS("dve", tmp[:, 0:4], lg[:, 0:4], sc[:, 0:1], None, ALU.is_equal, None, rb, rb)
            TS("dve", sc[:, 1:2], sc[:, 0:1], -1.0, None, ALU.mult, None, rb, rb)
            MSET("dve", sc[:, 2:3], 0.0, rb)
            ACT(tmp[:, 4:8], lg[:, 0:4], AF.Exp, rb, rb, bias=sc[:, 1:2], accum=sc[:, 2:3])
            RECIP(sc[:, 3:4], sc[:, 2:3], rb, rb)
            TS("dve", tmp[:, 0:4], tmp[:, 0:4], 1e30, -1e30, ALU.mult, ALU.add, rb, rb)
            for g in range(4):
                TS("dve", els[:, g * 8:(g + 1) * 8], lg[:, 4 + g * 8:12 + g * 8], tmp[:, g:g + 1], None, ALU.add, None, rb, rb)
            RMAX(sc[:, 4:5], els, rb, rb)
            TS("dve", k1, els, sc[:, 4:5], None, ALU.is_equal, None, rb, rb)
            STT("dve", els, k1, -1e30, els, ALU.mult, ALU.add, rb, rb)
            RMAX(sc[:, 5:6], els, rb, rb)
            TS("dve", k2, els, sc[:, 5:6], None, ALU.is_equal, None, rb, rb)
            TT("dve", sc[:, 6:7], sc[:, 5:6], sc[:, 4:5], ALU.subtract, rb, rb)
            ACT(sc[:, 7:8], sc[:, 6:7], AF.Exp, rb, rb)
            TS("dve", sc[:, 8:9], sc[:, 7:8], 1.0, None, ALU.add, None, rb, rb)
            RECIP(sc[:, 8:9], sc[:, 8:9], rb, rb)
            TT("dve", sc[:, 9:10], sc[:, 8:9], sc[:, 3:4], ALU.mult, rb, rb)
            TT("dve", sc[:, 10:11], sc[:, 9:10], sc[:, 7:8], ALU.mult, rb, rb)
            TS("dve", k1, k1, sc[:, 9:10], None, ALU.mult, None, rb, rb)
            STT("dve", Wd.ap[:, t, :], k2, sc[:, 10:11], k1, ALU.mult, ALU.add, rb, [Wd.b()])
        S.barrier()
        if stop == 8:
            return finish([("Wd", Wd.ap, []), ("h2T", hT.ap, [])])
        A.top = mixer_mark

        yacc = A.alloc("yacc", [128, TL, D], F32)
        gt2b = A.alloc("gt2b", [128, D], F32)
        rg = [A.alloc("rg%d" % i, [128, 8, 512], BF16) for i in range(2)]
        ru = [A.alloc("ru%d" % i, [128, 8, 512], BF16) for i in range(2)]
        rd = [A.alloc("rd%d" % i, [128, 4, D], BF16) for i in range(2)]
        sgt = [A.alloc("sgt%d" % i, [128, 512], F32) for i in range(2)]
        hTm = [A.alloc("hTm%d" % i, [128, 4, 512], BF16) for i in range(2)]
        DMA("sp", gt2b.ap, mods_d[b, 40:48, :].rearrange("(o a) b -> o (a b)", o=1).partition_broadcast(128), ["mods"], [gt2b.b()], "gt2b")
        for t in range(TL):
            DMA("sp", yacc.ap[:, t, :], x1s_d[b, t * 128:(t + 1) * 128, :], ["x1s"], [yacc.b(t)], "yacc%d" % (t % 4))
        h2all = [h2T.b("m%d" % t) for t in range(TL)]
        cnt = 0
        ycnt = 0
        for e in range(NE):
            k = e % 2
            DMA("pool", rg[k].ap, ewg_d[e], (), [rg[k].b()], "rg%d" % k)
            DMA("pool", ru[k].ap, ewu_d[e], (), [ru[k].b()], "ru%d" % k)
            DMA("pool", rd[k].ap, ewd_d[e], (), [rd[k].b()], "rd%d" % k)
            for hc in range(4):
                TT("pool", rd[k].ap[:, hc, :], rd[k].ap[:, hc, :], gt2b.ap, ALU.mult, [rd[k].b(), gt2b.b()], [rd[k].b()])
            for tb in range(4):
                hm = hTm[tb % 2]
                hr = h2all[tb * 4:tb * 4 + 4]
                for hc in range(4):
                    pg, pu = cnt % 2, 2 + cnt % 2
                    s_ = sgt[cnt % 2]
                    cnt += 1
                    for kc in range(8):
                        MM(ps[pg][:], rg[k].ap[:, kc, hc * 128:(hc + 1) * 128], h2T.ap[:, kc, tb * 512:(tb + 1) * 512],
                           kc == 0, kc == 7, [rg[k].b()] + hr, [PB(pg)])
                    for kc in range(8):
                        MM(ps[pu][:], ru[k].ap[:, kc, hc * 128:(hc + 1) * 128], h2T.ap[:, kc, tb * 512:(tb + 1) * 512],
                           kc == 0, kc == 7, [ru[k].b()] + hr, [PB(pu)])
                    ACT(s_.ap, ps[pg][:], AF.Silu, [PB(pg)], [s_.b()])
                    TT("dve", hm.ap[:, hc, :], ps[pu][:], s_.ap, ALU.mult, [PB(pu), s_.b()], [hm.b()])
                for ti in range(4):
                    t = tb * 4 + ti
                    for half in range(2):
                        py = 4 + ycnt % 4
                        ycnt += 1
                        for hc in range(4):
                            MM(ps[py][:], hm.ap[:, hc, ti * 128:(ti + 1) * 128], rd[k].ap[:, hc, half * 512:(half + 1) * 512],
                               hc == 0, hc == 3, [hm.b(), rd[k].b()], [PB(py)])
                        ya = yacc.ap[:, t, half * 512:(half + 1) * 512]
                        STT("dve", ya, ps[py][:], Wd.ap[:, t, e:e + 1], ya, ALU.mult, ALU.add,
                            [PB(py), Wd.b(), yacc.b(t)], [yacc.b(t)])
        if stop == 9:
            S.barrier()
            return finish([("yacc", yacc.ap, [])])
        fngb = A.alloc("fngb", [128, D], F32)
        DMA("sp", fngb.ap, fng_d.partition_broadcast(128), (), [fngb.b()], "fngb")
        ot = [A.alloc("ot%d" % i, [128, D], F32) for i in range(2)]
        junk = A.alloc("junk", [128, D], BF16)
        st = [A.alloc("st%d" % i, [128, 2], F32) for i in range(2)]
        for t in range(TL):
            o_, s_ = ot[t % 2], st[t % 2]
            rms_stats(yacc.ap[:, t, :], [yacc.b(t)], junk, s_, D)
            STT("dve", o_.ap, yacc.ap[:, t, :], s_.ap[:, 1:2], fngb.ap, ALU.mult, ALU.mult,
                [yacc.b(t), s_.b(), fngb.b()], [o_.b()])
            out_stores.append(DMA("sp", out_d[b, t * 128:(t + 1) * 128, :], o_.ap, [o_.b()], (), "ot%d" % (t % 2)))
        S.barrier()

    S.op("sp", None, extra_deps=out_stores)
    S.emit()
    return nc


def _pk(w, kc):
    K, N = w.shape
    return np.ascontiguousarray(w.reshape(kc, 128, N).transpose(1, 0, 2))


def _rope_tables():
    f = np.arange(16, dtype=np.float32)
    inv = np.power(np.float32(10000.0), -f / np.float32(16)).astype(np.float32)
    t = np.arange(2048)
    pos = np.stack([(t // 64).astype(np.float32), (t % 64).astype(np.float32)], 0)
    cos = np.zeros((128, 2048), np.float32)
    sin = np.zeros((128, 2048), np.float32)
    perm = np.zeros((128, 128), np.float32)
    for p in range(128):
        d = p % 64
        axis, half, fi = d // 32, (d % 32) // 16, d % 16
        ang = (pos[axis] * inv[fi]).astype(np.float32)
        cos[p] = np.cos(ang)
        sin[p] = np.sin(ang) * (-1.0 if half == 0 else 1.0)
        partner = p + 16 if half == 0 else p - 16
        perm[partner, p] = 1.0
    return cos, sin, perm


def _tri():
    j = np.arange(128)[:, None]
    i = np.arange(128)[None, :]
    tri = np.stack([(j <= i), (j >= i), (j > i), (j < i)], 1).astype(np.float32)
    return np.ascontiguousarray(tri)


_NC_CACHE = {}


def kernel(x, c, ctx, c_ctx, w_ada, b_ada, norm_mix_g, norm_ffn_g, w_in, da_lambda, da_subln_g, gla_gate_w2,
           gla_gate_b, gla_norm_g, w_out, router_group_w, router_group_b, router_expert_w, router_expert_b,
           expert_w_gate, expert_w_up, expert_w_down, final_norm_g):
    f = lambda a: np.asarray(a, dtype=np.float32)
    x, c, ctx, c_ctx = f(x), f(c), f(ctx), f(c_ctx)
    n_cores = 8
    cos, sin, perm = _rope_tables()
    w2 = np.zeros((32, 512), np.float32)
    gw2 = f(gla_gate_w2)[0]
    w2[0:16, 0:256] = gw2[0]
    w2[16:32, 256:512] = gw2[1]
    shared = {
        "w_ada": _pk(f(w_ada)[0], 8),
        "b_ada": np.ascontiguousarray(f(b_ada)[0].reshape(48, 128).T),
        "g1": np.ascontiguousarray(f(norm_mix_g)[0].reshape(8, 128).T),
        "g2": np.ascontiguousarray(f(norm_ffn_g)[0].reshape(8, 128).T),
        "w_in": _pk(f(w_in)[0], 8),
        "da_lambda": f(da_lambda)[0].reshape(1, 256),
        "subln_g": f(da_subln_g)[0].reshape(1, 128),
        "gla_norm_g": f(gla_norm_g)[0].reshape(1, 128),
        "final_g": f(final_norm_g).reshape(1, D),
        "gate_w2": w2,
        "gate_b": f(gla_gate_b)[0].reshape(1, 512),
        "w_out": _pk(f(w_out)[0], 8),
        "w_router": _pk(np.concatenate([f(router_group_w)[0], f(router_expert_w)[0]], 1), 8),
        "b_router": np.concatenate([f(router_group_b)[0], f(router_expert_b)[0]]).reshape(1, 36),
        "ewg": np.ascontiguousarray(f(expert_w_gate)[0].reshape(NE, 8, 128, 512).transpose(0, 2, 1, 3)),
        "ewu": np.ascontiguousarray(f(expert_w_up)[0].reshape(NE, 8, 128, 512).transpose(0, 2, 1, 3)),
        "ewd": np.ascontiguousarray(f(expert_w_down)[0].reshape(NE, 4, 128, D).transpose(0, 2, 1, 3)),
        "rope_cos": cos, "rope_sin": sin, "rope_perm": perm, "tri": _tri(),
    }
    in_maps = []
    for i in range(n_cores):
        bs = [NB * i + k for k in range(NB)]
        xin = np.stack([np.concatenate([ctx[bb], x[bb]], 0) for bb in bs], 0)
        cvec = np.stack([c[bs[0]], c[bs[1]], c_ctx], 1)
        m = dict(shared)
        m["xin"] = np.ascontiguousarray(xin)
        m["cT"] = np.ascontiguousarray(cvec.reshape(8, 128, 3).transpose(1, 0, 2))
        in_maps.append(m)
    if "nc" not in _NC_CACHE:
        _NC_CACHE["nc"] = build_program()
    res = run_bass_kernel_spmd(_NC_CACHE["nc"], in_maps, core_ids=list(range(n_cores)))
    out = np.concatenate([np.asarray(r["out"]) for r in res.results], 0)
    return out.astype(np.float32)
```

```python
from contextlib import ExitStack
import os
import math
import numpy as np
import concourse.bass as bass
import concourse.mybir as mybir
from concourse.bass_utils import run_bass_kernel_spmd

F32 = mybir.dt.float32
BF16 = mybir.dt.bfloat16
AF = mybir.ActivationFunctionType
ALU = mybir.AluOpType
AX = mybir.AxisListType

ENGS = ("pe", "act", "dve", "pool", "sp")
NB = 2
NT = 18
TL = 16
D = 1024
EPS = 1e-6
NE = 32


class Buf:
    __slots__ = ("name", "last_writer", "readers")

    def __init__(self, name):
        self.name = name
        self.last_writer = None
        self.readers = []


class Op:
    __slots__ = ("eng", "fn", "deps", "is_dma", "signal", "semval", "key", "dma_val")

    def __init__(self, eng, fn):
        self.eng = eng
        self.fn = fn
        self.deps = set()
        self.is_dma = False
        self.signal = False
        self.semval = 0
        self.key = None
        self.dma_val = 0


class Sched:
    def __init__(self, nc):
        self.nc = nc
        self.ops = {e: [] for e in ENGS}
        self.dma_keys = {}
        self.bufs = {}
        self.last = {e: None for e in ENGS}

    def buf(self, name):
        b = self.bufs.get(name)
        if b is None:
            b = Buf(name)
            self.bufs[name] = b
        return b

    def op(self, eng, fn, reads=(), writes=(), dma_key=None, extra_deps=()):
        o = Op(eng, fn)
        o.deps.update(extra_deps)
        reads = [self.buf(r) for r in reads]
        writes = [self.buf(w) for w in writes]
        for r in reads:
            if r.last_writer is not None:
                o.deps.add(r.last_writer)
        for w in writes:
            if w.last_writer is not None:
                o.deps.add(w.last_writer)
            o.deps.update(w.readers)
        for r in reads:
            r.readers.append(o)
        for w in writes:
            w.last_writer = o
            w.readers = []
        if dma_key is not None:
            o.is_dma = True
            o.key = dma_key
            st = self.dma_keys.setdefault(dma_key, [0, None])
            if st[1] is not None:
                o.deps.add(st[1])
            st[0] += 16
            st[1] = o
            o.dma_val = st[0]
        o.deps.discard(o)
        if eng == "pe":
            o.deps = {d for d in o.deps if d.is_dma or d.eng != "pe"}
        for d in o.deps:
            d.signal = True
        self.ops[eng].append(o)
        if not o.is_dma and fn is not None:
            self.last[eng] = o
        return o

    def barrier(self):
        deps = [o for o in self.last.values() if o is not None]
        deps += [st[1] for st in self.dma_keys.values() if st[1] is not None]
        for e in ENGS:
            self.op(e, None, extra_deps=deps)

    def emit(self):
        nc = self.nc
        with ExitStack() as es:
            esem = {e: es.enter_context(nc.semaphore("s_" + e)) for e in ENGS}
            dsem = {k: es.enter_context(nc.semaphore("d%d" % i)) for i, k in enumerate(self.dma_keys)}
            for e in ENGS:
                c = 0
                for o in self.ops[e]:
                    if o.signal and not o.is_dma and o.fn is not None:
                        c += 1
                        o.semval = c
            block = es.enter_context(nc.Block())

            self.icount = {}

            def run(ename, eng):
                known = {}
                ic = 0
                for o in self.ops[ename]:
                    self.icount[ename] = ic
                    need = {}
                    for d in o.deps:
                        if d.is_dma:
                            s, v = dsem[d.key], d.dma_val
                        else:
                            s, v = esem[d.eng], d.semval
                        if v > need.get(s, (0, None))[0]:
                            need[s] = (v, s)
                    for v, s in need.values():
                        if known.get(s, 0) < v:
                            eng.wait_ge(s, v)
                            known[s] = v
                            ic += 1
                    if o.fn is None:
                        continue
                    ins = o.fn(eng)
                    ic += 1
                    if o.is_dma:
                        ins.then_inc(dsem[o.key], 16)
                    elif o.signal:
                        ins.then_inc(esem[ename], 1)

            block.tensor(lambda eng: run("pe", eng))
            block.scalar(lambda eng: run("act", eng))
            block.vector(lambda eng: run("dve", eng))
            block.gpsimd(lambda eng: run("pool", eng))
            block.sync(lambda eng: run("sp", eng))


class TV:
    def __init__(self, ap, name):
        self.ap = ap
        self.name = name

    def b(self, i=None):
        return self.name if i is None else "%s#%s" % (self.name, i)


class Arena:
    def __init__(self, nc, nbytes):
        self.cap = nbytes
        self.t = nc.alloc_sbuf_tensor("arena", [128, nbytes // 4], F32)
        self.top = 0
        self.cnt = 0
        self.peak = 0

    def alloc(self, name, shape, dtype):
        esz = 4 if dtype == F32 else 2
        free = 1
        for s in shape[1:]:
            free *= s
        nbytes = (free * esz + 63) // 64 * 64
        off = self.top
        self.top += nbytes
        self.peak = max(self.peak, self.top)
        assert self.top <= self.cap, ("SBUF arena overflow", name, self.top)
        ap = self.t[0:shape[0], off // 4:(off + nbytes) // 4]
        if esz == 2:
            ap = ap.bitcast(BF16)
        ap = ap[:, 0:free]
        if len(shape) == 3:
            ap = ap.rearrange("p (a b) -> p a b", a=shape[1], b=shape[2])
        elif len(shape) == 4:
            ap = ap.rearrange("p (a b c) -> p a b c", a=shape[1], b=shape[2], c=shape[3])
        self.cnt += 1
        return TV(ap, "%s@%d" % (name, self.cnt))


def build_program(stop=None):
    nc = bass.Bass("TRN2", target_bir_lowering=False)
    S = Sched(nc)

    def din(name, shape):
        return nc.dram_tensor(name, list(shape), F32, kind="ExternalInput").ap()

    xin = din("xin", [NB, NT * 128, D])
    cT_d = din("cT", [128, 8, 3])
    wada_d = din("w_ada", [128, 8, 6 * D])
    bada_d = din("b_ada", [128, 48])
    g1_d = din("g1", [128, 8])
    g2_d = din("g2", [128, 8])
    win_d = din("w_in", [128, 8, 3104])
    lam_d = din("da_lambda", [1, 256])
    subln_d = din("subln_g", [1, 128])
    glang_d = din("gla_norm_g", [1, 128])
    fng_d = din("final_g", [1, D])
    w2_d = din("gate_w2", [32, 512])
    gb_d = din("gate_b", [1, 512])
    wout_d = din("w_out", [128, 8, D])
    wr_d = din("w_router", [128, 8, 36])
    br_d = din("b_router", [1, 36])
    ewg_d = din("ewg", [NE, 128, 8, 512])
    ewu_d = din("ewu", [NE, 128, 8, 512])
    ewd_d = din("ewd", [NE, 128, 4, D])
    cos_d = din("rope_cos", [128, 2048])
    sin_d = din("rope_sin", [128, 2048])
    perm_d = din("rope_perm", [128, 128])
    tri_d = din("tri", [128, 4, 128])
    out_d = nc.dram_tensor("out", [NB, TL * 128, D], F32, kind="ExternalOutput").ap()
    mods_d = nc.dram_tensor("mods_scr", [NB, 48, 128], F32, kind="Internal").ap()
    x1s_d = nc.dram_tensor("x1_scr", [NB, TL * 128, D], F32, kind="Internal").ap()

    A = Arena(nc, 200 * 1024)
    ps = [nc.alloc_psum_tensor("ps%d" % i, [128, 512], F32) for i in range(8)]

    def PB(i):
        return "psb%d" % i

    def MM(out, lhsT, rhs, start, stop, r, w, sgc=False):
        S.op("pe", lambda e: e.matmul(out, lhsT=lhsT, rhs=rhs, start=start, stop=stop, skip_group_check=sgc), r, w)

    def TR(out, in_, ident, r, w):
        S.op("pe", lambda e: e.transpose(out, in_, ident), r, w)

    def ACT(out, in_, func, r, w, bias=None, scale=None, accum=None):
        kw = {}
        if bias is not None:
            kw["bias"] = bias
        if scale is not None:
            kw["scale"] = scale
        if accum is not None:
            kw["accum_out"] = accum
        S.op("act", lambda e: e.activation(out=out, in_=in_, func=func, **kw), r, w)

    def TT(eng, out, in0, in1, op, r, w):
        S.op(eng, lambda e: e.tensor_tensor(out=out, in0=in0, in1=in1, op=op), r, w)

    def TS(eng, out, in0, s1, s2, op0, op1, r, w):
        if op1 is None:
            S.op(eng, lambda e: e.tensor_scalar(out=out, in0=in0, scalar1=s1, scalar2=None, op0=op0), r, w)
        else:
            S.op(eng, lambda e: e.tensor_scalar(out=out, in0=in0, scalar1=s1, scalar2=s2, op0=op0, op1=op1), r, w)

    def STT(eng, out, in0, sc, in1, op0, op1, r, w):
        S.op(eng, lambda e: e.scalar_tensor_tensor(out=out, in0=in0, scalar=sc, in1=in1, op0=op0, op1=op1), r, w)

    def CP(eng, out, in_, r, w):
        if eng == "act":
            S.op("act", lambda e: e.activation(out=out, in_=in_, func=AF.Identity), r, w)
        else:
            S.op(eng, lambda e: e.tensor_copy(out=out, in_=in_), r, w)

    def RECIP(out, in_, r, w):
        S.op("dve", lambda e: e.reciprocal(out=out, in_=in_), r, w)

    def RMAX(out, in_, r, w):
        S.op("dve", lambda e: e.reduce_max(out=out, in_=in_, axis=AX.X), r, w)

    def RSUM(out, in_, r, w):
        S.op("dve", lambda e: e.reduce_sum(out=out, in_=in_, axis=AX.X), r, w)

    def MSET(eng, ap, val, w):
        S.op(eng, lambda e: e.memset(ap, val), (), w)

    def DMA(q, out, in_, r, w, key):
        return S.op(q, lambda e: e.dma_start(out=out, in_=in_), r, w, dma_key=key)

    def finish(dumps):
        outs = []
        for i, (name, ap, bufs) in enumerate(dumps):
            d = nc.dram_tensor("dbg_" + name, list(ap.shape), ap.dtype, kind="ExternalOutput").ap()
            outs.append(DMA("sp", d, ap, bufs, (), "dbg%d" % i))
        S.op("sp", None, extra_deps=outs)
        S.emit()
        print("ICOUNT", S.icount, {e: max([o.semval for o in S.ops[e]] + [0]) for e in ENGS}, {k: v[0] for k, v in S.dma_keys.items()})
        return nc

    identb = A.alloc("identb", [128, 128], BF16)
    identf = A.alloc("identf", [128, 128], F32)
    permb = A.alloc("permb", [128, 128], BF16)
    tri = A.alloc("tri", [128, 4, 128], F32)
    epsb = A.alloc("epsb", [128, 1], F32)
    onesf = A.alloc("onesf", [128, 1], F32)
    onesrow = A.alloc("onesrow", [1, 128], BF16)
    g1 = A.alloc("g1", [128, 8], F32)
    g2 = A.alloc("g2", [128, 8], F32)
    bada = A.alloc("bada", [128, 48], F32)
    cT = A.alloc("cT", [128, 8, 3], F32)
    scT = A.alloc("scT", [128, 8, 3], BF16)
    modT = A.alloc("modT", [128, 48, 3], F32)
    A1 = A.alloc("A1", [128, 3, 8], F32)
    A2 = A.alloc("A2", [128, 3, 8], F32)
    wr32 = A.alloc("wr32", [128, 8, 36], F32)
    brb = A.alloc("brb", [128, 36], F32)
    w2blk = A.alloc("w2blk", [32, 512], BF16)
    gbrow = A.alloc("gbrow", [1, 512], BF16)
    sublnb = A.alloc("sublnb", [128, 128], F32)
    glangb = A.alloc("glangb", [128, 128], F32)
    lamb = A.alloc("lamb", [128, 256], F32)
    lamt = A.alloc("lamt", [128, 8], F32)
    Wd = A.alloc("Wd", [128, TL, NE], F32)
    hT = A.alloc("hT", [128, 8, NT * 128], BF16)

    for tv, val in ((identb, 1.0), (identf, 1.0)):
        MSET("pool", tv.ap, val, [tv.b()])
        S.op("pool", (lambda ap: lambda e: e.affine_select(out=ap, in_=ap, pattern=[[-1, 128]],
                                                             compare_op=ALU.is_equal, fill=0.0, base=0,
                                                             channel_multiplier=1))(tv.ap),
             [tv.b()], [tv.b()])
    MSET("pool", epsb.ap, EPS, [epsb.b()])
    MSET("pool", onesf.ap, 1.0, [onesf.b()])
    MSET("pool", onesrow.ap, 1.0, [onesrow.b()])
    DMA("pool", permb.ap, perm_d, (), [permb.b()], "permb")
    DMA("sp", tri.ap, tri_d, (), [tri.b()], "tri")
    DMA("sp", g1.ap, g1_d, (), [g1.b()], "g1")
    DMA("sp", g2.ap, g2_d, (), [g2.b()], "g2")
    DMA("sp", bada.ap, bada_d, (), [bada.b()], "bada")
    DMA("sp", cT.ap, cT_d, (), [cT.b()], "cT")
    DMA("sp", wr32.ap, wr_d, (), [wr32.b()], "wr32")
    DMA("sp", brb.ap, br_d.partition_broadcast(128), (), [brb.b()], "brb")
    DMA("pool", w2blk.ap, w2_d, (), [w2blk.b()], "w2blk")
    DMA("pool", gbrow.ap, gb_d, (), [gbrow.b()], "gbrow")
    DMA("sp", sublnb.ap, subln_d.partition_broadcast(128), (), [sublnb.b()], "sublnb")
    DMA("sp", glangb.ap, glang_d.partition_broadcast(128), (), [glangb.b()], "glangb")
    DMA("sp", lamb.ap, lam_d.partition_broadcast(128), (), [lamb.b()], "lamb")
    lambda_init = 0.8 - 0.6 * math.exp(0.0)
    TS("dve", sublnb.ap, sublnb.ap, 1.0 - lambda_init, None, ALU.mult, None, [sublnb.b()], [sublnb.b()])
    lt = lamt.ap
    lb = lamb.ap
    TT("dve", lb[:, 0:64], lb[:, 0:64], lb[:, 64:128], ALU.mult, [lamb.b()], [lamb.b()])
    TT("dve", lb[:, 128:192], lb[:, 128:192], lb[:, 192:256], ALU.mult, [lamb.b()], [lamb.b()])
    RSUM(lt[:, 0:1], lb[:, 0:64], [lamb.b()], [lamt.b()])
    RSUM(lt[:, 1:2], lb[:, 128:192], [lamb.b()], [lamt.b()])
    ACT(lt[:, 2:4], lt[:, 0:2], AF.Exp, [lamt.b()], [lamt.b()])
    TT("dve", lt[:, 4:5], lt[:, 3:4], lt[:, 2:3], ALU.subtract, [lamt.b()], [lamt.b()])
    TS("dve", lt[:, 7:8], lt[:, 4:5], -lambda_init, None, ALU.add, None, [lamt.b()], [lamt.b()])
    nlam = lt[:, 7:8]

    ACT(scT.ap, cT.ap, AF.Silu, [cT.b()], [scT.b()])
    m0 = A.top
    wb = [A.alloc("wbuf%d" % i, [128, 8, 512], BF16) for i in range(2)]
    psA = ps[0][:, 0:144]
    for grp in range(12):
        w = wb[grp % 2]
        DMA("pool", w.ap, wada_d[:, :, grp * 512:(grp + 1) * 512], (), [w.b()], "wbuf%d" % (grp % 2))
        for c in range(4):
            fo = grp * 4 + c
            for kc in range(8):
                MM(psA[:, fo * 3:fo * 3 + 3], w.ap[:, kc, c * 128:(c + 1) * 128], scT.ap[:, kc, :],
                   kc == 0, kc == 7, [w.b(), scT.b()], [PB(0)])
    psA3 = psA.rearrange("p (a b) -> p a b", a=48, b=3)
    for j in range(3):
        TT("dve", modT.ap[:, :, j], psA3[:, :, j], bada.ap, ALU.add, [PB(0), bada.b()], [modT.b()])
        STT("dve", A1.ap[:, j, :], modT.ap[:, 8:16, j], 1.0, g1.ap, ALU.add, ALU.mult, [modT.b(), g1.b()], [A1.b()])
        STT("dve", A2.ap[:, j, :], modT.ap[:, 32:40, j], 1.0, g2.ap, ALU.add, ALU.mult, [modT.b(), g2.b()], [A2.b()])
    mtmp = A.alloc("mtmp", [128, 48], F32)
    mrow = A.alloc("mrow", [48, 128], F32)
    for j in range(NB):
        CP("dve", mtmp.ap, modT.ap[:, :, j], [modT.b()], [mtmp.b()])
        TR(ps[1][0:48, 0:128], mtmp.ap, identf.ap, [mtmp.b(), identf.b()], [PB(1)])
        CP("dve", mrow.ap, ps[1][0:48, 0:128], [PB(1)], [mrow.b()])
        DMA("sp", mods_d[j], mrow.ap, [mrow.b()], ["mods"], "mods_w")
    S.barrier()
    if stop == 0:
        g1t = A.alloc("g1t", [128, D], F32)
        DMA("sp", g1t.ap, mods_d[0, 16:24, :].rearrange("(o a) b -> o (a b)", o=1).partition_broadcast(128), ["mods"], [g1t.b()], "gt1b")
        return finish([("modT", modT.ap, [modT.b()]), ("A1", A1.ap, [A1.b()]), ("A2", A2.ap, [A2.b()]),
                       ("lamt", lamt.ap, [lamt.b()]), ("gt1b", g1t.ap, [g1t.b()]), ("subln", sublnb.ap, [sublnb.b()]),
                       ("identb", identb.ap, [identb.b()])])
    A.top = m0
    base_top = A.top
    if os.environ.get("PAD"):
        pd_ = A.alloc("pad", [128, 8], F32)
        base_top = A.top
        for i in range(int(os.environ["PAD"])):
            MSET("dve", pd_.ap, float(i), [pd_.b()])

    def rms_stats(src_ap, src_b, junk, st, n):
        MSET("pool", st.ap[:, 0:1], 0.0, [st.b()])
        ACT(junk.ap, src_ap, AF.Square, src_b, [junk.b(), st.b()], accum=st.ap[:, 0:1])
        ACT(st.ap[:, 1:2], st.ap[:, 0:1], AF.Sqrt, [st.b(), epsb.b()], [st.b()], bias=epsb.ap, scale=1.0 / n)
        RECIP(st.ap[:, 1:2], st.ap[:, 1:2], [st.b()], [st.b()])

    out_stores = []
    TOKB = [(0, 512), (512, 512), (1024, 512), (1536, 512), (2048, 256)]
    for b in range(NB):
        A.top = base_top
        mixer_mark = A.top
        mix = A.alloc("mix", [128, TL, D], BF16)
        att_mark = A.top
        cosb = A.alloc("cosb", [128, 2048], BF16)
        sinb = A.alloc("sinb", [128, 2048], BF16)
        QT = A.alloc("QT", [128, 4, 2048], BF16)
        KT = A.alloc("KT", [128, 4, NT * 128], BF16)
        V = A.alloc("V", [128, NT, 4, 130], BF16)
        DMA("pool", cosb.ap, cos_d, (), [cosb.b()], "cosb")
        DMA("pool", sinb.ap, sin_d, (), [sinb.b()], "sinb")
        MSET("pool", V.ap[:, :, :, 128:130], 1.0, [V.b("ones")])
        pa_mark = A.top
        xt = [A.alloc("xt%d" % i, [128, D], F32) for i in range(2)]
        xs = [A.alloc("xs%d" % i, [128, D], BF16) for i in range(2)]
        junk = A.alloc("junk", [128, D], BF16)
        st = [A.alloc("st%d" % i, [128, 2], F32) for i in range(2)]
        for t in range(NT):
            j = 2 if t < 2 else b
            x_, s_, t_ = xt[t % 2], xs[t % 2], st[t % 2]
            DMA("sp", x_.ap, xin[b, t * 128:(t + 1) * 128, :], (), [x_.b()], "xt%d" % (t % 2))
            rms_stats(x_.ap, [x_.b()], junk, t_, D)
            S.op("dve", (lambda o, i, s: lambda e: e.tensor_scalar_mul(out=o, in0=i, scalar1=s))(s_.ap, x_.ap, t_.ap[:, 1:2]),
                 [x_.b(), t_.b()], [s_.b()])
            pb = 2 + (t % 2)
            pT = ps[pb][:].bitcast(BF16)
            for kc in range(8):
                TR(pT[:, kc * 128:(kc + 1) * 128], s_.ap[:, kc * 128:(kc + 1) * 128], identb.ap,
                   [s_.b(), identb.b()], [PB(pb)])
            for kc in range(8):
                o_ = hT.ap[:, kc, t * 128:(t + 1) * 128]
                i_ = pT[:, kc * 128:(kc + 1) * 128]
                if kc % 2 == 0:
                    ACT(o_, i_, AF.Identity, [PB(pb), A1.b(), modT.b()], [hT.b(t)],
                        bias=modT.ap[:, kc, j:j + 1], scale=A1.ap[:, j, kc:kc + 1])
                else:
                    TS("dve", o_, i_, A1.ap[:, j, kc:kc + 1], modT.ap[:, kc, j:j + 1], ALU.mult, ALU.add,
                       [PB(pb), A1.b(), modT.b()], [hT.b(t)])
        hT_all = [hT.b(t) for t in range(NT)]
        if stop == 1:
            S.barrier()
            return finish([("hT", hT.ap, hT_all)])

        wb = [A.alloc("wbuf%d" % i, [128, 8, 512], BF16) for i in range(2)]
        qraw = [A.alloc("qraw%d" % i, [128, 512], BF16) for i in range(2)]
        rt1 = [A.alloc("rt1_%d" % i, [128, 512], F32) for i in range(2)]
        rt2 = [A.alloc("rt2_%d" % i, [128, 512], F32) for i in range(2)]
        cnt = 0
        for gi, (dst, col0) in enumerate(((QT, 0), (KT, 512))):
            w = wb[gi % 2]
            DMA("pool", w.ap, win_d[:, :, col0:col0 + 512], (), [w.b()], "wbuf%d" % (gi % 2))
            for h in range(4):
                if dst is KT:
                    pb = cnt % 2
                    for kc in range(8):
                        MM(ps[pb][:, 0:256], w.ap[:, kc, h * 128:(h + 1) * 128], hT.ap[:, kc, 0:256], kc == 0, kc == 7,
                           [w.b(), hT.b(0), hT.b(1)], [PB(pb)])
                    CP("act", KT.ap[:, h, 0:256], ps[pb][:, 0:256], [PB(pb)], [KT.b("%d_c" % h)])
                    cnt += 1
                for tb_i in range(4):
                    tb = 0 if os.environ.get('TB0') else tb_i
                    pz_ = 0 if os.environ.get("PAR0") else cnt % 2
                    pb = pz_ if not os.environ.get("PARB") else cnt % 2
                    pw = 2 + pz_
                    qr, r1, r2 = qraw[pz_], rt1[pz_], rt2[pz_]
                    tok = 256 + tb * 512
                    hr = [hT.b(2 + tb * 4 + i) for i in range(4)]
                    for kc in range(8):
                        MM(ps[pb][:], w.ap[:, kc, h * 128:(h + 1) * 128], hT.ap[:, kc, tok:tok + 512], kc == 0, kc == 7,
                           [w.b()] + hr, [PB(pb)])
                    CP("act", qr.ap, ps[pb][:], [PB(pb)], [qr.b()])
                    MM(ps[pw][:], permb.ap, qr.ap, True, True, [permb.b(), qr.b()], [PB(pw)])
                    TT("dve", r1.ap, ps[pb][:], cosb.ap[:, tb * 512:(tb + 1) * 512], ALU.mult, [PB(pb), cosb.b(), qr.b()], [r1.b()])
                    TT("dve", r2.ap, ps[pw][:], sinb.ap[:, tb * 512:(tb + 1) * 512], ALU.mult, [PB(pw), sinb.b()], [r2.b()])
                    if dst is QT:
                        o_ = QT.ap[:, h, tb * 512:(tb + 1) * 512]
                        ob = QT.b("%d_%d" % (h, tb))
                    else:
                        o_ = KT.ap[:, h, tok:tok + 512]
                        ob = KT.b("%d_%d" % (h, tb))
                    TT("dve" if os.environ.get("POOLDVE") else "pool", o_, r1.ap, r2.ap, ALU.add, [r1.b(), r2.b()], [ob])
                    if cnt + 1 == int(os.environ.get("CUTN", "0")):
                        S.barrier()
                        return finish([("QT", QT.ap, []), ("KT", KT.ap, [])])
                    cnt += 1
        w = wb[0]
        DMA("pool", w.ap, win_d[:, :, 1024:1536], (), [w.b()], "wbuf0")
        for t in range(NT):
            pb = 4 + t % 2
            for kc in range(8):
                MM(ps[pb][:], hT.ap[:, kc, t * 128:(t + 1) * 128], w.ap[:, kc, :], kc == 0, kc == 7,
                   [w.b(), hT.b(t)], [PB(pb)])
            CP("act" if t % 2 == 0 else "dve", V.ap[:, t, :, 0:128],
               ps[pb][:].rearrange("p (a b) -> p a b", a=4, b=128), [PB(pb)], [V.b(t)])
        S.barrier()
        if stop == 2:
            return finish([("QT", QT.ap, []), ("KT", KT.ap, []), ("V", V.ap, [])])
        A.top = pa_mark

        ET = [A.alloc("ET%d" % i, [128, 512], BF16) for i in range(3)]
        fin = [A.alloc("fin%d" % i, [128, 8], F32) for i in range(2)]
        ta = [A.alloc("ta%d" % i, [128, 128], F32) for i in range(2)]
        tb_ = [A.alloc("tb%d" % i, [128, 128], F32) for i in range(2)]
        junk = A.alloc("junk", [128, 128], BF16)
        Vall = [V.b(t) for t in range(NT)] + [V.b("ones")]
        fcnt = 0
        steps = [(h, qb, m, kt) for h in range(4) for qb in range(4) for m in range(2) for kt in range(NT)]

        def acc_of(h, qb, m, qs):
            par = (h * 4 + qb) % 2
            r = m * 4 + qs
            return ps[2 + par * 3 + r // 3][:, (r % 3) * 129:(r % 3) * 129 + 129], "acc%d_%d" % (par, r)

        def emit_S(i):
            h, qb, m, kt = steps[i]
            Kall = [KT.b("%d_c" % h)] + [KT.b("%d_%d" % (h, j)) for j in range(4)]
            MM(ps[i % 2][:], KT.ap[m * 64:(m + 1) * 64, h, kt * 128:(kt + 1) * 128],
               QT.ap[m * 64:(m + 1) * 64, h, qb * 512:(qb + 1) * 512], True, True,
               Kall + [QT.b("%d_%d" % (h, qb))], [PB(i % 2)])

        started = set()
        emit_S(0)
        for i, (h, qb, m, kt) in enumerate(steps):
            if m == 0 and kt == 0:
                started = set()
            e_ = ET[i % 3]
            ACT(e_.ap, ps[i % 2][:], AF.Exp, [PB(i % 2)], [e_.b()], scale=0.125)
            if i + 1 < len(steps):
                emit_S(i + 1)
            for qs in range(4):
                ap_, bn = acc_of(h, qb, m, qs)
                bank = (m * 4 + qs) // 3
                st_ = kt == 0 and bank not in started
                started.add(bank)
                MM(ap_, e_.ap[:, qs * 128:(qs + 1) * 128], V.ap[:, kt, h, 0:129], st_, kt == NT - 1,
                   [e_.b()] + Vall, [bn], sgc=True)
            if m == 1 and kt == NT - 1:
                for qs in range(4):
                    a1, b1 = acc_of(h, qb, 0, qs)
                    a2, b2 = acc_of(h, qb, 1, qs)
                    f, x_, y_ = fin[fcnt % 2], ta[fcnt % 2], tb_[fcnt % 2]
                    fcnt += 1
                    tq = qb * 4 + qs
                    RECIP(f.ap[:, 0:1], a1[:, 128:129], [b1], [f.b()])
                    RECIP(f.ap[:, 1:2], a2[:, 128:129], [b2], [f.b()])
                    TT("dve", f.ap[:, 2:3], f.ap[:, 1:2], nlam, ALU.mult, [f.b(), lamt.b()], [f.b()])
                    TS("dve", x_.ap, a1[:, 0:128], f.ap[:, 0:1], None, ALU.mult, None, [b1, f.b()], [x_.b()])
                    STT("dve", y_.ap, a2[:, 0:128], f.ap[:, 2:3], x_.ap, ALU.mult, ALU.add, [b2, f.b(), x_.b()], [y_.b()])
                    MSET("pool", f.ap[:, 3:4], 0.0, [f.b()])
                    ACT(junk.ap, y_.ap, AF.Square, [y_.b()], [junk.b(), f.b()], accum=f.ap[:, 3:4])
                    ACT(f.ap[:, 4:5], f.ap[:, 3:4], AF.Sqrt, [f.b(), epsb.b()], [f.b()], bias=epsb.ap, scale=1.0 / 128)
                    RECIP(f.ap[:, 4:5], f.ap[:, 4:5], [f.b()], [f.b()])
                    STT("dve", mix.ap[:, tq, h * 128:(h + 1) * 128], y_.ap, f.ap[:, 4:5], sublnb.ap, ALU.mult, ALU.mult,
                        [y_.b(), f.b(), sublnb.b()], [mix.b(tq)])
        S.barrier()
        if stop == 3:
            return finish([("mix", mix.ap, [])])
        A.top = att_mark

        gqk = A.alloc("gqk", [128, 4, NT * 128], BF16)
        gkt = A.alloc("gkt", [128, NT, 256], BF16)
        gv = A.alloc("gv", [128, NT, 512], BF16)
        lrT = A.alloc("lrT", [32, NT * 128], BF16)
        Sb = A.alloc("Sb", [128, 4, TL, 128], BF16)
        gla_mark = A.top
        wb = [A.alloc("wbuf%d" % i, [128, 8, 512], BF16) for i in range(2)]
        w = wb[0]
        DMA("pool", w.ap, win_d[:, :, 1536:2048], (), [w.b()], "wbuf0")
        cnt = 0
        for c in range(4):
            for (tok, n) in TOKB:
                pb = cnt % 2
                hr = [hT.b(tok // 128 + i) for i in range(n // 128)]
                for kc in range(8):
                    MM(ps[pb][:, 0:n], w.ap[:, kc, c * 128:(c + 1) * 128], hT.ap[:, kc, tok:tok + n], kc == 0, kc == 7,
                       [w.b()] + hr, [PB(pb)])
                CP("act" if cnt % 2 == 0 else "dve", gqk.ap[:, c, tok:tok + n], ps[pb][:, 0:n], [PB(pb)], [gqk.b()])
                cnt += 1
        for t in range(NT):
            pb = 2 + t % 2
            for kc in range(8):
                MM(ps[pb][:, 0:256], hT.ap[:, kc, t * 128:(t + 1) * 128], w.ap[:, kc, 256:512], kc == 0, kc == 7,
                   [w.b(), hT.b(t)], [PB(pb)])
            CP("act" if t % 2 == 0 else "dve", gkt.ap[:, t, :], ps[pb][:, 0:256], [PB(pb)], [gkt.b()])
        w = wb[1]
        DMA("pool", w.ap, win_d[:, :, 2048:2560], (), [w.b()], "wbuf1")
        for t in range(NT):
            pb = 4 + t % 2
            for kc in range(8):
                MM(ps[pb][:], hT.ap[:, kc, t * 128:(t + 1) * 128], w.ap[:, kc, :], kc == 0, kc == 7,
                   [w.b(), hT.b(t)], [PB(pb)])
            CP("act" if t % 2 == 0 else "dve", gv.ap[:, t, :], ps[pb][:], [PB(pb)], [gv.b()])
        w = wb[0]
        DMA("pool", w.ap[:, :, 0:32], win_d[:, :, 3072:3104], (), [w.b()], "wbuf0")
        for (tok, n) in TOKB:
            pb = 6 + cnt % 2
            cnt += 1
            hr = [hT.b(tok // 128 + i) for i in range(n // 128)]
            for kc in range(8):
                MM(ps[pb][0:32, 0:n], w.ap[:, kc, 0:32], hT.ap[:, kc, tok:tok + n], kc == 0, kc == 7, [w.b()] + hr, [PB(pb)])
            CP("act", lrT.ap[:, tok:tok + n], ps[pb][0:32, 0:n], [PB(pb)], [lrT.b()])
        S.barrier()
        if stop == 4:
            return finish([("gqk", gqk.ap, []), ("gkt", gkt.ap, []), ("gv", gv.ap, []), ("lrT", lrT.ap, [])])
        A.top = gla_mark

        St = A.alloc("St", [128, 4, 128], F32)
        MSET("dve", St.ap, 0.0, [St.b(i) for i in range(4)])
        G_ = [A.alloc("G%d" % i, [128, 256], F32) for i in range(2)]
        ER = [A.alloc("ER%d" % i, [128, 256], F32) for i in range(2)]
        k2e = [A.alloc("k2e%d" % i, [128, 256], BF16) for i in range(2)]
        dec = [A.alloc("dec%d" % i, [128, 2], F32) for i in range(2)]
        orders = (list(range(NT)), [1, 0] + list(range(NT - 1, 1, -1)))
        cnt = 0
        for s in range(NT):
            for d in range(2):
                t = orders[d][s]
                g_, er, ke, dc = G_[cnt % 2], ER[cnt % 2], k2e[cnt % 2], dec[cnt % 2]
                pz = cnt % 2
                pu = 2 + cnt % 2
                pd = 4 + cnt % 2
                cnt += 1
                MM(ps[pz][:, 0:256], lrT.ap[:, t * 128:(t + 1) * 128], w2blk.ap[:, d * 256:(d + 1) * 256], True, False,
                   [lrT.b(), w2blk.b()], [PB(pz)])
                MM(ps[pz][:, 0:256], onesrow.ap, gbrow.ap[:, d * 256:(d + 1) * 256], False, True,
                   [onesrow.b(), gbrow.b()], [PB(pz)])
                ACT(g_.ap, ps[pz][:, 0:256], AF.Exp, [PB(pz)], [g_.b()], scale=-1.0)
                ACT(g_.ap, g_.ap, AF.Ln, [g_.b()], [g_.b()], bias=1.0)
                MM(ps[pz][:, 256:512], tri.ap[:, 2 + d, :], g_.ap, True, True, [tri.b(), g_.b()], [PB(pz)])
                ACT(er.ap, ps[pz][:, 256:512], AF.Exp, [PB(pz)], [er.b()], scale=-1.0 / 16)
                TT("dve", ke.ap, gkt.ap[:, t, :], er.ap, ALU.mult, [gkt.b(), er.b()], [ke.b()])
                for p in range(2):
                    MM(ps[pd][:, p:p + 1], g_.ap[:, p * 128:(p + 1) * 128], onesf.ap, True, True, [g_.b(), onesf.b()], [PB(pd)])
                    MM(ps[pu][:, p * 256:(p + 1) * 256], ke.ap[:, p * 128:(p + 1) * 128], gv.ap[:, t, p * 256:(p + 1) * 256],
                       True, True, [ke.b(), gv.b()], [PB(pu)])
                ACT(dc.ap, ps[pd][:, 0:2], AF.Exp, [PB(pd)], [dc.b()], scale=-1.0 / 16)
                for p in range(2):
                    i = d * 2 + p
                    if t >= 2:
                        CP("pool", Sb.ap[:, i, t - 2, :], St.ap[:, i, :], [St.b(i)], [Sb.b()])
                    for hh in range(2):
                        r0, r1_ = hh * 64, hh * 64 + 64
                        STT("dve", St.ap[r0:r1_, i, :], St.ap[r0:r1_, i, :], dc.ap[r0:r1_, p:p + 1],
                            ps[pu][r0:r1_, p * 256 + hh * 128:p * 256 + hh * 128 + 128], ALU.mult, ALU.add,
                            [St.b(i), dc.b(), PB(pu)], [St.b(i)])
        S.barrier()
        if stop == 5:
            return finish([("Sb", Sb.ap, []), ("St", St.ap, [])])
        A.top = gla_mark

        G2 = [A.alloc("G2_%d" % i, [128, 512], F32) for i in range(2)]
        eb = [A.alloc("eb%d" % i, [128, 512], F32) for i in range(2)]
        ei = [A.alloc("ei%d" % i, [128, 512], F32) for i in range(2)]
        qd = [A.alloc("qd%d" % i, [128, 2, 2, 128], BF16) for i in range(2)]
        ki = [A.alloc("ki%d" % i, [128, 2, 2, 128], BF16) for i in range(2)]
        qz = [A.alloc("qz%d" % i, [128, 2, 4, 128], BF16) for i in range(2)]
        for i in range(2):
            MSET("pool", qz[i].ap, 0.0, [qz[i].b()])
        sTm = [A.alloc("sTm%d" % i, [128, 2, 4, 128], BF16) for i in range(2)]
        osq = [A.alloc("osq%d" % i, [128, 4, 128], F32) for i in range(2)]
        fs = [A.alloc("fs%d" % i, [128, 8], F32) for i in range(2)]
        for t in range(2, NT):
            k = t % 2
            g_, eb_, ei_, qd_, ki_, sm, oq, f = G2[k], eb[k], ei[k], qz[k], ki[k], sTm[k], osq[k], fs[k]
            pz, pbk, ps0, ps1, po = 0 + k, 2 + k, 4, 5, 6 + k
            MM(ps[pz][:], lrT.ap[:, t * 128:(t + 1) * 128], w2blk.ap, True, False, [lrT.b(), w2blk.b()], [PB(pz)])
            MM(ps[pz][:], onesrow.ap, gbrow.ap, False, True, [onesrow.b(), gbrow.b()], [PB(pz)])
            ACT(g_.ap, ps[pz][:], AF.Exp, [PB(pz)], [g_.b()], scale=-1.0)
            ACT(g_.ap, g_.ap, AF.Ln, [g_.b()], [g_.b()], bias=1.0)
            for d in range(2):
                for p in range(2):
                    c0 = (d * 2 + p) * 128
                    MM(ps[pbk][:, c0:c0 + 128], g_.ap[:, d * 256 + p * 128:d * 256 + p * 128 + 128], tri.ap[:, d, :], True, True,
                       [g_.b(), tri.b()], [PB(pbk)])
            ACT(eb_.ap, ps[pbk][:], AF.Exp, [PB(pbk)], [eb_.b()], scale=-1.0 / 16)
            ACT(ei_.ap, ps[pbk][:], AF.Exp, [PB(pbk)], [ei_.b()], scale=1.0 / 16)
            if os.environ.get("P2CUT") == "2":
                S.barrier()
                return finish([("f", f.ap, [])])

            for d in range(2):
                for p in range(2):
                    c0 = (d * 2 + p) * 128
                    for hh in range(2):
                        q0 = hh * 64
                        STT("dve", qd_.ap[q0:q0 + 64, d, 2 * p + hh, :], gqk.ap[q0:q0 + 64, p, t * 128:(t + 1) * 128], 0.125,
                            eb_.ap[q0:q0 + 64, c0:c0 + 128], ALU.mult, ALU.mult, [gqk.b(), eb_.b()], [qd_.b()])
                    TT("dve", ki_.ap[:, d, p, :], gqk.ap[:, 2 + p, t * 128:(t + 1) * 128], ei_.ap[:, c0:c0 + 128], ALU.mult,
                       [gqk.b(), ei_.b()], [ki_.b()])
            if os.environ.get("P2CUT") == "3":
                S.barrier()
                return finish([("f", f.ap, [])])

            for d in range(2):
                psd = ps0 if d == 0 else ps1
                for h in range(4):
                    r0 = (h % 2) * 64
                    MM(ps[psd][:, h * 128:(h + 1) * 128], ki_.ap[:, d, h // 2, :], qd_.ap[:, d, h, :],
                       True, True, [ki_.b(), qd_.b()], [PB(psd)])
                if os.environ.get("P2CUT") == "41":
                    S.barrier()
                    return finish([("f", f.ap, [])])
                for h in range(4):
                    TT("dve", sm.ap[:, d, h, :], ps[psd][:, h * 128:(h + 1) * 128], tri.ap[:, d, :], ALU.mult,
                       [PB(psd), tri.b()], [sm.b()])
                if os.environ.get("P2CUT") == "42":
                    S.barrier()
                    return finish([("f", f.ap, [])])
            if os.environ.get("P2CUT") == "4":
                S.barrier()
                return finish([("f", f.ap, [])])

            for h in range(4):
                r0 = (h % 2) * 64
                o_ = ps[po][:, h * 128:(h + 1) * 128]
                MM(o_, sm.ap[:, 0, h, :], gv.ap[:, t, h * 128:(h + 1) * 128], True, False, [sm.b(), gv.b()], [PB(po)])
                MM(o_, sm.ap[:, 1, h, :], gv.ap[:, t, h * 128:(h + 1) * 128], False, True, [sm.b(), gv.b()], [PB(po)])
            if os.environ.get("P2CUT") == "5":
                S.barrier()
                return finish([("f", f.ap, [])])

            for h in range(4):
                r0 = (h % 2) * 64
                o_ = ps[pz][:, h * 128:(h + 1) * 128]
                MM(o_, qd_.ap[:, 0, h, :], Sb.ap[:, 0 + h // 2, t - 2, :], True, False, [qd_.b(), Sb.b()], [PB(pz)])
                MM(o_, qd_.ap[:, 1, h, :], Sb.ap[:, 2 + h // 2, t - 2, :], False, True, [qd_.b(), Sb.b()], [PB(pz)])
            osb = oq
            ACT(osb.ap.rearrange("p a b -> p (a b)"), ps[pz][:], AF.Identity, [PB(pz)], [oq.b()])
            TT("dve", osb.ap.rearrange("p a b -> p (a b)"), ps[po][:], osb.ap.rearrange("p a b -> p (a b)"), ALU.add,
               [PB(po), oq.b()], [oq.b()])
            if os.environ.get("P2CUT") == "6":
                S.barrier()
                return finish([("f", f.ap, [])])

            o3 = osb.ap
            sq_ = eb_.ap.rearrange("p (a b) -> p a b", a=4, b=128)
            MSET("pool", f.ap[:, 0:4], 0.0, [f.b()])
            for h in range(4):
                ACT(sq_[:, h, :], o3[:, h, :], AF.Square, [oq.b()], [eb_.b(), f.b()], accum=f.ap[:, h:h + 1])
            ACT(f.ap[:, 4:8], f.ap[:, 0:4], AF.Sqrt, [f.b(), epsb.b()], [f.b()], bias=epsb.ap, scale=1.0 / 128)
            RECIP(f.ap[:, 4:8], f.ap[:, 4:8], [f.b()], [f.b()])
            for h in range(4):
                STT("dve", mix.ap[:, t - 2, 512 + h * 128:512 + (h + 1) * 128], o3[:, h, :], f.ap[:, 4 + h:5 + h], glangb.ap,
                    ALU.mult, ALU.mult, [oq.b(), f.b(), glangb.b()], [mix.b(t - 2)])
            if os.environ.get("P2CUT") == "7":
                S.barrier()
                return finish([("f", f.ap, [])])

        S.barrier()
        if stop == 6:
            return finish([("mix", mix.ap, [])])
        A.top = att_mark

        wb = [A.alloc("wbuf%d" % i, [128, 8, 512], BF16) for i in range(1)]
        sg = [A.alloc("sg%d" % i, [128, 512], F32) for i in range(2)]
        w = wb[0]
        DMA("pool", w.ap, win_d[:, :, 2560:3072], (), [w.b()], "wbuf0")
        for t in range(TL):
            pb = t % 2
            s_ = sg[t % 2]
            for kc in range(8):
                MM(ps[pb][:], hT.ap[:, kc, (t + 2) * 128:(t + 3) * 128], w.ap[:, kc, :], kc == 0, kc == 7,
                   [w.b(), hT.b(t + 2)], [PB(pb)])
            ACT(s_.ap, ps[pb][:], AF.Silu, [PB(pb)], [s_.b()])
            TT("dve" if t % 2 == 0 else "pool", mix.ap[:, t, 512:1024], mix.ap[:, t, 512:1024], s_.ap, ALU.mult,
               [mix.b(t), s_.b()], [mix.b(t)])
        S.barrier()
        if stop == 7:
            return finish([("mix", mix.ap, [])])
        A.top = att_mark

        wo = A.alloc("wo", [128, 8, D], BF16)
        gt1b = A.alloc("gt1b", [128, D], F32)
        DMA("pool", wo.ap, wout_d, (), [wo.b()], "wo")
        DMA("sp", gt1b.ap, mods_d[b, 16:24, :].rearrange("(o a) b -> o (a b)", o=1).partition_broadcast(128), ["mods"], [gt1b.b()], "gt1b")
        mixT = [A.alloc("mixT%d" % i, [128, 8, 128], BF16) for i in range(2)]
        xt = [A.alloc("xt%d" % i, [128, D], F32) for i in range(2)]
        x1 = [A.alloc("x1_%d" % i, [128, D], F32) for i in range(2)]
        ytmp = A.alloc("ytmp", [128, D], F32)
        xs2 = A.alloc("xs2", [128, D], F32)
        h32 = A.alloc("h32", [128, 8, 128], F32)
        junk = A.alloc("junk", [128, D], BF16)
        st = [A.alloc("st%d" % i, [128, 2], F32) for i in range(2)]
        rt = [A.alloc("rt%d" % i, [128, 160], F32) for i in range(2)]
        h2T = hT
        for t in range(TL):
            k = t % 2
            mT, x_, x1_, s_, r_ = mixT[k], xt[k], x1[k], st[k], rt[k]
            DMA("sp", x_.ap, xin[b, (t + 2) * 128:(t + 3) * 128, :], (), [x_.b()], "xt%d" % k)
            pT = ps[0][:].bitcast(BF16)
            for kc in range(8):
                TR(pT[:, kc * 128:(kc + 1) * 128], mix.ap[:, t, kc * 128:(kc + 1) * 128], identb.ap, [mix.b(t), identb.b()], [PB(0)])
            CP("act", mT.ap, pT.rearrange("p (a b) -> p a b", a=8, b=128), [PB(0)], [mT.b()])
            for half in range(2):
                pb = 1 + half
                for kc in range(8):
                    MM(ps[pb][:], mT.ap[:, kc, :], wo.ap[:, kc, half * 512:(half + 1) * 512], kc == 0, kc == 7,
                       [mT.b(), wo.b()], [PB(pb)])
                sl = slice(half * 512, (half + 1) * 512)
                TT("dve", ytmp.ap[:, sl], ps[pb][:], gt1b.ap[:, sl], ALU.mult, [PB(pb), gt1b.b()], [ytmp.b(half)])
                TT("pool", x1_.ap[:, sl], ytmp.ap[:, sl], x_.ap[:, sl], ALU.add, [ytmp.b(half), x_.b()], [x1_.b()])
            DMA("sp", x1s_d[b, t * 128:(t + 1) * 128, :], x1_.ap, [x1_.b()], ["x1s"], "x1_%d" % k)
            rms_stats(x1_.ap, [x1_.b()], junk, s_, D)
            S.op("dve", (lambda o, i, s: lambda e: e.tensor_scalar_mul(out=o, in0=i, scalar1=s))(xs2.ap, x1_.ap, s_.ap[:, 1:2]),
                 [x1_.b(), s_.b()], [xs2.b()])
            for kc in range(8):
                pb = 3 + kc // 4
                TR(ps[pb][:, (kc % 4) * 128:(kc % 4 + 1) * 128], xs2.ap[:, kc * 128:(kc + 1) * 128], identf.ap,
                   [xs2.b(), identf.b()], [PB(pb)])
            for kc in range(8):
                pb = 3 + kc // 4
                i_ = ps[pb][:, (kc % 4) * 128:(kc % 4 + 1) * 128]
                if kc % 2 == 0:
                    ACT(h32.ap[:, kc, :], i_, AF.Identity, [PB(pb), A2.b(), modT.b()], [h32.b()],
                        bias=modT.ap[:, 24 + kc, b:b + 1], scale=A2.ap[:, b, kc:kc + 1])
                else:
                    TS("dve", h32.ap[:, kc, :], i_, A2.ap[:, b, kc:kc + 1], modT.ap[:, 24 + kc, b:b + 1], ALU.mult, ALU.add,
                       [PB(pb), A2.b(), modT.b()], [h32.b()])
            CP("pool", h2T.ap[:, :, t * 128:(t + 1) * 128], h32.ap, [h32.b()], [h2T.b("m%d" % t)])
            for kc in range(8):
                MM(ps[5][:, 0:36], h32.ap[:, kc, :], wr32.ap[:, kc, :], kc == 0, kc == 7, [h32.b(), wr32.b()], [PB(5)])
            R = r_.ap
            rb = [r_.b()]
            lg, els, k1, k2, tmp = R[:, 0:36], R[:, 36:68], R[:, 68:100], R[:, 100:132], R[:, 132:140]
            sc = R[:, 140:160]
            TT("dve", lg, ps[5][:, 0:36], brb.ap, ALU.add, [PB(5), brb.b()], rb)
            RMAX(sc[:, 0:1], lg[:, 0:4], rb, rb)
            TS("dve", tmp[:, 0:4], lg[:, 0:4], sc[:, 0:1], None, ALU.is_equal, None, rb, rb)
            TS("dve", sc[:, 1:2], sc[:, 0:1], -1.0, None, ALU.mult, None, rb, rb)
            MSET("dve", sc[:, 2:3], 0.0, rb)
            ACT(tmp[:, 4:8], lg[:, 0:4], AF.Exp, rb, rb, bias=sc[:, 1:2], accum=sc[:, 2:3])
            RECIP(sc[:, 3:4], sc[:, 2:3], rb, rb)
            TS("dve", tmp[:, 0:4], tmp[:, 0:4], 1e30, -1e30, ALU.mult, ALU.add, rb, rb)
            for g in range(4):
                TS("dve", els[:, g * 8:(g + 1) * 8], lg[:, 4 + g * 8:12 + g * 8], tmp[:, g:g + 1], None, ALU.add, None, rb, rb)
            RMAX(sc[:, 4:5], els, rb, rb)
            TS("dve", k1, els, sc[:, 4:5], None, ALU.is_equal, None, rb, rb)
            STT("dve", els, k1, -1e30, els, ALU.mult, ALU.add, rb, rb)
            RMAX(sc[:, 5:6], els, rb, rb)
            TS("dve", k2, els, sc[:, 5:6], None, ALU.is_equal, None, rb, rb)
            TT("dve", sc[:, 6:7], sc[:, 5:6], sc[:, 4:5], ALU.subtract, rb, rb)
            ACT(sc[:, 7:8], sc[:, 6:7], AF.Exp, rb, rb)
            TS("dve", sc[:, 8:9], sc[:, 7:8], 1.0, None, ALU.add, None, rb, rb)
            RECIP(sc[:, 8:9], sc[:, 8:9], rb, rb)
            TT("dve", sc[:, 9:10], sc[:, 8:9], sc[:, 3:4], ALU.mult, rb, rb)
            TT("dve", sc[:, 10:11], sc[:, 9:10], sc[:, 7:8], ALU.mult, rb, rb)
            TS("dve", k1, k1, sc[:, 9:10], None, ALU.mult, None, rb, rb)
            STT("dve", Wd.ap[:, t, :], k2, sc[:, 10:11], k1, ALU.mult, ALU.add, rb, [Wd.b()])
        S.barrier()
        if stop == 8:
            return finish([("Wd", Wd.ap, []), ("h2T", hT.ap, [])])
        A.top = mixer_mark

        yacc = A.alloc("yacc", [128, TL, D], F32)
        gt2b = A.alloc("gt2b", [128, D], F32)
        rg = [A.alloc("rg%d" % i, [128, 8, 512], BF16) for i in range(2)]
        ru = [A.alloc("ru%d" % i, [128, 8, 512], BF16) for i in range(2)]
        rd = [A.alloc("rd%d" % i, [128, 4, D], BF16) for i in range(2)]
        sgt = [A.alloc("sgt%d" % i, [128, 512], F32) for i in range(2)]
        hTm = [A.alloc("hTm%d" % i, [128, 4, 512], BF16) for i in range(2)]
        DMA("sp", gt2b.ap, mods_d[b, 40:48, :].rearrange("(o a) b -> o (a b)", o=1).partition_broadcast(128), ["mods"], [gt2b.b()], "gt2b")
        for t in range(TL):
            DMA("sp", yacc.ap[:, t, :], x1s_d[b, t * 128:(t + 1) * 128, :], ["x1s"], [yacc.b(t)], "yacc%d" % (t % 4))
        h2all = [h2T.b("m%d" % t) for t in range(TL)]
        cnt = 0
        ycnt = 0
        for e in range(NE):
            k = e % 2
            DMA("pool", rg[k].ap, ewg_d[e], (), [rg[k].b()], "rg%d" % k)
            DMA("pool", ru[k].ap, ewu_d[e], (), [ru[k].b()], "ru%d" % k)
            DMA("pool", rd[k].ap, ewd_d[e], (), [rd[k].b()], "rd%d" % k)
            for hc in range(4):
                TT("pool", rd[k].ap[:, hc, :], rd[k].ap[:, hc, :], gt2b.ap, ALU.mult, [rd[k].b(), gt2b.b()], [rd[k].b()])
            for tb in range(4):
                hm = hTm[tb % 2]
                hr = h2all[tb * 4:tb * 4 + 4]
                for hc in range(4):
                    pg, pu = cnt % 2, 2 + cnt % 2
                    s_ = sgt[cnt % 2]
                    cnt += 1
                    for kc in range(8):
                        MM(ps[pg][:], rg[k].ap[:, kc, hc * 128:(hc + 1) * 128], h2T.ap[:, kc, tb * 512:(tb + 1) * 512],
                           kc == 0, kc == 7, [rg[k].b()] + hr, [PB(pg)])
                    for kc in range(8):
                        MM(ps[pu][:], ru[k].ap[:, kc, hc * 128:(hc + 1) * 128], h2T.ap[:, kc, tb * 512:(tb + 1) * 512],
                           kc == 0, kc == 7, [ru[k].b()] + hr, [PB(pu)])
                    ACT(s_.ap, ps[pg][:], AF.Silu, [PB(pg)], [s_.b()])
                    TT("dve", hm.ap[:, hc, :], ps[pu][:], s_.ap, ALU.mult, [PB(pu), s_.b()], [hm.b()])
                for ti in range(4):
                    t = tb * 4 + ti
                    for half in range(2):
                        py = 4 + ycnt % 4
                        ycnt += 1
                        for hc in range(4):
                            MM(ps[py][:], hm.ap[:, hc, ti * 128:(ti + 1) * 128], rd[k].ap[:, hc, half * 512:(half + 1) * 512],
                               hc == 0, hc == 3, [hm.b(), rd[k].b()], [PB(py)])
                        ya = yacc.ap[:, t, half * 512:(half + 1) * 512]
                        STT("dve", ya, ps[py][:], Wd.ap[:, t, e:e + 1], ya, ALU.mult, ALU.add,
                            [PB(py), Wd.b(), yacc.b(t)], [yacc.b(t)])
        if stop == 9:
            S.barrier()
            return finish([("yacc", yacc.ap, [])])
        fngb = A.alloc("fngb", [128, D], F32)
        DMA("sp", fngb.ap, fng_d.partition_broadcast(128), (), [fngb.b()], "fngb")
        ot = [A.alloc("ot%d" % i, [128, D], F32) for i in range(2)]
        junk = A.alloc("junk", [128, D], BF16)
        st = [A.alloc("st%d" % i, [128, 2], F32) for i in range(2)]
        for t in range(TL):
            o_, s_ = ot[t % 2], st[t % 2]
            rms_stats(yacc.ap[:, t, :], [yacc.b(t)], junk, s_, D)
            STT("dve", o_.ap, yacc.ap[:, t, :], s_.ap[:, 1:2], fngb.ap, ALU.mult, ALU.mult,
                [yacc.b(t), s_.b(), fngb.b()], [o_.b()])
            out_stores.append(DMA("sp", out_d[b, t * 128:(t + 1) * 128, :], o_.ap, [o_.b()], (), "ot%d" % (t % 2)))
        S.barrier()

    S.op("sp", None, extra_deps=out_stores)
    S.emit()
    return nc


def _pk(w, kc):
    K, N = w.shape
    return np.ascontiguousarray(w.reshape(kc, 128, N).transpose(1, 0, 2))


def _rope_tables():
    f = np.arange(16, dtype=np.float32)
    inv = np.power(np.float32(10000.0), -f / np.float32(16)).astype(np.float32)
    t = np.arange(2048)
    pos = np.stack([(t // 64).astype(np.float32), (t % 64).astype(np.float32)], 0)
    cos = np.zeros((128, 2048), np.float32)
    sin = np.zeros((128, 2048), np.float32)
    perm = np.zeros((128, 128), np.float32)
    for p in range(128):
        d = p % 64
        axis, half, fi = d // 32, (d % 32) // 16, d % 16
        ang = (pos[axis] * inv[fi]).astype(np.float32)
        cos[p] = np.cos(ang)
        sin[p] = np.sin(ang) * (-1.0 if half == 0 else 1.0)
        partner = p + 16 if half == 0 else p - 16
        perm[partner, p] = 1.0
    return cos, sin, perm


def _tri():
    j = np.arange(128)[:, None]
    i = np.arange(128)[None, :]
    tri = np.stack([(j <= i), (j >= i), (j > i), (j < i)], 1).astype(np.float32)
    return np.ascontiguousarray(tri)


_NC_CACHE = {}


def kernel(x, c, ctx, c_ctx, w_ada, b_ada, norm_mix_g, norm_ffn_g, w_in, da_lambda, da_subln_g, gla_gate_w2,
           gla_gate_b, gla_norm_g, w_out, router_group_w, router_group_b, router_expert_w, router_expert_b,
           expert_w_gate, expert_w_up, expert_w_down, final_norm_g):
    f = lambda a: np.asarray(a, dtype=np.float32)
    x, c, ctx, c_ctx = f(x), f(c), f(ctx), f(c_ctx)
    n_cores = 8
    cos, sin, perm = _rope_tables()
    w2 = np.zeros((32, 512), np.float32)
    gw2 = f(gla_gate_w2)[0]
    w2[0:16, 0:256] = gw2[0]
    w2[16:32, 256:512] = gw2[1]
    shared = {
        "w_ada": _pk(f(w_ada)[0], 8),
        "b_ada": np.ascontiguousarray(f(b_ada)[0].reshape(48, 128).T),
        "g1": np.ascontiguousarray(f(norm_mix_g)[0].reshape(8, 128).T),
        "g2": np.ascontiguousarray(f(norm_ffn_g)[0].reshape(8, 128).T),
        "w_in": _pk(f(w_in)[0], 8),
        "da_lambda": f(da_lambda)[0].reshape(1, 256),
        "subln_g": f(da_subln_g)[0].reshape(1, 128),
        "gla_norm_g": f(gla_norm_g)[0].reshape(1, 128),
        "final_g": f(final_norm_g).reshape(1, D),
        "gate_w2": w2,
        "gate_b": f(gla_gate_b)[0].reshape(1, 512),
        "w_out": _pk(f(w_out)[0], 8),
        "w_router": _pk(np.concatenate([f(router_group_w)[0], f(router_expert_w)[0]], 1), 8),
        "b_router": np.concatenate([f(router_group_b)[0], f(router_expert_b)[0]]).reshape(1, 36),
        "ewg": np.ascontiguousarray(f(expert_w_gate)[0].reshape(NE, 8, 128, 512).transpose(0, 2, 1, 3)),
        "ewu": np.ascontiguousarray(f(expert_w_up)[0].reshape(NE, 8, 128, 512).transpose(0, 2, 1, 3)),
        "ewd": np.ascontiguousarray(f(expert_w_down)[0].reshape(NE, 4, 128, D).transpose(0, 2, 1, 3)),
        "rope_cos": cos, "rope_sin": sin, "rope_perm": perm, "tri": _tri(),
    }
    in_maps = []
    for i in range(n_cores):
        bs = [NB * i + k for k in range(NB)]
        xin = np.stack([np.concatenate([ctx[bb], x[bb]], 0) for bb in bs], 0)
        cvec = np.stack([c[bs[0]], c[bs[1]], c_ctx], 1)
        m = dict(shared)
        m["xin"] = np.ascontiguousarray(xin)
        m["cT"] = np.ascontiguousarray(cvec.reshape(8, 128, 3).transpose(1, 0, 2))
        in_maps.append(m)
    if "nc" not in _NC_CACHE:
        _NC_CACHE["nc"] = build_program()
    res = run_bass_kernel_spmd(_NC_CACHE["nc"], in_maps, core_ids=list(range(n_cores)))
    out = np.concatenate([np.asarray(r["out"]) for r in res.results], 0)
    return out.astype(np.float32)
```

```python
from contextlib import ExitStack
import os
import math
import numpy as np
import concourse.bass as bass
import concourse.mybir as mybir
from concourse.bass_utils import run_bass_kernel_spmd

F32 = mybir.dt.float32
BF16 = mybir.dt.bfloat16
AF = mybir.ActivationFunctionType
ALU = mybir.AluOpType
AX = mybir.AxisListType

ENGS = ("pe", "act", "dve", "pool", "sp")
NB = 2
NT = 18
TL = 16
D = 1024
EPS = 1e-6
NE = 32


class Buf:
    __slots__ = ("name", "last_writer", "readers")

    def __init__(self, name):
        self.name = name
        self.last_writer = None
        self.readers = []


class Op:
    __slots__ = ("eng", "fn", "deps", "is_dma", "signal", "semval", "key", "dma_val")

    def __init__(self, eng, fn):
        self.eng = eng
        self.fn = fn
        self.deps = set()
        self.is_dma = False
        self.signal = False
        self.semval = 0
        self.key = None
        self.dma_val = 0


class Sched:
    def __init__(self, nc):
        self.nc = nc
        self.ops = {e: [] for e in ENGS}
        self.dma_keys = {}
        self.bufs = {}
        self.last = {e: None for e in ENGS}

    def buf(self, name):
        b = self.bufs.get(name)
        if b is None:
            b = Buf(name)
            self.bufs[name] = b
        return b

    def op(self, eng, fn, reads=(), writes=(), dma_key=None, extra_deps=()):
        o = Op(eng, fn)
        o.deps.update(extra_deps)
        reads = [self.buf(r) for r in reads]
        writes = [self.buf(w) for w in writes]
        for r in reads:
            if r.last_writer is not None:
                o.deps.add(r.last_writer)
        for w in writes:
            if w.last_writer is not None:
                o.deps.add(w.last_writer)
            o.deps.update(w.readers)
        for r in reads:
            r.readers.append(o)
        for w in writes:
            w.last_writer = o
            w.readers = []
        if dma_key is not None:
            o.is_dma = True
            o.key = dma_key
            st = self.dma_keys.setdefault(dma_key, [0, None])
            if st[1] is not None:
                o.deps.add(st[1])
            st[0] += 16
            st[1] = o
            o.dma_val = st[0]
        o.deps.discard(o)
        if eng == "pe":
            o.deps = {d for d in o.deps if d.is_dma or d.eng != "pe"}
        for d in o.deps:
            d.signal = True
        self.ops[eng].append(o)
        if not o.is_dma and fn is not None:
            self.last[eng] = o
        return o

    def barrier(self):
        deps = [o for o in self.last.values() if o is not None]
        deps += [st[1] for st in self.dma_keys.values() if st[1] is not None]
        for e in ENGS:
            self.op(e, None, extra_deps=deps)

    def emit(self):
        nc = self.nc
        with ExitStack() as es:
            esem = {e: es.enter_context(nc.semaphore("s_" + e)) for e in ENGS}
            dsem = {k: es.enter_context(nc.semaphore("d%d" % i)) for i, k in enumerate(self.dma_keys)}
            for e in ENGS:
                c = 0
                for o in self.ops[e]:
                    if o.signal and not o.is_dma and o.fn is not None:
                        c += 1
                        o.semval = c
            block = es.enter_context(nc.Block())

            self.icount = {}

            def run(ename, eng):
                known = {}
                ic = 0
                for o in self.ops[ename]:
                    self.icount[ename] = ic
                    need = {}
                    for d in o.deps:
                        if d.is_dma:
                            s, v = dsem[d.key], d.dma_val
                        else:
                            s, v = esem[d.eng], d.semval
                        if v > need.get(s, (0, None))[0]:
                            need[s] = (v, s)
                    for v, s in need.values():
                        if known.get(s, 0) < v:
                            eng.wait_ge(s, v)
                            known[s] = v
                            ic += 1
                    if o.fn is None:
                        continue
                    ins = o.fn(eng)
                    ic += 1
                    if o.is_dma:
                        ins.then_inc(dsem[o.key], 16)
                    elif o.signal:
                        ins.then_inc(esem[ename], 1)

            block.tensor(lambda eng: run("pe", eng))
            block.scalar(lambda eng: run("act", eng))
            block.vector(lambda eng: run("dve", eng))
            block.gpsimd(lambda eng: run("pool", eng))
            block.sync(lambda eng: run("sp", eng))


class TV:
    def __init__(self, ap, name):
        self.ap = ap
        self.name = name

    def b(self, i=None):
        return self.name if i is None else "%s#%s" % (self.name, i)


class Arena:
    def __init__(self, nc, nbytes):
        self.cap = nbytes
        self.t = nc.alloc_sbuf_tensor("arena", [128, nbytes // 4], F32)
        self.top = 0
        self.cnt = 0
        self.peak = 0

    def alloc(self, name, shape, dtype):
        esz = 4 if dtype == F32 else 2
        free = 1
        for s in shape[1:]:
            free *= s
        nbytes = (free * esz + 63) // 64 * 64
        off = self.top
        self.top += nbytes
        self.peak = max(self.peak, self.top)
        assert self.top <= self.cap, ("SBUF arena overflow", name, self.top)
        ap = self.t[0:shape[0], off // 4:(off + nbytes) // 4]
        if esz == 2:
            ap = ap.bitcast(BF16)
        ap = ap[:, 0:free]
        if len(shape) == 3:
            ap = ap.rearrange("p (a b) -> p a b", a=shape[1], b=shape[2])
        elif len(shape) == 4:
            ap = ap.rearrange("p (a b c) -> p a b c", a=shape[1], b=shape[2], c=shape[3])
        self.cnt += 1
        return TV(ap, "%s@%d" % (name, self.cnt))


def build_program(stop=None):
    nc = bass.Bass("TRN2", target_bir_lowering=False)
    S = Sched(nc)

    def din(name, shape):
        return nc.dram_tensor(name, list(shape), F32, kind="ExternalInput").ap()

    xin = din("xin", [NB, NT * 128, D])
    cT_d = din("cT", [128, 8, 3])
    wada_d = din("w_ada", [128, 8, 6 * D])
    bada_d = din("b_ada", [128, 48])
    g1_d = din("g1", [128, 8])
    g2_d = din("g2", [128, 8])
    win_d = din("w_in", [128, 8, 3104])
    lam_d = din("da_lambda", [1, 256])
    subln_d = din("subln_g", [1, 128])
    glang_d = din("gla_norm_g", [1, 128])
    fng_d = din("final_g", [1, D])
    w2_d = din("gate_w2", [32, 512])
    gb_d = din("gate_b", [1, 512])
    wout_d = din("w_out", [128, 8, D])
    wr_d = din("w_router", [128, 8, 36])
    br_d = din("b_router", [1, 36])
    ewg_d = din("ewg", [NE, 128, 8, 512])
    ewu_d = din("ewu", [NE, 128, 8, 512])
    ewd_d = din("ewd", [NE, 128, 4, D])
    cos_d = din("rope_cos", [128, 2048])
    sin_d = din("rope_sin", [128, 2048])
    perm_d = din("rope_perm", [128, 128])
    tri_d = din("tri", [128, 4, 128])
    out_d = nc.dram_tensor("out", [NB, TL * 128, D], F32, kind="ExternalOutput").ap()
    mods_d = nc.dram_tensor("mods_scr", [NB, 48, 128], F32, kind="Internal").ap()
    x1s_d = nc.dram_tensor("x1_scr", [NB, TL * 128, D], F32, kind="Internal").ap()

    A = Arena(nc, 200 * 1024)
    ps = [nc.alloc_psum_tensor("ps%d" % i, [128, 512], F32) for i in range(8)]

    def PB(i):
        return "psb%d" % i

    def MM(out, lhsT, rhs, start, stop, r, w, sgc=False):
        S.op("pe", lambda e: e.matmul(out, lhsT=lhsT, rhs=rhs, start=start, stop=stop, skip_group_check=sgc), r, w)

    def TR(out, in_, ident, r, w):
        S.op("pe", lambda e: e.transpose(out, in_, ident), r, w)

    def ACT(out, in_, func, r, w, bias=None, scale=None, accum=None):
        kw = {}
        if bias is not None:
            kw["bias"] = bias
        if scale is not None:
            kw["scale"] = scale
        if accum is not None:
            kw["accum_out"] = accum
        S.op("act", lambda e: e.activation(out=out, in_=in_, func=func, **kw), r, w)

    def TT(eng, out, in0, in1, op, r, w):
        S.op(eng, lambda e: e.tensor_tensor(out=out, in0=in0, in1=in1, op=op), r, w)

    def TS(eng, out, in0, s1, s2, op0, op1, r, w):
        if op1 is None:
            S.op(eng, lambda e: e.tensor_scalar(out=out, in0=in0, scalar1=s1, scalar2=None, op0=op0), r, w)
        else:
            S.op(eng, lambda e: e.tensor_scalar(out=out, in0=in0, scalar1=s1, scalar2=s2, op0=op0, op1=op1), r, w)

    def STT(eng, out, in0, sc, in1, op0, op1, r, w):
        S.op(eng, lambda e: e.scalar_tensor_tensor(out=out, in0=in0, scalar=sc, in1=in1, op0=op0, op1=op1), r, w)

    def CP(eng, out, in_, r, w):
        if eng == "act":
            S.op("act", lambda e: e.activation(out=out, in_=in_, func=AF.Identity), r, w)
        else:
            S.op(eng, lambda e: e.tensor_copy(out=out, in_=in_), r, w)

    def RECIP(out, in_, r, w):
        S.op("dve", lambda e: e.reciprocal(out=out, in_=in_), r, w)

    def RMAX(out, in_, r, w):
        S.op("dve", lambda e: e.reduce_max(out=out, in_=in_, axis=AX.X), r, w)

    def RSUM(out, in_, r, w):
        S.op("dve", lambda e: e.reduce_sum(out=out, in_=in_, axis=AX.X), r, w)

    def MSET(eng, ap, val, w):
        S.op(eng, lambda e: e.memset(ap, val), (), w)

    def DMA(q, out, in_, r, w, key):
        return S.op(q, lambda e: e.dma_start(out=out, in_=in_), r, w, dma_key=key)

    def finish(dumps):
        outs = []
        for i, (name, ap, bufs) in enumerate(dumps):
            d = nc.dram_tensor("dbg_" + name, list(ap.shape), ap.dtype, kind="ExternalOutput").ap()
            outs.append(DMA("sp", d, ap, bufs, (), "dbg%d" % i))
        S.op("sp", None, extra_deps=outs)
        S.emit()
        print("ICOUNT", S.icount, {e: max([o.semval for o in S.ops[e]] + [0]) for e in ENGS}, {k: v[0] for k, v in S.dma_keys.items()})
        return nc

    identb = A.alloc("identb", [128, 128], BF16)
    identf = A.alloc("identf", [128, 128], F32)
    permb = A.alloc("permb", [128, 128], BF16)
    tri = A.alloc("tri", [128, 4, 128], F32)
    epsb = A.alloc("epsb", [128, 1], F32)
    onesf = A.alloc("onesf", [128, 1], F32)
    onesrow = A.alloc("onesrow", [1, 128], BF16)
    g1 = A.alloc("g1", [128, 8], F32)
    g2 = A.alloc("g2", [128, 8], F32)
    bada = A.alloc("bada", [128, 48], F32)
    cT = A.alloc("cT", [128, 8, 3], F32)
    scT = A.alloc("scT", [128, 8, 3], BF16)
    modT = A.alloc("modT", [128, 48, 3], F32)
    A1 = A.alloc("A1", [128, 3, 8], F32)
    A2 = A.alloc("A2", [128, 3, 8], F32)
    wr32 = A.alloc("wr32", [128, 8, 36], F32)
    brb = A.alloc("brb", [128, 36], F32)
    w2blk = A.alloc("w2blk", [32, 512], BF16)
    gbrow = A.alloc("gbrow", [1, 512], BF16)
    sublnb = A.alloc("sublnb", [128, 128], F32)
    glangb = A.alloc("glangb", [128, 128], F32)
    lamb = A.alloc("lamb", [128, 256], F32)
    lamt = A.alloc("lamt", [128, 8], F32)
    Wd = A.alloc("Wd", [128, TL, NE], F32)
    hT = A.alloc("hT", [128, 8, NT * 128], BF16)

    for tv, val in ((identb, 1.0), (identf, 1.0)):
        MSET("pool", tv.ap, val, [tv.b()])
        S.op("pool", (lambda ap: lambda e: e.affine_select(out=ap, in_=ap, pattern=[[-1, 128]],
                                                             compare_op=ALU.is_equal, fill=0.0, base=0,
                                                             channel_multiplier=1))(tv.ap),
             [tv.b()], [tv.b()])
    MSET("pool", epsb.ap, EPS, [epsb.b()])
    MSET("pool", onesf.ap, 1.0, [onesf.b()])
    MSET("pool", onesrow.ap, 1.0, [onesrow.b()])
    DMA("pool", permb.ap, perm_d, (), [permb.b()], "permb")
    DMA("sp", tri.ap, tri_d, (), [tri.b()], "tri")
    DMA("sp", g1.ap, g1_d, (), [g1.b()], "g1")
    DMA("sp", g2.ap, g2_d, (), [g2.b()], "g2")
    DMA("sp", bada.ap, bada_d, (), [bada.b()], "bada")
    DMA("sp", cT.ap, cT_d, (), [cT.b()], "cT")
    DMA("sp", wr32.ap, wr_d, (), [wr32.b()], "wr32")
    DMA("sp", brb.ap, br_d.partition_broadcast(128), (), [brb.b()], "brb")
    DMA("pool", w2blk.ap, w2_d, (), [w2blk.b()], "w2blk")
    DMA("pool", gbrow.ap, gb_d, (), [gbrow.b()], "gbrow")
    DMA("sp", sublnb.ap, subln_d.partition_broadcast(128), (), [sublnb.b()], "sublnb")
    DMA("sp", glangb.ap, glang_d.partition_broadcast(128), (), [glangb.b()], "glangb")
    DMA("sp", lamb.ap, lam_d.partition_broadcast(128), (), [lamb.b()], "lamb")
    lambda_init = 0.8 - 0.6 * math.exp(0.0)
    TS("dve", sublnb.ap, sublnb.ap, 1.0 - lambda_init, None, ALU.mult, None, [sublnb.b()], [sublnb.b()])
    lt = lamt.ap
    lb = lamb.ap
    TT("dve", lb[:, 0:64], lb[:, 0:64], lb[:, 64:128], ALU.mult, [lamb.b()], [lamb.b()])
    TT("dve", lb[:, 128:192], lb[:, 128:192], lb[:, 192:256], ALU.mult, [lamb.b()], [lamb.b()])
    RSUM(lt[:, 0:1], lb[:, 0:64], [lamb.b()], [lamt.b()])
    RSUM(lt[:, 1:2], lb[:, 128:192], [lamb.b()], [lamt.b()])
    ACT(lt[:, 2:4], lt[:, 0:2], AF.Exp, [lamt.b()], [lamt.b()])
    TT("dve", lt[:, 4:5], lt[:, 3:4], lt[:, 2:3], ALU.subtract, [lamt.b()], [lamt.b()])
    TS("dve", lt[:, 7:8], lt[:, 4:5], -lambda_init, None, ALU.add, None, [lamt.b()], [lamt.b()])
    nlam = lt[:, 7:8]

    ACT(scT.ap, cT.ap, AF.Silu, [cT.b()], [scT.b()])
    m0 = A.top
    wb = [A.alloc("wbuf%d" % i, [128, 8, 512], BF16) for i in range(2)]
    psA = ps[0][:, 0:144]
    for grp in range(12):
        w = wb[grp % 2]
        DMA("pool", w.ap, wada_d[:, :, grp * 512:(grp + 1) * 512], (), [w.b()], "wbuf%d" % (grp % 2))
        for c in range(4):
            fo = grp * 4 + c
            for kc in range(8):
                MM(psA[:, fo * 3:fo * 3 + 3], w.ap[:, kc, c * 128:(c + 1) * 128], scT.ap[:, kc, :],
                   kc == 0, kc == 7, [w.b(), scT.b()], [PB(0)])
    psA3 = psA.rearrange("p (a b) -> p a b", a=48, b=3)
    for j in range(3):
        TT("dve", modT.ap[:, :, j], psA3[:, :, j], bada.ap, ALU.add, [PB(0), bada.b()], [modT.b()])
        STT("dve", A1.ap[:, j, :], modT.ap[:, 8:16, j], 1.0, g1.ap, ALU.add, ALU.mult, [modT.b(), g1.b()], [A1.b()])
        STT("dve", A2.ap[:, j, :], modT.ap[:, 32:40, j], 1.0, g2.ap, ALU.add, ALU.mult, [modT.b(), g2.b()], [A2.b()])
    mtmp = A.alloc("mtmp", [128, 48], F32)
    mrow = A.alloc("mrow", [48, 128], F32)
    for j in range(NB):
        CP("dve", mtmp.ap, modT.ap[:, :, j], [modT.b()], [mtmp.b()])
        TR(ps[1][0:48, 0:128], mtmp.ap, identf.ap, [mtmp.b(), identf.b()], [PB(1)])
        CP("dve", mrow.ap, ps[1][0:48, 0:128], [PB(1)], [mrow.b()])
        DMA("sp", mods_d[j], mrow.ap, [mrow.b()], ["mods"], "mods_w")
    S.barrier()
    if stop == 0:
        g1t = A.alloc("g1t", [128, D], F32)
        DMA("sp", g1t.ap, mods_d[0, 16:24, :].rearrange("(o a) b -> o (a b)", o=1).partition_broadcast(128), ["mods"], [g1t.b()], "gt1b")
        return finish([("modT", modT.ap, [modT.b()]), ("A1", A1.ap, [A1.b()]), ("A2", A2.ap, [A2.b()]),
                       ("lamt", lamt.ap, [lamt.b()]), ("gt1b", g1t.ap, [g1t.b()]), ("subln", sublnb.ap, [sublnb.b()]),
                       ("identb", identb.ap, [identb.b()])])
    A.top = m0
    base_top = A.top
    if os.environ.get("PAD"):
        pd_ = A.alloc("pad", [128, 8], F32)
        base_top = A.top
        for i in range(int(os.environ["PAD"])):
            MSET("dve", pd_.ap, float(i), [pd_.b()])

    def rms_stats(src_ap, src_b, junk, st, n):
        MSET("pool", st.ap[:, 0:1], 0.0, [st.b()])
        ACT(junk.ap, src_ap, AF.Square, src_b, [junk.b(), st.b()], accum=st.ap[:, 0:1])
        ACT(st.ap[:, 1:2], st.ap[:, 0:1], AF.Sqrt, [st.b(), epsb.b()], [st.b()], bias=epsb.ap, scale=1.0 / n)
        RECIP(st.ap[:, 1:2], st.ap[:, 1:2], [st.b()], [st.b()])

    out_stores = []
    TOKB = [(0, 512), (512, 512), (1024, 512), (1536, 512), (2048, 256)]
    for b in range(NB):
        A.top = base_top
        mixer_mark = A.top
        mix = A.alloc("mix", [128, TL, D], BF16)
        att_mark = A.top
        cosb = A.alloc("cosb", [128, 2048], BF16)
        sinb = A.alloc("sinb", [128, 2048], BF16)
        QT = A.alloc("QT", [128, 4, 2048], BF16)
        KT = A.alloc("KT", [128, 4, NT * 128], BF16)
        V = A.alloc("V", [128, NT, 4, 130], BF16)
        DMA("pool", cosb.ap, cos_d, (), [cosb.b()], "cosb")
        DMA("pool", sinb.ap, sin_d, (), [sinb.b()], "sinb")
        MSET("pool", V.ap[:, :, :, 128:130], 1.0, [V.b("ones")])
        pa_mark = A.top
        xt = [A.alloc("xt%d" % i, [128, D], F32) for i in range(2)]
        xs = [A.alloc("xs%d" % i, [128, D], BF16) for i in range(2)]
        junk = A.alloc("junk", [128, D], BF16)
        st = [A.alloc("st%d" % i, [128, 2], F32) for i in range(2)]
        for t in range(NT):
            j = 2 if t < 2 else b
            x_, s_, t_ = xt[t % 2], xs[t % 2], st[t % 2]
            DMA("sp", x_.ap, xin[b, t * 128:(t + 1) * 128, :], (), [x_.b()], "xt%d" % (t % 2))
            rms_stats(x_.ap, [x_.b()], junk, t_, D)
            S.op("dve", (lambda o, i, s: lambda e: e.tensor_scalar_mul(out=o, in0=i, scalar1=s))(s_.ap, x_.ap, t_.ap[:, 1:2]),
                 [x_.b(), t_.b()], [s_.b()])
            pb = 2 + (t % 2)
            pT = ps[pb][:].bitcast(BF16)
            for kc in range(8):
                TR(pT[:, kc * 128:(kc + 1) * 128], s_.ap[:, kc * 128:(kc + 1) * 128], identb.ap,
                   [s_.b(), identb.b()], [PB(pb)])
            for kc in range(8):
                o_ = hT.ap[:, kc, t * 128:(t + 1) * 128]
                i_ = pT[:, kc * 128:(kc + 1) * 128]
                if kc % 2 == 0:
                    ACT(o_, i_, AF.Identity, [PB(pb), A1.b(), modT.b()], [hT.b(t)],
                        bias=modT.ap[:, kc, j:j + 1], scale=A1.ap[:, j, kc:kc + 1])
                else:
                    TS("dve", o_, i_, A1.ap[:, j, kc:kc + 1], modT.ap[:, kc, j:j + 1], ALU.mult, ALU.add,
                       [PB(pb), A1.b(), modT.b()], [hT.b(t)])
        hT_all = [hT.b(t) for t in range(NT)]
        if stop == 1:
            S.barrier()
            return finish([("hT", hT.ap, hT_all)])

        wb = [A.alloc("wbuf%d" % i, [128, 8, 512], BF16) for i in range(2)]
        qraw = [A.alloc("qraw%d" % i, [128, 512], BF16) for i in range(2)]
        rt1 = [A.alloc("rt1_%d" % i, [128, 512], F32) for i in range(2)]
        rt2 = [A.alloc("rt2_%d" % i, [128, 512], F32) for i in range(2)]
        cnt = 0
        for gi, (dst, col0) in enumerate(((QT, 0), (KT, 512))):
            w = wb[gi % 2]
            DMA("pool", w.ap, win_d[:, :, col0:col0 + 512], (), [w.b()], "wbuf%d" % (gi % 2))
            for h in range(4):
                if dst is KT:
                    pb = cnt % 2
                    for kc in range(8):
                        MM(ps[pb][:, 0:256], w.ap[:, kc, h * 128:(h + 1) * 128], hT.ap[:, kc, 0:256], kc == 0, kc == 7,
                           [w.b(), hT.b(0), hT.b(1)], [PB(pb)])
                    CP("act", KT.ap[:, h, 0:256], ps[pb][:, 0:256], [PB(pb)], [KT.b("%d_c" % h)])
                    cnt += 1
                for tb_i in range(4):
                    tb = 0 if os.environ.get('TB0') else tb_i
                    pz_ = 0 if os.environ.get("PAR0") else cnt % 2
                    pb = pz_ if not os.environ.get("PARB") else cnt % 2
                    pw = 2 + pz_
                    qr, r1, r2 = qraw[pz_], rt1[pz_], rt2[pz_]
                    tok = 256 + tb * 512
                    hr = [hT.b(2 + tb * 4 + i) for i in range(4)]
                    for kc in range(8):
                        MM(ps[pb][:], w.ap[:, kc, h * 128:(h + 1) * 128], hT.ap[:, kc, tok:tok + 512], kc == 0, kc == 7,
                           [w.b()] + hr, [PB(pb)])
                    CP("act", qr.ap, ps[pb][:], [PB(pb)], [qr.b()])
                    MM(ps[pw][:], permb.ap, qr.ap, True, True, [permb.b(), qr.b()], [PB(pw)])
                    TT("dve", r1.ap, ps[pb][:], cosb.ap[:, tb * 512:(tb + 1) * 512], ALU.mult, [PB(pb), cosb.b(), qr.b()], [r1.b()])
                    TT("dve", r2.ap, ps[pw][:], sinb.ap[:, tb * 512:(tb + 1) * 512], ALU.mult, [PB(pw), sinb.b()], [r2.b()])
                    if dst is QT:
                        o_ = QT.ap[:, h, tb * 512:(tb + 1) * 512]
                        ob = QT.b("%d_%d" % (h, tb))
                    else:
                        o_ = KT.ap[:, h, tok:tok + 512]
                        ob = KT.b("%d_%d" % (h, tb))
                    TT("dve" if os.environ.get("POOLDVE") else "pool", o_, r1.ap, r2.ap, ALU.add, [r1.b(), r2.b()], [ob])
                    if cnt + 1 == int(os.environ.get("CUTN", "0")):
                        S.barrier()
                        return finish([("QT", QT.ap, []), ("KT", KT.ap, [])])
                    cnt += 1
        w = wb[0]
        DMA("pool", w.ap, win_d[:, :, 1024:1536], (), [w.b()], "wbuf0")
        for t in range(NT):
            pb = 4 + t % 2
            for kc in range(8):
                MM(ps[pb][:], hT.ap[:, kc, t * 128:(t + 1) * 128], w.ap[:, kc, :], kc == 0, kc == 7,
                   [w.b(), hT.b(t)], [PB(pb)])
            CP("act" if t % 2 == 0 else "dve", V.ap[:, t, :, 0:128],
               ps[pb][:].rearrange("p (a b) -> p a b", a=4, b=128), [PB(pb)], [V.b(t)])
        S.barrier()
        if stop == 2:
            return finish([("QT", QT.ap, []), ("KT", KT.ap, []), ("V", V.ap, [])])
        A.top = pa_mark

        ET = [A.alloc("ET%d" % i, [128, 512], BF16) for i in range(3)]
        fin = [A.alloc("fin%d" % i, [128, 8], F32) for i in range(2)]
        ta = [A.alloc("ta%d" % i, [128, 128], F32) for i in range(2)]
        tb_ = [A.alloc("tb%d" % i, [128, 128], F32) for i in range(2)]
        junk = A.alloc("junk", [128, 128], BF16)
        Vall = [V.b(t) for t in range(NT)] + [V.b("ones")]
        fcnt = 0
        steps = [(h, qb, m, kt) for h in range(4) for qb in range(4) for m in range(2) for kt in range(NT)]

        def acc_of(h, qb, m, qs):
            par = (h * 4 + qb) % 2
            r = m * 4 + qs
            return ps[2 + par * 3 + r // 3][:, (r % 3) * 129:(r % 3) * 129 + 129], "acc%d_%d" % (par, r)

        def emit_S(i):
            h, qb, m, kt = steps[i]
            Kall = [KT.b("%d_c" % h)] + [KT.b("%d_%d" % (h, j)) for j in range(4)]
            MM(ps[i % 2][:], KT.ap[m * 64:(m + 1) * 64, h, kt * 128:(kt + 1) * 128],
               QT.ap[m * 64:(m + 1) * 64, h, qb * 512:(qb + 1) * 512], True, True,
               Kall + [QT.b("%d_%d" % (h, qb))], [PB(i % 2)])

        started = set()
        emit_S(0)
        emit_S(1)
        for i, (h, qb, m, kt) in enumerate(steps):
            if m == 0 and kt == 0:
                started = set()
            e_ = ET[i % 3]
            ACT(e_.ap, ps[i % 2][:], AF.Exp, [PB(i % 2)], [e_.b()], scale=0.125)
            if i + 2 < len(steps):
                emit_S(i + 2)
            for qs in range(4):
                ap_, bn = acc_of(h, qb, m, qs)
                bank = (m * 4 + qs) // 3
                st_ = kt == 0 and bank not in started
                started.add(bank)
                MM(ap_, e_.ap[:, qs * 128:(qs + 1) * 128], V.ap[:, kt, h, 0:129], st_, kt == NT - 1,
                   [e_.b()] + Vall, [bn], sgc=True)
            if m == 1 and kt == NT - 1:
                for qs in range(4):
                    a1, b1 = acc_of(h, qb, 0, qs)
                    a2, b2 = acc_of(h, qb, 1, qs)
                    f, x_, y_ = fin[fcnt % 2], ta[fcnt % 2], tb_[fcnt % 2]
                    fcnt += 1
                    tq = qb * 4 + qs
                    RECIP(f.ap[:, 0:1], a1[:, 128:129], [b1], [f.b()])
                    RECIP(f.ap[:, 1:2], a2[:, 128:129], [b2], [f.b()])
                    TT("dve", f.ap[:, 2:3], f.ap[:, 1:2], nlam, ALU.mult, [f.b(), lamt.b()], [f.b()])
                    TS("dve", x_.ap, a1[:, 0:128], f.ap[:, 0:1], None, ALU.mult, None, [b1, f.b()], [x_.b()])
                    STT("dve", y_.ap, a2[:, 0:128], f.ap[:, 2:3], x_.ap, ALU.mult, ALU.add, [b2, f.b(), x_.b()], [y_.b()])
                    MSET("pool", f.ap[:, 3:4], 0.0, [f.b()])
                    ACT(junk.ap, y_.ap, AF.Square, [y_.b()], [junk.b(), f.b()], accum=f.ap[:, 3:4])
                    ACT(f.ap[:, 4:5], f.ap[:, 3:4], AF.Sqrt, [f.b(), epsb.b()], [f.b()], bias=epsb.ap, scale=1.0 / 128)
                    RECIP(f.ap[:, 4:5], f.ap[:, 4:5], [f.b()], [f.b()])
                    STT("dve", mix.ap[:, tq, h * 128:(h + 1) * 128], y_.ap, f.ap[:, 4:5], sublnb.ap, ALU.mult, ALU.mult,
                        [y_.b(), f.b(), sublnb.b()], [mix.b(tq)])
        S.barrier()
        if stop == 3:
            return finish([("mix", mix.ap, [])])
        A.top = att_mark

        gqk = A.alloc("gqk", [128, 4, NT * 128], BF16)
        gkt = A.alloc("gkt", [128, NT, 256], BF16)
        gv = A.alloc("gv", [128, NT, 512], BF16)
        lrT = A.alloc("lrT", [32, NT * 128], BF16)
        Sb = A.alloc("Sb", [128, 4, TL, 128], BF16)
        gla_mark = A.top
        wb = [A.alloc("wbuf%d" % i, [128, 8, 512], BF16) for i in range(2)]
        w = wb[0]
        DMA("pool", w.ap, win_d[:, :, 1536:2048], (), [w.b()], "wbuf0")
        cnt = 0
        for c in range(4):
            for (tok, n) in TOKB:
                pb = cnt % 2
                hr = [hT.b(tok // 128 + i) for i in range(n // 128)]
                for kc in range(8):
                    MM(ps[pb][:, 0:n], w.ap[:, kc, c * 128:(c + 1) * 128], hT.ap[:, kc, tok:tok + n], kc == 0, kc == 7,
                       [w.b()] + hr, [PB(pb)])
                CP("act" if cnt % 2 == 0 else "dve", gqk.ap[:, c, tok:tok + n], ps[pb][:, 0:n], [PB(pb)], [gqk.b()])
                cnt += 1
        for t in range(NT):
            pb = 2 + t % 2
            for kc in range(8):
                MM(ps[pb][:, 0:256], hT.ap[:, kc, t * 128:(t + 1) * 128], w.ap[:, kc, 256:512], kc == 0, kc == 7,
                   [w.b(), hT.b(t)], [PB(pb)])
            CP("act" if t % 2 == 0 else "dve", gkt.ap[:, t, :], ps[pb][:, 0:256], [PB(pb)], [gkt.b()])
        w = wb[1]
        DMA("pool", w.ap, win_d[:, :, 2048:2560], (), [w.b()], "wbuf1")
        for t in range(NT):
            pb = 4 + t % 2
            for kc in range(8):
                MM(ps[pb][:], hT.ap[:, kc, t * 128:(t + 1) * 128], w.ap[:, kc, :], kc == 0, kc == 7,
                   [w.b(), hT.b(t)], [PB(pb)])
            CP("act" if t % 2 == 0 else "dve", gv.ap[:, t, :], ps[pb][:], [PB(pb)], [gv.b()])
        w = wb[0]
        DMA("pool", w.ap[:, :, 0:32], win_d[:, :, 3072:3104], (), [w.b()], "wbuf0")
        for (tok, n) in TOKB:
            pb = 6 + cnt % 2
            cnt += 1
            hr = [hT.b(tok // 128 + i) for i in range(n // 128)]
            for kc in range(8):
                MM(ps[pb][0:32, 0:n], w.ap[:, kc, 0:32], hT.ap[:, kc, tok:tok + n], kc == 0, kc == 7, [w.b()] + hr, [PB(pb)])
            CP("act", lrT.ap[:, tok:tok + n], ps[pb][0:32, 0:n], [PB(pb)], [lrT.b()])
        S.barrier()
        if stop == 4:
            return finish([("gqk", gqk.ap, []), ("gkt", gkt.ap, []), ("gv", gv.ap, []), ("lrT", lrT.ap, [])])
        A.top = gla_mark

        St = A.alloc("St", [128, 4, 128], F32)
        MSET("dve", St.ap, 0.0, [St.b(i) for i in range(4)])
        G_ = [A.alloc("G%d" % i, [128, 256], F32) for i in range(2)]
        ER = [A.alloc("ER%d" % i, [128, 256], F32) for i in range(2)]
        k2e = [A.alloc("k2e%d" % i, [128, 256], BF16) for i in range(2)]
        dec = [A.alloc("dec%d" % i, [128, 2], F32) for i in range(2)]
        orders = (list(range(NT)), [1, 0] + list(range(NT - 1, 1, -1)))
        cnt = 0
        for s in range(NT):
            for d in range(2):
                t = orders[d][s]
                g_, er, ke, dc = G_[cnt % 2], ER[cnt % 2], k2e[cnt % 2], dec[cnt % 2]
                pz = cnt % 2
                pu = 2 + cnt % 2
                pd = 4 + cnt % 2
                cnt += 1
                MM(ps[pz][:, 0:256], lrT.ap[:, t * 128:(t + 1) * 128], w2blk.ap[:, d * 256:(d + 1) * 256], True, False,
                   [lrT.b(), w2blk.b()], [PB(pz)])
                MM(ps[pz][:, 0:256], onesrow.ap, gbrow.ap[:, d * 256:(d + 1) * 256], False, True,
                   [onesrow.b(), gbrow.b()], [PB(pz)])
                ACT(g_.ap, ps[pz][:, 0:256], AF.Exp, [PB(pz)], [g_.b()], scale=-1.0)
                ACT(g_.ap, g_.ap, AF.Ln, [g_.b()], [g_.b()], bias=1.0)
                MM(ps[pz][:, 256:512], tri.ap[:, 2 + d, :], g_.ap, True, True, [tri.b(), g_.b()], [PB(pz)])
                ACT(er.ap, ps[pz][:, 256:512], AF.Exp, [PB(pz)], [er.b()], scale=-1.0 / 16)
                TT("dve", ke.ap, gkt.ap[:, t, :], er.ap, ALU.mult, [gkt.b(), er.b()], [ke.b()])
                for p in range(2):
                    MM(ps[pd][:, p:p + 1], g_.ap[:, p * 128:(p + 1) * 128], onesf.ap, True, True, [g_.b(), onesf.b()], [PB(pd)])
                    MM(ps[pu][:, p * 256:(p + 1) * 256], ke.ap[:, p * 128:(p + 1) * 128], gv.ap[:, t, p * 256:(p + 1) * 256],
                       True, True, [ke.b(), gv.b()], [PB(pu)])
                ACT(dc.ap, ps[pd][:, 0:2], AF.Exp, [PB(pd)], [dc.b()], scale=-1.0 / 16)
                for p in range(2):
                    i = d * 2 + p
                    if t >= 2:
                        CP("pool", Sb.ap[:, i, t - 2, :], St.ap[:, i, :], [St.b(i)], [Sb.b()])
                    for hh in range(2):
                        r0, r1_ = hh * 64, hh * 64 + 64
                        STT("dve", St.ap[r0:r1_, i, :], St.ap[r0:r1_, i, :], dc.ap[r0:r1_, p:p + 1],
                            ps[pu][r0:r1_, p * 256 + hh * 128:p * 256 + hh * 128 + 128], ALU.mult, ALU.add,
                            [St.b(i), dc.b(), PB(pu)], [St.b(i)])
        S.barrier()
        if stop == 5:
            return finish([("Sb", Sb.ap, []), ("St", St.ap, [])])
        A.top = gla_mark

        G2 = [A.alloc("G2_%d" % i, [128, 512], F32) for i in range(2)]
        eb = [A.alloc("eb%d" % i, [128, 512], F32) for i in range(2)]
        ei = [A.alloc("ei%d" % i, [128, 512], F32) for i in range(2)]
        qd = [A.alloc("qd%d" % i, [128, 2, 2, 128], BF16) for i in range(2)]
        ki = [A.alloc("ki%d" % i, [128, 2, 2, 128], BF16) for i in range(2)]
        qz = [A.alloc("qz%d" % i, [128, 2, 4, 128], BF16) for i in range(2)]
        for i in range(2):
            MSET("pool", qz[i].ap, 0.0, [qz[i].b()])
        sTm = [A.alloc("sTm%d" % i, [128, 2, 4, 128], BF16) for i in range(2)]
        osq = [A.alloc("osq%d" % i, [128, 4, 128], F32) for i in range(2)]
        fs = [A.alloc("fs%d" % i, [128, 8], F32) for i in range(2)]
        for t in range(2, NT):
            k = t % 2
            g_, eb_, ei_, qd_, ki_, sm, oq, f = G2[k], eb[k], ei[k], qz[k], ki[k], sTm[k], osq[k], fs[k]
            pz, pbk, ps0, ps1, po = 0 + k, 2 + k, 4, 5, 6 + k
            MM(ps[pz][:], lrT.ap[:, t * 128:(t + 1) * 128], w2blk.ap, True, False, [lrT.b(), w2blk.b()], [PB(pz)])
            MM(ps[pz][:], onesrow.ap, gbrow.ap, False, True, [onesrow.b(), gbrow.b()], [PB(pz)])
            ACT(g_.ap, ps[pz][:], AF.Exp, [PB(pz)], [g_.b()], scale=-1.0)
            ACT(g_.ap, g_.ap, AF.Ln, [g_.b()], [g_.b()], bias=1.0)
            for d in range(2):
                for p in range(2):
                    c0 = (d * 2 + p) * 128
                    MM(ps[pbk][:, c0:c0 + 128], g_.ap[:, d * 256 + p * 128:d * 256 + p * 128 + 128], tri.ap[:, d, :], True, True,
                       [g_.b(), tri.b()], [PB(pbk)])
            ACT(eb_.ap, ps[pbk][:], AF.Exp, [PB(pbk)], [eb_.b()], scale=-1.0 / 16)
            ACT(ei_.ap, ps[pbk][:], AF.Exp, [PB(pbk)], [ei_.b()], scale=1.0 / 16)
            if os.environ.get("P2CUT") == "2":
                S.barrier()
                return finish([("f", f.ap, [])])

            for d in range(2):
                for p in range(2):
                    c0 = (d * 2 + p) * 128
                    for hh in range(2):
                        q0 = hh * 64
                        STT("dve", qd_.ap[q0:q0 + 64, d, 2 * p + hh, :], gqk.ap[q0:q0 + 64, p, t * 128:(t + 1) * 128], 0.125,
                            eb_.ap[q0:q0 + 64, c0:c0 + 128], ALU.mult, ALU.mult, [gqk.b(), eb_.b()], [qd_.b()])
                    TT("dve", ki_.ap[:, d, p, :], gqk.ap[:, 2 + p, t * 128:(t + 1) * 128], ei_.ap[:, c0:c0 + 128], ALU.mult,
                       [gqk.b(), ei_.b()], [ki_.b()])
            if os.environ.get("P2CUT") == "3":
                S.barrier()
                return finish([("f", f.ap, [])])

            for d in range(2):
                psd = ps0 if d == 0 else ps1
                for h in range(4):
                    r0 = (h % 2) * 64
                    MM(ps[psd][:, h * 128:(h + 1) * 128], ki_.ap[:, d, h // 2, :], qd_.ap[:, d, h, :],
                       True, True, [ki_.b(), qd_.b()], [PB(psd)])
                if os.environ.get("P2CUT") == "41":
                    S.barrier()
                    return finish([("f", f.ap, [])])
                for h in range(4):
                    TT("dve", sm.ap[:, d, h, :], ps[psd][:, h * 128:(h + 1) * 128], tri.ap[:, d, :], ALU.mult,
                       [PB(psd), tri.b()], [sm.b()])
                if os.environ.get("P2CUT") == "42":
                    S.barrier()
                    return finish([("f", f.ap, [])])
            if os.environ.get("P2CUT") == "4":
                S.barrier()
                return finish([("f", f.ap, [])])

            for h in range(4):
                r0 = (h % 2) * 64
                o_ = ps[po][:, h * 128:(h + 1) * 128]
                MM(o_, sm.ap[:, 0, h, :], gv.ap[:, t, h * 128:(h + 1) * 128], True, False, [sm.b(), gv.b()], [PB(po)])
                MM(o_, sm.ap[:, 1, h, :], gv.ap[:, t, h * 128:(h + 1) * 128], False, True, [sm.b(), gv.b()], [PB(po)])
            if os.environ.get("P2CUT") == "5":
                S.barrier()
                return finish([("f", f.ap, [])])

            for h in range(4):
                r0 = (h % 2) * 64
                o_ = ps[pz][:, h * 128:(h + 1) * 128]
                MM(o_, qd_.ap[:, 0, h, :], Sb.ap[:, 0 + h // 2, t - 2, :], True, False, [qd_.b(), Sb.b()], [PB(pz)])
                MM(o_, qd_.ap[:, 1, h, :], Sb.ap[:, 2 + h // 2, t - 2, :], False, True, [qd_.b(), Sb.b()], [PB(pz)])
            osb = oq
            ACT(osb.ap.rearrange("p a b -> p (a b)"), ps[pz][:], AF.Identity, [PB(pz)], [oq.b()])
            TT("dve", osb.ap.rearrange("p a b -> p (a b)"), ps[po][:], osb.ap.rearrange("p a b -> p (a b)"), ALU.add,
               [PB(po), oq.b()], [oq.b()])
            if os.environ.get("P2CUT") == "6":
                S.barrier()
                return finish([("f", f.ap, [])])

            o3 = osb.ap
            sq_ = eb_.ap.rearrange("p (a b) -> p a b", a=4, b=128)
            MSET("pool", f.ap[:, 0:4], 0.0, [f.b()])
            for h in range(4):
                ACT(sq_[:, h, :], o3[:, h, :], AF.Square, [oq.b()], [eb_.b(), f.b()], accum=f.ap[:, h:h + 1])
            ACT(f.ap[:, 4:8], f.ap[:, 0:4], AF.Sqrt, [f.b(), epsb.b()], [f.b()], bias=epsb.ap, scale=1.0 / 128)
            RECIP(f.ap[:, 4:8], f.ap[:, 4:8], [f.b()], [f.b()])
            for h in range(4):
                STT("dve", mix.ap[:, t - 2, 512 + h * 128:512 + (h + 1) * 128], o3[:, h, :], f.ap[:, 4 + h:5 + h], glangb.ap,
                    ALU.mult, ALU.mult, [oq.b(), f.b(), glangb.b()], [mix.b(t - 2)])
            if os.environ.get("P2CUT") == "7":
                S.barrier()
                return finish([("f", f.ap, [])])

        S.barrier()
        if stop == 6:
            return finish([("mix", mix.ap, [])])
        A.top = att_mark

        wb = [A.alloc("wbuf%d" % i, [128, 8, 512], BF16) for i in range(1)]
        sg = [A.alloc("sg%d" % i, [128, 512], F32) for i in range(2)]
        w = wb[0]
        DMA("pool", w.ap, win_d[:, :, 2560:3072], (), [w.b()], "wbuf0")
        for t in range(TL):
            pb = t % 2
            s_ = sg[t % 2]
            for kc in range(8):
                MM(ps[pb][:], hT.ap[:, kc, (t + 2) * 128:(t + 3) * 128], w.ap[:, kc, :], kc == 0, kc == 7,
                   [w.b(), hT.b(t + 2)], [PB(pb)])
            ACT(s_.ap, ps[pb][:], AF.Silu, [PB(pb)], [s_.b()])
            TT("dve" if t % 2 == 0 else "pool", mix.ap[:, t, 512:1024], mix.ap[:, t, 512:1024], s_.ap, ALU.mult,
               [mix.b(t), s_.b()], [mix.b(t)])
        S.barrier()
        if stop == 7:
            return finish([("mix", mix.ap, [])])
        A.top = att_mark

        wo = A.alloc("wo", [128, 8, D], BF16)
        gt1b = A.alloc("gt1b", [128, D], F32)
        DMA("pool", wo.ap, wout_d, (), [wo.b()], "wo")
        DMA("sp", gt1b.ap, mods_d[b, 16:24, :].rearrange("(o a) b -> o (a b)", o=1).partition_broadcast(128), ["mods"], [gt1b.b()], "gt1b")
        mixT = [A.alloc("mixT%d" % i, [128, 8, 128], BF16) for i in range(2)]
        xt = [A.alloc("xt%d" % i, [128, D], F32) for i in range(2)]
        x1 = [A.alloc("x1_%d" % i, [128, D], F32) for i in range(2)]
        ytmp = A.alloc("ytmp", [128, D], F32)
        xs2 = A.alloc("xs2", [128, D], F32)
        h32 = A.alloc("h32", [128, 8, 128], F32)
        junk = A.alloc("junk", [128, D], BF16)
        st = [A.alloc("st%d" % i, [128, 2], F32) for i in range(2)]
        rt = [A.alloc("rt%d" % i, [128, 160], F32) for i in range(2)]
        h2T = hT
        for t in range(TL):
            k = t % 2
            mT, x_, x1_, s_, r_ = mixT[k], xt[k], x1[k], st[k], rt[k]
            DMA("sp", x_.ap, xin[b, (t + 2) * 128:(t + 3) * 128, :], (), [x_.b()], "xt%d" % k)
            pT = ps[0][:].bitcast(BF16)
            for kc in range(8):
                TR(pT[:, kc * 128:(kc + 1) * 128], mix.ap[:, t, kc * 128:(kc + 1) * 128], identb.ap, [mix.b(t), identb.b()], [PB(0)])
            CP("act", mT.ap, pT.rearrange("p (a b) -> p a b", a=8, b=128), [PB(0)], [mT.b()])
            for half in range(2):
                pb = 1 + half
                for kc in range(8):
                    MM(ps[pb][:], mT.ap[:, kc, :], wo.ap[:, kc, half * 512:(half + 1) * 512], kc == 0, kc == 7,
                       [mT.b(), wo.b()], [PB(pb)])
                sl = slice(half * 512, (half + 1) * 512)
                TT("dve", ytmp.ap[:, sl], ps[pb][:], gt1b.ap[:, sl], ALU.mult, [PB(pb), gt1b.b()], [ytmp.b(half)])
                TT("pool", x1_.ap[:, sl], ytmp.ap[:, sl], x_.ap[:, sl], ALU.add, [ytmp.b(half), x_.b()], [x1_.b()])
            DMA("sp", x1s_d[b, t * 128:(t + 1) * 128, :], x1_.ap, [x1_.b()], ["x1s"], "x1_%d" % k)
            rms_stats(x1_.ap, [x1_.b()], junk, s_, D)
            S.op("dve", (lambda o, i, s: lambda e: e.tensor_scalar_mul(out=o, in0=i, scalar1=s))(xs2.ap, x1_.ap, s_.ap[:, 1:2]),
                 [x1_.b(), s_.b()], [xs2.b()])
            for kc in range(8):
                pb = 3 + kc // 4
                TR(ps[pb][:, (kc % 4) * 128:(kc % 4 + 1) * 128], xs2.ap[:, kc * 128:(kc + 1) * 128], identf.ap,
                   [xs2.b(), identf.b()], [PB(pb)])
            for kc in range(8):
                pb = 3 + kc // 4
                i_ = ps[pb][:, (kc % 4) * 128:(kc % 4 + 1) * 128]
                if kc % 2 == 0:
                    ACT(h32.ap[:, kc, :], i_, AF.Identity, [PB(pb), A2.b(), modT.b()], [h32.b()],
                        bias=modT.ap[:, 24 + kc, b:b + 1], scale=A2.ap[:, b, kc:kc + 1])
                else:
                    TS("dve", h32.ap[:, kc, :], i_, A2.ap[:, b, kc:kc + 1], modT.ap[:, 24 + kc, b:b + 1], ALU.mult, ALU.add,
                       [PB(pb), A2.b(), modT.b()], [h32.b()])
            CP("pool", h2T.ap[:, :, t * 128:(t + 1) * 128], h32.ap, [h32.b()], [h2T.b("m%d" % t)])
            for kc in range(8):
                MM(ps[5][:, 0:36], h32.ap[:, kc, :], wr32.ap[:, kc, :], kc == 0, kc == 7, [h32.b(), wr32.b()], [PB(5)])
            R = r_.ap
            rb = [r_.b()]
            lg, els, k1, k2, tmp = R[:, 0:36], R[:, 36:68], R[:, 68:100], R[:, 100:132], R[:, 132:140]
            sc = R[:, 140:160]
            TT("dve", lg, ps[5][:, 0:36], brb.ap, ALU.add, [PB(5), brb.b()], rb)
            RMAX(sc[:, 0:1], lg[:, 0:4], rb, rb)
            TS("dve", tmp[:, 0:4], lg[:, 0:4], sc[:, 0:1], None, ALU.is_equal, None, rb, rb)
            TS("dve", sc[:, 1:2], sc[:, 0:1], -1.0, None, ALU.mult, None, rb, rb)
            MSET("dve", sc[:, 2:3], 0.0, rb)
            ACT(tmp[:, 4:8], lg[:, 0:4], AF.Exp, rb, rb, bias=sc[:, 1:2], accum=sc[:, 2:3])
            RECIP(sc[:, 3:4], sc[:, 2:3], rb, rb)
            TS("dve", tmp[:, 0:4], tmp[:, 0:4], 1e30, -1e30, ALU.mult, ALU.add, rb, rb)
            for g in range(4):
                TS("dve", els[:, g * 8:(g + 1) * 8], lg[:, 4 + g * 8:12 + g * 8], tmp[:, g:g + 1], None, ALU.add, None, rb, rb)
            RMAX(sc[:, 4:5], els, rb, rb)
            TS("dve", k1, els, sc[:, 4:5], None, ALU.is_equal, None, rb, rb)
            STT("dve", els, k1, -1e30, els, ALU.mult, ALU.add, rb, rb)
            RMAX(sc[:, 5:6], els, rb, rb)
            TS("dve", k2, els, sc[:, 5:6], None, ALU.is_equal, None, rb, rb)
            TT("dve", sc[:, 6:7], sc[:, 5:6], sc[:, 4:5], ALU.subtract, rb, rb)
            ACT(sc[:, 7:8], sc[:, 6:7], AF.Exp, rb, rb)
            TS("dve", sc[:, 8:9], sc[:, 7:8], 1.0, None, ALU.add, None, rb, rb)
            RECIP(sc[:, 8:9], sc[:, 8:9], rb, rb)
            TT("dve", sc[:, 9:10], sc[:, 8:9], sc[:, 3:4], ALU.mult, rb, rb)
            TT("dve", sc[:, 10:11], sc[:, 9:10], sc[:, 7:8], ALU.mult, rb, rb)
            TS("dve", k1, k1, sc[:, 9:10], None, ALU.mult, None, rb, rb)
            STT("dve", Wd.ap[:, t, :], k2, sc[:, 10:11], k1, ALU.mult, ALU.add, rb, [Wd.b()])
        S.barrier()
        if stop == 8:
            return finish([("Wd", Wd.ap, []), ("h2T", hT.ap, [])])
        A.top = mixer_mark

        yacc = A.alloc("yacc", [128, TL, D], F32)
        gt2b = A.alloc("gt2b", [128, D], F32)
        rg = [A.alloc("rg%d" % i, [128, 8, 512], BF16) for i in range(2)]
        ru = [A.alloc("ru%d" % i, [128, 8, 512], BF16) for i in range(2)]
        rd = [A.alloc("rd%d" % i, [128, 4, D], BF16) for i in range(2)]
        sgt = [A.alloc("sgt%d" % i, [128, 512], F32) for i in range(2)]
        hTm = [A.alloc("hTm%d" % i, [128, 4, 512], BF16) for i in range(2)]
        DMA("sp", gt2b.ap, mods_d[b, 40:48, :].rearrange("(o a) b -> o (a b)", o=1).partition_broadcast(128), ["mods"], [gt2b.b()], "gt2b")
        for t in range(TL):
            DMA("sp", yacc.ap[:, t, :], x1s_d[b, t * 128:(t + 1) * 128, :], ["x1s"], [yacc.b(t)], "yacc%d" % (t % 4))
        h2all = [h2T.b("m%d" % t) for t in range(TL)]
        cnt = 0
        ycnt = 0
        for e in range(NE):
            k = e % 2
            DMA("pool", rg[k].ap, ewg_d[e], (), [rg[k].b()], "rg%d" % k)
            DMA("pool", ru[k].ap, ewu_d[e], (), [ru[k].b()], "ru%d" % k)
            DMA("pool", rd[k].ap, ewd_d[e], (), [rd[k].b()], "rd%d" % k)
            for hc in range(4):
                TT("pool", rd[k].ap[:, hc, :], rd[k].ap[:, hc, :], gt2b.ap, ALU.mult, [rd[k].b(), gt2b.b()], [rd[k].b()])
            for tb in range(4):
                hm = hTm[tb % 2]
                hr = h2all[tb * 4:tb * 4 + 4]
                for hc in range(4):
                    pg, pu = cnt % 2, 2 + cnt % 2
                    s_ = sgt[cnt % 2]
                    cnt += 1
                    for kc in range(8):
                        MM(ps[pg][:], rg[k].ap[:, kc, hc * 128:(hc + 1) * 128], h2T.ap[:, kc, tb * 512:(tb + 1) * 512],
                           kc == 0, kc == 7, [rg[k].b()] + hr, [PB(pg)])
                    for kc in range(8):
                        MM(ps[pu][:], ru[k].ap[:, kc, hc * 128:(hc + 1) * 128], h2T.ap[:, kc, tb * 512:(tb + 1) * 512],
                           kc == 0, kc == 7, [ru[k].b()] + hr, [PB(pu)])
                    ACT(s_.ap, ps[pg][:], AF.Silu, [PB(pg)], [s_.b()])
                    TT("dve", hm.ap[:, hc, :], ps[pu][:], s_.ap, ALU.mult, [PB(pu), s_.b()], [hm.b()])
                for ti in range(4):
                    t = tb * 4 + ti
                    for half in range(2):
                        py = 4 + ycnt % 4
                        ycnt += 1
                        for hc in range(4):
                            MM(ps[py][:], hm.ap[:, hc, ti * 128:(ti + 1) * 128], rd[k].ap[:, hc, half * 512:(half + 1) * 512],
                               hc == 0, hc == 3, [hm.b(), rd[k].b()], [PB(py)])
                        ya = yacc.ap[:, t, half * 512:(half + 1) * 512]
                        STT("dve", ya, ps[py][:], Wd.ap[:, t, e:e + 1], ya, ALU.mult, ALU.add,
                            [PB(py), Wd.b(), yacc.b(t)], [yacc.b(t)])
        if stop == 9:
            S.barrier()
            return finish([("yacc", yacc.ap, [])])
        fngb = A.alloc("fngb", [128, D], F32)
        DMA("sp", fngb.ap, fng_d.partition_broadcast(128), (), [fngb.b()], "fngb")
        ot = [A.alloc("ot%d" % i, [128, D], F32) for i in range(2)]
        junk = A.alloc("junk", [128, D], BF16)
        st = [A.alloc("st%d" % i, [128, 2], F32) for i in range(2)]
        for t in range(TL):
            o_, s_ = ot[t % 2], st[t % 2]
            rms_stats(yacc.ap[:, t, :], [yacc.b(t)], junk, s_, D)
            STT("dve", o_.ap, yacc.ap[:, t, :], s_.ap[:, 1:2], fngb.ap, ALU.mult, ALU.mult,
                [yacc.b(t), s_.b(), fngb.b()], [o_.b()])
            out_stores.append(DMA("sp", out_d[b, t * 128:(t + 1) * 128, :], o_.ap, [o_.b()], (), "ot%d" % (t % 2)))
        S.barrier()

    S.op("sp", None, extra_deps=out_stores)
    S.emit()
    return nc


def _pk(w, kc):
    K, N = w.shape
    return np.ascontiguousarray(w.reshape(kc, 128, N).transpose(1, 0, 2))


def _rope_tables():
    f = np.arange(16, dtype=np.float32)
    inv = np.power(np.float32(10000.0), -f / np.float32(16)).astype(np.float32)
    t = np.arange(2048)
    pos = np.stack([(t // 64).astype(np.float32), (t % 64).astype(np.float32)], 0)
    cos = np.zeros((128, 2048), np.float32)
    sin = np.zeros((128, 2048), np.float32)
    perm = np.zeros((128, 128), np.float32)
    for p in range(128):
        d = p % 64
        axis, half, fi = d // 32, (d % 32) // 16, d % 16
        ang = (pos[axis] * inv[fi]).astype(np.float32)
        cos[p] = np.cos(ang)
        sin[p] = np.sin(ang) * (-1.0 if half == 0 else 1.0)
        partner = p + 16 if half == 0 else p - 16
        perm[partner, p] = 1.0
    return cos, sin, perm


def _tri():
    j = np.arange(128)[:, None]
    i = np.arange(128)[None, :]
    tri = np.stack([(j <= i), (j >= i), (j > i), (j < i)], 1).astype(np.float32)
    return np.ascontiguousarray(tri)


_NC_CACHE = {}


def kernel(x, c, ctx, c_ctx, w_ada, b_ada, norm_mix_g, norm_ffn_g, w_in, da_lambda, da_subln_g, gla_gate_w2,
           gla_gate_b, gla_norm_g, w_out, router_group_w, router_group_b, router_expert_w, router_expert_b,
           expert_w_gate, expert_w_up, expert_w_down, final_norm_g):
    f = lambda a: np.asarray(a, dtype=np.float32)
    x, c, ctx, c_ctx = f(x), f(c), f(ctx), f(c_ctx)
    n_cores = 8
    cos, sin, perm = _rope_tables()
    w2 = np.zeros((32, 512), np.float32)
    gw2 = f(gla_gate_w2)[0]
    w2[0:16, 0:256] = gw2[0]
    w2[16:32, 256:512] = gw2[1]
    shared = {
        "w_ada": _pk(f(w_ada)[0], 8),
        "b_ada": np.ascontiguousarray(f(b_ada)[0].reshape(48, 128).T),
        "g1": np.ascontiguousarray(f(norm_mix_g)[0].reshape(8, 128).T),
        "g2": np.ascontiguousarray(f(norm_ffn_g)[0].reshape(8, 128).T),
        "w_in": _pk(f(w_in)[0], 8),
        "da_lambda": f(da_lambda)[0].reshape(1, 256),
        "subln_g": f(da_subln_g)[0].reshape(1, 128),
        "gla_norm_g": f(gla_norm_g)[0].reshape(1, 128),
        "final_g": f(final_norm_g).reshape(1, D),
        "gate_w2": w2,
        "gate_b": f(gla_gate_b)[0].reshape(1, 512),
        "w_out": _pk(f(w_out)[0], 8),
        "w_router": _pk(np.concatenate([f(router_group_w)[0], f(router_expert_w)[0]], 1), 8),
        "b_router": np.concatenate([f(router_group_b)[0], f(router_expert_b)[0]]).reshape(1, 36),
        "ewg": np.ascontiguousarray(f(expert_w_gate)[0].reshape(NE, 8, 128, 512).transpose(0, 2, 1, 3)),
        "ewu": np.ascontiguousarray(f(expert_w_up)[0].reshape(NE, 8, 128, 512).transpose(0, 2, 1, 3)),
        "ewd": np.ascontiguousarray(f(expert_w_down)[0].reshape(NE, 4, 128, D).transpose(0, 2, 1, 3)),
        "rope_cos": cos, "rope_sin": sin, "rope_perm": perm, "tri": _tri(),
    }
    in_maps = []
    for i in range(n_cores):
        bs = [NB * i + k for k in range(NB)]
        xin = np.stack([np.concatenate([ctx[bb], x[bb]], 0) for bb in bs], 0)
        cvec = np.stack([c[bs[0]], c[bs[1]], c_ctx], 1)
        m = dict(shared)
        m["xin"] = np.ascontiguousarray(xin)
        m["cT"] = np.ascontiguousarray(cvec.reshape(8, 128, 3).transpose(1, 0, 2))
        in_maps.append(m)
    if "nc" not in _NC_CACHE:
        _NC_CACHE["nc"] = build_program()
    res = run_bass_kernel_spmd(_NC_CACHE["nc"], in_maps, core_ids=list(range(n_cores)))
    out = np.concatenate([np.asarray(r["out"]) for r in res.results], 0)
    return out.astype(np.float32)
```

```python
from contextlib import ExitStack
import os
import math
import numpy as np
import concourse.bass as bass
import concourse.mybir as mybir
from concourse.bass_utils import run_bass_kernel_spmd

F32 = mybir.dt.float32
BF16 = mybir.dt.bfloat16
AF = mybir.ActivationFunctionType
ALU = mybir.AluOpType
AX = mybir.AxisListType

ENGS = ("pe", "act", "dve", "pool", "sp")
NB = 2
NT = 18
TL = 16
D = 1024
EPS = 1e-6
NE = 32


class Buf:
    __slots__ = ("name", "last_writer", "readers")

    def __init__(self, name):
        self.name = name
        self.last_writer = None
        self.readers = []


class Op:
    __slots__ = ("eng", "fn", "deps", "is_dma", "signal", "semval", "key", "dma_val")

    def __init__(self, eng, fn):
        self.eng = eng
        self.fn = fn
        self.deps = set()
        self.is_dma = False
        self.signal = False
        self.semval = 0
        self.key = None
        self.dma_val = 0


class Sched:
    def __init__(self, nc):
        self.nc = nc
        self.ops = {e: [] for e in ENGS}
        self.dma_keys = {}
        self.bufs = {}
        self.last = {e: None for e in ENGS}

    def buf(self, name):
        b = self.bufs.get(name)
        if b is None:
            b = Buf(name)
            self.bufs[name] = b
        return b

    def op(self, eng, fn, reads=(), writes=(), dma_key=None, extra_deps=()):
        o = Op(eng, fn)
        o.deps.update(extra_deps)
        reads = [self.buf(r) for r in reads]
        writes = [self.buf(w) for w in writes]
        for r in reads:
            if r.last_writer is not None:
                o.deps.add(r.last_writer)
        for w in writes:
            if w.last_writer is not None:
                o.deps.add(w.last_writer)
            o.deps.update(w.readers)
        for r in reads:
            r.readers.append(o)
        for w in writes:
            w.last_writer = o
            w.readers = []
        if dma_key is not None:
            o.is_dma = True
            o.key = dma_key
            st = self.dma_keys.setdefault(dma_key, [0, None])
            if st[1] is not None:
                o.deps.add(st[1])
            st[0] += 16
            st[1] = o
            o.dma_val = st[0]
        o.deps.discard(o)
        if eng == "pe":
            o.deps = {d for d in o.deps if d.is_dma or d.eng != "pe"}
        for d in o.deps:
            d.signal = True
        self.ops[eng].append(o)
        if not o.is_dma and fn is not None:
            self.last[eng] = o
        return o

    def barrier(self):
        deps = [o for o in self.last.values() if o is not None]
        deps += [st[1] for st in self.dma_keys.values() if st[1] is not None]
        for e in ENGS:
            self.op(e, None, extra_deps=deps)

    def emit(self):
        nc = self.nc
        with ExitStack() as es:
            esem = {e: es.enter_context(nc.semaphore("s_" + e)) for e in ENGS}
            dsem = {k: es.enter_context(nc.semaphore("d%d" % i)) for i, k in enumerate(self.dma_keys)}
            for e in ENGS:
                c = 0
                for o in self.ops[e]:
                    if o.signal and not o.is_dma and o.fn is not None:
                        c += 1
                        o.semval = c
            block = es.enter_context(nc.Block())

            self.icount = {}

            def run(ename, eng):
                known = {}
                ic = 0
                for o in self.ops[ename]:
                    self.icount[ename] = ic
                    need = {}
                    for d in o.deps:
                        if d.is_dma:
                            s, v = dsem[d.key], d.dma_val
                        else:
                            s, v = esem[d.eng], d.semval
                        if v > need.get(s, (0, None))[0]:
                            need[s] = (v, s)
                    for v, s in need.values():
                        if known.get(s, 0) < v:
                            eng.wait_ge(s, v)
                            known[s] = v
                            ic += 1
                    if o.fn is None:
                        continue
                    ins = o.fn(eng)
                    ic += 1
                    if o.is_dma:
                        ins.then_inc(dsem[o.key], 16)
                    elif o.signal:
                        ins.then_inc(esem[ename], 1)

            block.tensor(lambda eng: run("pe", eng))
            block.scalar(lambda eng: run("act", eng))
            block.vector(lambda eng: run("dve", eng))
            block.gpsimd(lambda eng: run("pool", eng))
            block.sync(lambda eng: run("sp", eng))


class TV:
    def __init__(self, ap, name):
        self.ap = ap
        self.name = name

    def b(self, i=None):
        return self.name if i is None else "%s#%s" % (self.name, i)


class Arena:
    def __init__(self, nc, nbytes):
        self.cap = nbytes
        self.t = nc.alloc_sbuf_tensor("arena", [128, nbytes // 4], F32)
        self.top = 0
        self.cnt = 0
        self.peak = 0

    def alloc(self, name, shape, dtype):
        esz = 4 if dtype == F32 else 2
        free = 1
        for s in shape[1:]:
            free *= s
        nbytes = (free * esz + 63) // 64 * 64
        off = self.top
        self.top += nbytes
        self.peak = max(self.peak, self.top)
        assert self.top <= self.cap, ("SBUF arena overflow", name, self.top)
        ap = self.t[0:shape[0], off // 4:(off + nbytes) // 4]
        if esz == 2:
            ap = ap.bitcast(BF16)
        ap = ap[:, 0:free]
        if len(shape) == 3:
            ap = ap.rearrange("p (a b) -> p a b", a=shape[1], b=shape[2])
        elif len(shape) == 4:
            ap = ap.rearrange("p (a b c) -> p a b c", a=shape[1], b=shape[2], c=shape[3])
        self.cnt += 1
        return TV(ap, "%s@%d" % (name, self.cnt))


def build_program(stop=None):
    nc = bass.Bass("TRN2", target_bir_lowering=False)
    S = Sched(nc)

    def din(name, shape):
        return nc.dram_tensor(name, list(shape), F32, kind="ExternalInput").ap()

    xin = din("xin", [NB, NT * 128, D])
    cT_d = din("cT", [128, 8, 3])
    wada_d = din("w_ada", [128, 8, 6 * D])
    bada_d = din("b_ada", [128, 48])
    g1_d = din("g1", [128, 8])
    g2_d = din("g2", [128, 8])
    win_d = din("w_in", [128, 8, 3104])
    lam_d = din("da_lambda", [1, 256])
    subln_d = din("subln_g", [1, 128])
    glang_d = din("gla_norm_g", [1, 128])
    fng_d = din("final_g", [1, D])
    w2_d = din("gate_w2", [32, 512])
    gb_d = din("gate_b", [1, 512])
    wout_d = din("w_out", [128, 8, D])
    wr_d = din("w_router", [128, 8, 36])
    br_d = din("b_router", [1, 36])
    ewg_d = din("ewg", [NE, 128, 8, 512])
    ewu_d = din("ewu", [NE, 128, 8, 512])
    ewd_d = din("ewd", [NE, 128, 4, D])
    cos_d = din("rope_cos", [128, 2048])
    sin_d = din("rope_sin", [128, 2048])
    perm_d = din("rope_perm", [128, 128])
    tri_d = din("tri", [128, 4, 128])
    out_d = nc.dram_tensor("out", [NB, TL * 128, D], F32, kind="ExternalOutput").ap()
    mods_d = nc.dram_tensor("mods_scr", [NB, 48, 128], F32, kind="Internal").ap()
    x1s_d = nc.dram_tensor("x1_scr", [NB, TL * 128, D], F32, kind="Internal").ap()

    A = Arena(nc, 200 * 1024)
    ps = [nc.alloc_psum_tensor("ps%d" % i, [128, 512], F32) for i in range(8)]

    def PB(i):
        return "psb%d" % i

    def MM(out, lhsT, rhs, start, stop, r, w, sgc=False):
        S.op("pe", lambda e: e.matmul(out, lhsT=lhsT, rhs=rhs, start=start, stop=stop, skip_group_check=sgc), r, w)

    def TR(out, in_, ident, r, w):
        S.op("pe", lambda e: e.transpose(out, in_, ident), r, w)

    def ACT(out, in_, func, r, w, bias=None, scale=None, accum=None):
        kw = {}
        if bias is not None:
            kw["bias"] = bias
        if scale is not None:
            kw["scale"] = scale
        if accum is not None:
            kw["accum_out"] = accum
        S.op("act", lambda e: e.activation(out=out, in_=in_, func=func, **kw), r, w)

    def TT(eng, out, in0, in1, op, r, w):
        S.op(eng, lambda e: e.tensor_tensor(out=out, in0=in0, in1=in1, op=op), r, w)

    def TS(eng, out, in0, s1, s2, op0, op1, r, w):
        if op1 is None:
            S.op(eng, lambda e: e.tensor_scalar(out=out, in0=in0, scalar1=s1, scalar2=None, op0=op0), r, w)
        else:
            S.op(eng, lambda e: e.tensor_scalar(out=out, in0=in0, scalar1=s1, scalar2=s2, op0=op0, op1=op1), r, w)

    def STT(eng, out, in0, sc, in1, op0, op1, r, w):
        S.op(eng, lambda e: e.scalar_tensor_tensor(out=out, in0=in0, scalar=sc, in1=in1, op0=op0, op1=op1), r, w)

    def CP(eng, out, in_, r, w):
        if eng == "act":
            S.op("act", lambda e: e.activation(out=out, in_=in_, func=AF.Identity), r, w)
        else:
            S.op(eng, lambda e: e.tensor_copy(out=out, in_=in_), r, w)

    def RECIP(out, in_, r, w):
        S.op("dve", lambda e: e.reciprocal(out=out, in_=in_), r, w)

    def RMAX(out, in_, r, w):
        S.op("dve", lambda e: e.reduce_max(out=out, in_=in_, axis=AX.X), r, w)

    def RSUM(out, in_, r, w):
        S.op("dve", lambda e: e.reduce_sum(out=out, in_=in_, axis=AX.X), r, w)

    def MSET(eng, ap, val, w):
        S.op(eng, lambda e: e.memset(ap, val), (), w)

    def DMA(q, out, in_, r, w, key):
        return S.op(q, lambda e: e.dma_start(out=out, in_=in_), r, w, dma_key=key)

    def finish(dumps):
        outs = []
        for i, (name, ap, bufs) in enumerate(dumps):
            d = nc.dram_tensor("dbg_" + name, list(ap.shape), ap.dtype, kind="ExternalOutput").ap()
            outs.append(DMA("sp", d, ap, bufs, (), "dbg%d" % i))
        S.op("sp", None, extra_deps=outs)
        S.emit()
        print("ICOUNT", S.icount, {e: max([o.semval for o in S.ops[e]] + [0]) for e in ENGS}, {k: v[0] for k, v in S.dma_keys.items()})
        return nc

    identb = A.alloc("identb", [128, 128], BF16)
    identf = A.alloc("identf", [128, 128], F32)
    permb = A.alloc("permb", [128, 128], BF16)
    tri = A.alloc("tri", [128, 4, 128], F32)
    epsb = A.alloc("epsb", [128, 1], F32)
    onesf = A.alloc("onesf", [128, 1], F32)
    onesrow = A.alloc("onesrow", [1, 128], BF16)
    g1 = A.alloc("g1", [128, 8], F32)
    g2 = A.alloc("g2", [128, 8], F32)
    bada = A.alloc("bada", [128, 48], F32)
    cT = A.alloc("cT", [128, 8, 3], F32)
    scT = A.alloc("scT", [128, 8, 3], BF16)
    modT = A.alloc("modT", [128, 48, 3], F32)
    A1 = A.alloc("A1", [128, 3, 8], F32)
    A2 = A.alloc("A2", [128, 3, 8], F32)
    wr32 = A.alloc("wr32", [128, 8, 36], F32)
    brb = A.alloc("brb", [128, 36], F32)
    w2blk = A.alloc("w2blk", [32, 512], BF16)
    gbrow = A.alloc("gbrow", [1, 512], BF16)
    sublnb = A.alloc("sublnb", [128, 128], F32)
    glangb = A.alloc("glangb", [128, 128], F32)
    lamb = A.alloc("lamb", [128, 256], F32)
    lamt = A.alloc("lamt", [128, 8], F32)
    Wd = A.alloc("Wd", [128, TL, NE], F32)
    hT = A.alloc("hT", [128, 8, NT * 128], BF16)

    for tv, val in ((identb, 1.0), (identf, 1.0)):
        MSET("pool", tv.ap, val, [tv.b()])
        S.op("pool", (lambda ap: lambda e: e.affine_select(out=ap, in_=ap, pattern=[[-1, 128]],
                                                             compare_op=ALU.is_equal, fill=0.0, base=0,
                                                             channel_multiplier=1))(tv.ap),
             [tv.b()], [tv.b()])
    MSET("pool", epsb.ap, EPS, [epsb.b()])
    MSET("pool", onesf.ap, 1.0, [onesf.b()])
    MSET("pool", onesrow.ap, 1.0, [onesrow.b()])
    DMA("pool", permb.ap, perm_d, (), [permb.b()], "permb")
    DMA("sp", tri.ap, tri_d, (), [tri.b()], "tri")
    DMA("sp", g1.ap, g1_d, (), [g1.b()], "g1")
    DMA("sp", g2.ap, g2_d, (), [g2.b()], "g2")
    DMA("sp", bada.ap, bada_d, (), [bada.b()], "bada")
    DMA("sp", cT.ap, cT_d, (), [cT.b()], "cT")
    DMA("sp", wr32.ap, wr_d, (), [wr32.b()], "wr32")
    DMA("sp", brb.ap, br_d.partition_broadcast(128), (), [brb.b()], "brb")
    DMA("pool", w2blk.ap, w2_d, (), [w2blk.b()], "w2blk")
    DMA("pool", gbrow.ap, gb_d, (), [gbrow.b()], "gbrow")
    DMA("sp", sublnb.ap, subln_d.partition_broadcast(128), (), [sublnb.b()], "sublnb")
    DMA("sp", glangb.ap, glang_d.partition_broadcast(128), (), [glangb.b()], "glangb")
    DMA("sp", lamb.ap, lam_d.partition_broadcast(128), (), [lamb.b()], "lamb")
    lambda_init = 0.8 - 0.6 * math.exp(0.0)
    TS("dve", sublnb.ap, sublnb.ap, 1.0 - lambda_init, None, ALU.mult, None, [sublnb.b()], [sublnb.b()])
    lt = lamt.ap
    lb = lamb.ap
    TT("dve", lb[:, 0:64], lb[:, 0:64], lb[:, 64:128], ALU.mult, [lamb.b()], [lamb.b()])
    TT("dve", lb[:, 128:192], lb[:, 128:192], lb[:, 192:256], ALU.mult, [lamb.b()], [lamb.b()])
    RSUM(lt[:, 0:1], lb[:, 0:64], [lamb.b()], [lamt.b()])
    RSUM(lt[:, 1:2], lb[:, 128:192], [lamb.b()], [lamt.b()])
    ACT(lt[:, 2:4], lt[:, 0:2], AF.Exp, [lamt.b()], [lamt.b()])
    TT("dve", lt[:, 4:5], lt[:, 3:4], lt[:, 2:3], ALU.subtract, [lamt.b()], [lamt.b()])
    TS("dve", lt[:, 7:8], lt[:, 4:5], -lambda_init, None, ALU.add, None, [lamt.b()], [lamt.b()])
    nlam = lt[:, 7:8]

    ACT(scT.ap, cT.ap, AF.Silu, [cT.b()], [scT.b()])
    m0 = A.top
    wb = [A.alloc("wbuf%d" % i, [128, 8, 512], BF16) for i in range(2)]
    psA = ps[0][:, 0:144]
    for grp in range(12):
        w = wb[grp % 2]
        DMA("pool", w.ap, wada_d[:, :, grp * 512:(grp + 1) * 512], (), [w.b()], "wbuf%d" % (grp % 2))
        for c in range(4):
            fo = grp * 4 + c
            for kc in range(8):
                MM(psA[:, fo * 3:fo * 3 + 3], w.ap[:, kc, c * 128:(c + 1) * 128], scT.ap[:, kc, :],
                   kc == 0, kc == 7, [w.b(), scT.b()], [PB(0)])
    psA3 = psA.rearrange("p (a b) -> p a b", a=48, b=3)
    for j in range(3):
        TT("dve", modT.ap[:, :, j], psA3[:, :, j], bada.ap, ALU.add, [PB(0), bada.b()], [modT.b()])
        STT("dve", A1.ap[:, j, :], modT.ap[:, 8:16, j], 1.0, g1.ap, ALU.add, ALU.mult, [modT.b(), g1.b()], [A1.b()])
        STT("dve", A2.ap[:, j, :], modT.ap[:, 32:40, j], 1.0, g2.ap, ALU.add, ALU.mult, [modT.b(), g2.b()], [A2.b()])
    mtmp = A.alloc("mtmp", [128, 48], F32)
    mrow = A.alloc("mrow", [48, 128], F32)
    for j in range(NB):
        CP("dve", mtmp.ap, modT.ap[:, :, j], [modT.b()], [mtmp.b()])
        TR(ps[1][0:48, 0:128], mtmp.ap, identf.ap, [mtmp.b(), identf.b()], [PB(1)])
        CP("dve", mrow.ap, ps[1][0:48, 0:128], [PB(1)], [mrow.b()])
        DMA("sp", mods_d[j], mrow.ap, [mrow.b()], ["mods"], "mods_w")
    S.barrier()
    if stop == 0:
        g1t = A.alloc("g1t", [128, D], F32)
        DMA("sp", g1t.ap, mods_d[0, 16:24, :].rearrange("(o a) b -> o (a b)", o=1).partition_broadcast(128), ["mods"], [g1t.b()], "gt1b")
        return finish([("modT", modT.ap, [modT.b()]), ("A1", A1.ap, [A1.b()]), ("A2", A2.ap, [A2.b()]),
                       ("lamt", lamt.ap, [lamt.b()]), ("gt1b", g1t.ap, [g1t.b()]), ("subln", sublnb.ap, [sublnb.b()]),
                       ("identb", identb.ap, [identb.b()])])
    A.top = m0
    base_top = A.top
    if os.environ.get("PAD"):
        pd_ = A.alloc("pad", [128, 8], F32)
        base_top = A.top
        for i in range(int(os.environ["PAD"])):
            MSET("dve", pd_.ap, float(i), [pd_.b()])

    def rms_stats(src_ap, src_b, junk, st, n):
        MSET("pool", st.ap[:, 0:1], 0.0, [st.b()])
        ACT(junk.ap, src_ap, AF.Square, src_b, [junk.b(), st.b()], accum=st.ap[:, 0:1])
        ACT(st.ap[:, 1:2], st.ap[:, 0:1], AF.Sqrt, [st.b(), epsb.b()], [st.b()], bias=epsb.ap, scale=1.0 / n)
        RECIP(st.ap[:, 1:2], st.ap[:, 1:2], [st.b()], [st.b()])

    out_stores = []
    TOKB = [(0, 512), (512, 512), (1024, 512), (1536, 512), (2048, 256)]
    for b in range(NB):
        A.top = base_top
        mixer_mark = A.top
        mix = A.alloc("mix", [128, TL, D], BF16)
        att_mark = A.top
        cosb = A.alloc("cosb", [128, 2048], BF16)
        sinb = A.alloc("sinb", [128, 2048], BF16)
        QT = A.alloc("QT", [128, 4, 2048], BF16)
        KT = A.alloc("KT", [128, 4, NT * 128], BF16)
        V = A.alloc("V", [128, NT, 4, 130], BF16)
        DMA("pool", cosb.ap, cos_d, (), [cosb.b()], "cosb")
        DMA("pool", sinb.ap, sin_d, (), [sinb.b()], "sinb")
        MSET("pool", V.ap[:, :, :, 128:130], 1.0, [V.b("ones")])
        pa_mark = A.top
        xt = [A.alloc("xt%d" % i, [128, D], F32) for i in range(2)]
        xs = [A.alloc("xs%d" % i, [128, D], BF16) for i in range(2)]
        junk = A.alloc("junk", [128, D], BF16)
        st = [A.alloc("st%d" % i, [128, 2], F32) for i in range(2)]
        for t in range(NT):
            j = 2 if t < 2 else b
            x_, s_, t_ = xt[t % 2], xs[t % 2], st[t % 2]
            DMA("sp", x_.ap, xin[b, t * 128:(t + 1) * 128, :], (), [x_.b()], "xt%d" % (t % 2))
            rms_stats(x_.ap, [x_.b()], junk, t_, D)
            S.op("dve", (lambda o, i, s: lambda e: e.tensor_scalar_mul(out=o, in0=i, scalar1=s))(s_.ap, x_.ap, t_.ap[:, 1:2]),
                 [x_.b(), t_.b()], [s_.b()])
            pb = 2 + (t % 2)
            pT = ps[pb][:].bitcast(BF16)
            for kc in range(8):
                TR(pT[:, kc * 128:(kc + 1) * 128], s_.ap[:, kc * 128:(kc + 1) * 128], identb.ap,
                   [s_.b(), identb.b()], [PB(pb)])
            for kc in range(8):
                o_ = hT.ap[:, kc, t * 128:(t + 1) * 128]
                i_ = pT[:, kc * 128:(kc + 1) * 128]
                if kc % 2 == 0:
                    ACT(o_, i_, AF.Identity, [PB(pb), A1.b(), modT.b()], [hT.b(t)],
                        bias=modT.ap[:, kc, j:j + 1], scale=A1.ap[:, j, kc:kc + 1])
                else:
                    TS("dve", o_, i_, A1.ap[:, j, kc:kc + 1], modT.ap[:, kc, j:j + 1], ALU.mult, ALU.add,
                       [PB(pb), A1.b(), modT.b()], [hT.b(t)])
        hT_all = [hT.b(t) for t in range(NT)]
        if stop == 1:
            S.barrier()
            return finish([("hT", hT.ap, hT_all)])

        wb = [A.alloc("wbuf%d" % i, [128, 8, 512], BF16) for i in range(2)]
        qraw = [A.alloc("qraw%d" % i, [128, 512], BF16) for i in range(2)]
        rt1 = [A.alloc("rt1_%d" % i, [128, 512], F32) for i in range(2)]
        rt2 = [A.alloc("rt2_%d" % i, [128, 512], F32) for i in range(2)]
        cnt = 0
        for gi, (dst, col0) in enumerate(((QT, 0), (KT, 512))):
            w = wb[gi % 2]
            DMA("pool", w.ap, win_d[:, :, col0:col0 + 512], (), [w.b()], "wbuf%d" % (gi % 2))
            for h in range(4):
                if dst is KT:
                    pb = cnt % 2
                    for kc in range(8):
                        MM(ps[pb][:, 0:256], w.ap[:, kc, h * 128:(h + 1) * 128], hT.ap[:, kc, 0:256], kc == 0, kc == 7,
                           [w.b(), hT.b(0), hT.b(1)], [PB(pb)])
                    CP("act", KT.ap[:, h, 0:256], ps[pb][:, 0:256], [PB(pb)], [KT.b("%d_c" % h)])
                    cnt += 1
                for tb_i in range(4):
                    tb = 0 if os.environ.get('TB0') else tb_i
                    pz_ = 0 if os.environ.get("PAR0") else cnt % 2
                    pb = pz_ if not os.environ.get("PARB") else cnt % 2
                    pw = 2 + pz_
                    qr, r1, r2 = qraw[pz_], rt1[pz_], rt2[pz_]
                    tok = 256 + tb * 512
                    hr = [hT.b(2 + tb * 4 + i) for i in range(4)]
                    for kc in range(8):
                        MM(ps[pb][:], w.ap[:, kc, h * 128:(h + 1) * 128], hT.ap[:, kc, tok:tok + 512], kc == 0, kc == 7,
                           [w.b()] + hr, [PB(pb)])
                    CP("act", qr.ap, ps[pb][:], [PB(pb)], [qr.b()])
                    MM(ps[pw][:], permb.ap, qr.ap, True, True, [permb.b(), qr.b()], [PB(pw)])
                    TT("dve", r1.ap, ps[pb][:], cosb.ap[:, tb * 512:(tb + 1) * 512], ALU.mult, [PB(pb), cosb.b(), qr.b()], [r1.b()])
                    TT("dve", r2.ap, ps[pw][:], sinb.ap[:, tb * 512:(tb + 1) * 512], ALU.mult, [PB(pw), sinb.b()], [r2.b()])
                    if dst is QT:
                        o_ = QT.ap[:, h, tb * 512:(tb + 1) * 512]
                        ob = QT.b("%d_%d" % (h, tb))
                    else:
                        o_ = KT.ap[:, h, tok:tok + 512]
                        ob = KT.b("%d_%d" % (h, tb))
                    TT("dve" if os.environ.get("POOLDVE") else "pool", o_, r1.ap, r2.ap, ALU.add, [r1.b(), r2.b()], [ob])
                    if cnt + 1 == int(os.environ.get("CUTN", "0")):
                        S.barrier()
                        return finish([("QT", QT.ap, []), ("KT", KT.ap, [])])
                    cnt += 1
        w = wb[0]
        DMA("pool", w.ap, win_d[:, :, 1024:1536], (), [w.b()], "wbuf0")
        for t in range(NT):
            pb = 4 + t % 2
            for kc in range(8):
                MM(ps[pb][:], hT.ap[:, kc, t * 128:(t + 1) * 128], w.ap[:, kc, :], kc == 0, kc == 7,
                   [w.b(), hT.b(t)], [PB(pb)])
            CP("act" if t % 2 == 0 else "dve", V.ap[:, t, :, 0:128],
               ps[pb][:].rearrange("p (a b) -> p a b", a=4, b=128), [PB(pb)], [V.b(t)])
        S.barrier()
        if stop == 2:
            return finish([("QT", QT.ap, []), ("KT", KT.ap, []), ("V", V.ap, [])])
        A.top = pa_mark

        ET = [A.alloc("ET%d" % i, [128, 512], BF16) for i in range(3)]
        fin = [A.alloc("fin%d" % i, [128, 8], F32) for i in range(2)]
        ta = [A.alloc("ta%d" % i, [128, 128], F32) for i in range(2)]
        tb_ = [A.alloc("tb%d" % i, [128, 128], F32) for i in range(2)]
        junk = A.alloc("junk", [128, 128], BF16)
        Vall = [V.b(t) for t in range(NT)] + [V.b("ones")]
        fcnt = 0
        steps = [(h, qb, m, kt) for h in range(4) for qb in range(4) for m in range(2) for kt in range(NT)]

        def acc_of(h, qb, m, qs):
            par = (h * 4 + qb) % 2
            r = m * 4 + qs
            return ps[2 + par * 3 + r // 3][:, (r % 3) * 129:(r % 3) * 129 + 129], "acc%d_%d" % (par, r)

        def emit_S(i):
            h, qb, m, kt = steps[i]
            Kall = [KT.b("%d_c" % h)] + [KT.b("%d_%d" % (h, j)) for j in range(4)]
            MM(ps[i % 2][:], KT.ap[m * 64:(m + 1) * 64, h, kt * 128:(kt + 1) * 128],
               QT.ap[m * 64:(m + 1) * 64, h, qb * 512:(qb + 1) * 512], True, True,
               Kall + [QT.b("%d_%d" % (h, qb))], [PB(i % 2)])

        started = set()
        emit_S(0)
        emit_S(1)
        for i, (h, qb, m, kt) in enumerate(steps):
            if m == 0 and kt == 0:
                started = set()
            e_ = ET[i % 3]
            ACT(e_.ap, ps[i % 2][:], AF.Exp, [PB(i % 2)], [e_.b()], scale=0.125)
            if i + 2 < len(steps):
                emit_S(i + 2)
            for qs in range(4):
                ap_, bn = acc_of(h, qb, m, qs)
                bank = (m * 4 + qs) // 3
                st_ = kt == 0 and bank not in started
                started.add(bank)
                MM(ap_, e_.ap[:, qs * 128:(qs + 1) * 128], V.ap[:, kt, h, 0:129], st_, kt == NT - 1,
                   [e_.b()] + Vall, [bn], sgc=True)
            if m == 1 and kt == NT - 1:
                for qs in range(4):
                    a1, b1 = acc_of(h, qb, 0, qs)
                    a2, b2 = acc_of(h, qb, 1, qs)
                    f, x_, y_ = fin[fcnt % 2], ta[fcnt % 2], tb_[fcnt % 2]
                    fcnt += 1
                    tq = qb * 4 + qs
                    RECIP(f.ap[:, 0:1], a1[:, 128:129], [b1], [f.b()])
                    RECIP(f.ap[:, 1:2], a2[:, 128:129], [b2], [f.b()])
                    TT("dve", f.ap[:, 2:3], f.ap[:, 1:2], nlam, ALU.mult, [f.b(), lamt.b()], [f.b()])
                    TS("dve", x_.ap, a1[:, 0:128], f.ap[:, 0:1], None, ALU.mult, None, [b1, f.b()], [x_.b()])
                    STT("dve", y_.ap, a2[:, 0:128], f.ap[:, 2:3], x_.ap, ALU.mult, ALU.add, [b2, f.b(), x_.b()], [y_.b()])
                    MSET("pool", f.ap[:, 3:4], 0.0, [f.b()])
                    ACT(junk.ap, y_.ap, AF.Square, [y_.b()], [junk.b(), f.b()], accum=f.ap[:, 3:4])
                    ACT(f.ap[:, 4:5], f.ap[:, 3:4], AF.Sqrt, [f.b(), epsb.b()], [f.b()], bias=epsb.ap, scale=1.0 / 128)
                    RECIP(f.ap[:, 4:5], f.ap[:, 4:5], [f.b()], [f.b()])
                    STT("dve", mix.ap[:, tq, h * 128:(h + 1) * 128], y_.ap, f.ap[:, 4:5], sublnb.ap, ALU.mult, ALU.mult,
                        [y_.b(), f.b(), sublnb.b()], [mix.b(tq)])
        S.barrier()
        if stop == 3:
            return finish([("mix", mix.ap, [])])
        A.top = att_mark

        gqk = A.alloc("gqk", [128, 4, NT * 128], BF16)
        gkt = A.alloc("gkt", [128, NT, 256], BF16)
        gv = A.alloc("gv", [128, NT, 512], BF16)
        lrT = A.alloc("lrT", [32, NT * 128], BF16)
        Sb = A.alloc("Sb", [128, 4, TL, 128], BF16)
        gla_mark = A.top
        wb = [A.alloc("wbuf%d" % i, [128, 8, 512], BF16) for i in range(2)]
        w = wb[0]
        DMA("pool", w.ap, win_d[:, :, 1536:2048], (), [w.b()], "wbuf0")
        cnt = 0
        for c in range(4):
            for (tok, n) in TOKB:
                pb = cnt % 2
                hr = [hT.b(tok // 128 + i) for i in range(n // 128)]
                for kc in range(8):
                    MM(ps[pb][:, 0:n], w.ap[:, kc, c * 128:(c + 1) * 128], hT.ap[:, kc, tok:tok + n], kc == 0, kc == 7,
                       [w.b()] + hr, [PB(pb)])
                CP("act" if cnt % 2 == 0 else "dve", gqk.ap[:, c, tok:tok + n], ps[pb][:, 0:n], [PB(pb)], [gqk.b()])
                cnt += 1
        for t in range(NT):
            pb = 2 + t % 2
            for kc in range(8):
                MM(ps[pb][:, 0:256], hT.ap[:, kc, t * 128:(t + 1) * 128], w.ap[:, kc, 256:512], kc == 0, kc == 7,
                   [w.b(), hT.b(t)], [PB(pb)])
            CP("act" if t % 2 == 0 else "dve", gkt.ap[:, t, :], ps[pb][:, 0:256], [PB(pb)], [gkt.b()])
        w = wb[1]
        DMA("pool", w.ap, win_d[:, :, 2048:2560], (), [w.b()], "wbuf1")
        for t in range(NT):
            pb = 4 + t % 2
            for kc in range(8):
                MM(ps[pb][:], hT.ap[:, kc, t * 128:(t + 1) * 128], w.ap[:, kc, :], kc == 0, kc == 7,
                   [w.b(), hT.b(t)], [PB(pb)])
            CP("act" if t % 2 == 0 else "dve", gv.ap[:, t, :], ps[pb][:], [PB(pb)], [gv.b()])
        w = wb[0]
        DMA("pool", w.ap[:, :, 0:32], win_d[:, :, 3072:3104], (), [w.b()], "wbuf0")
        for (tok, n) in TOKB:
            pb = 6 + cnt % 2
            cnt += 1
            hr = [hT.b(tok // 128 + i) for i in range(n // 128)]
            for kc in range(8):
                MM(ps[pb][0:32, 0:n], w.ap[:, kc, 0:32], hT.ap[:, kc, tok:tok + n], kc == 0, kc == 7, [w.b()] + hr, [PB(pb)])
            CP("act", lrT.ap[:, tok:tok + n], ps[pb][0:32, 0:n], [PB(pb)], [lrT.b()])
        S.barrier()
        if stop == 4:
            return finish([("gqk", gqk.ap, []), ("gkt", gkt.ap, []), ("gv", gv.ap, []), ("lrT", lrT.ap, [])])
        A.top = gla_mark

        St = A.alloc("St", [128, 4, 128], F32)
        MSET("dve", St.ap, 0.0, [St.b(i) for i in range(4)])
        G_ = [A.alloc("G%d" % i, [128, 256], F32) for i in range(2)]
        ER = [A.alloc("ER%d" % i, [128, 256], F32) for i in range(2)]
        k2e = [A.alloc("k2e%d" % i, [128, 256], BF16) for i in range(2)]
        dec = [A.alloc("dec%d" % i, [128, 2], F32) for i in range(2)]
        orders = (list(range(NT)), [1, 0] + list(range(NT - 1, 1, -1)))
        cnt = 0
        for s in range(NT):
            for d in range(2):
                t = orders[d][s]
                g_, er, ke, dc = G_[cnt % 2], ER[cnt % 2], k2e[cnt % 2], dec[cnt % 2]
                pz = cnt % 2
                pu = 2 + cnt % 2
                pd = 4 + cnt % 2
                cnt += 1
                MM(ps[pz][:, 0:256], lrT.ap[:, t * 128:(t + 1) * 128], w2blk.ap[:, d * 256:(d + 1) * 256], True, False,
                   [lrT.b(), w2blk.b()], [PB(pz)])
                MM(ps[pz][:, 0:256], onesrow.ap, gbrow.ap[:, d * 256:(d + 1) * 256], False, True,
                   [onesrow.b(), gbrow.b()], [PB(pz)])
                ACT(g_.ap, ps[pz][:, 0:256], AF.Exp, [PB(pz)], [g_.b()], scale=-1.0)
                ACT(g_.ap, g_.ap, AF.Ln, [g_.b()], [g_.b()], bias=1.0)
                MM(ps[pz][:, 256:512], tri.ap[:, 2 + d, :], g_.ap, True, True, [tri.b(), g_.b()], [PB(pz)])
                ACT(er.ap, ps[pz][:, 256:512], AF.Exp, [PB(pz)], [er.b()], scale=-1.0 / 16)
                TT("dve", ke.ap, gkt.ap[:, t, :], er.ap, ALU.mult, [gkt.b(), er.b()], [ke.b()])
                for p in range(2):
                    MM(ps[pd][:, p:p + 1], g_.ap[:, p * 128:(p + 1) * 128], onesf.ap, True, True, [g_.b(), onesf.b()], [PB(pd)])
                    MM(ps[pu][:, p * 256:(p + 1) * 256], ke.ap[:, p * 128:(p + 1) * 128], gv.ap[:, t, p * 256:(p + 1) * 256],
                       True, True, [ke.b(), gv.b()], [PB(pu)])
                ACT(dc.ap, ps[pd][:, 0:2], AF.Exp, [PB(pd)], [dc.b()], scale=-1.0 / 16)
                for p in range(2):
                    i = d * 2 + p
                    if t >= 2:
                        CP("pool", Sb.ap[:, i, t - 2, :], St.ap[:, i, :], [St.b(i)], [Sb.b()])
                    for hh in range(2):
                        r0, r1_ = hh * 64, hh * 64 + 64
                        STT("dve", St.ap[r0:r1_, i, :], St.ap[r0:r1_, i, :], dc.ap[r0:r1_, p:p + 1],
                            ps[pu][r0:r1_, p * 256 + hh * 128:p * 256 + hh * 128 + 128], ALU.mult, ALU.add,
                            [St.b(i), dc.b(), PB(pu)], [St.b(i)])
        S.barrier()
        if stop == 5:
            return finish([("Sb", Sb.ap, []), ("St", St.ap, [])])
        A.top = gla_mark

        G2 = [A.alloc("G2_%d" % i, [128, 512], F32) for i in range(2)]
        eb = [A.alloc("eb%d" % i, [128, 512], F32) for i in range(2)]
        ei = [A.alloc("ei%d" % i, [128, 512], F32) for i in range(2)]
        qd = [A.alloc("qd%d" % i, [128, 2, 2, 128], BF16) for i in range(2)]
        ki = [A.alloc("ki%d" % i, [128, 2, 2, 128], BF16) for i in range(2)]
        qz = [A.alloc("qz%d" % i, [128, 2, 4, 128], BF16) for i in range(2)]
        for i in range(2):
            MSET("pool", qz[i].ap, 0.0, [qz[i].b()])
        sTm = [A.alloc("sTm%d" % i, [128, 2, 4, 128], BF16) for i in range(2)]
        osq = [A.alloc("osq%d" % i, [128, 4, 128], F32) for i in range(2)]
        fs = [A.alloc("fs%d" % i, [128, 8], F32) for i in range(2)]
        for t in range(2, NT):
            k = t % 2
            g_, eb_, ei_, qd_, ki_, sm, oq, f = G2[k], eb[k], ei[k], qz[k], ki[k], sTm[k], osq[k], fs[k]
            pz, pbk, ps0, ps1, po = 0 + k, 2 + k, 4, 5, 6 + k
            MM(ps[pz][:], lrT.ap[:, t * 128:(t + 1) * 128], w2blk.ap, True, False, [lrT.b(), w2blk.b()], [PB(pz)])
            MM(ps[pz][:], onesrow.ap, gbrow.ap, False, True, [onesrow.b(), gbrow.b()], [PB(pz)])
            ACT(g_.ap, ps[pz][:], AF.Exp, [PB(pz)], [g_.b()], scale=-1.0)
            ACT(g_.ap, g_.ap, AF.Ln, [g_.b()], [g_.b()], bias=1.0)
            for d in range(2):
                for p in range(2):
                    c0 = (d * 2 + p) * 128
                    MM(ps[pbk][:, c0:c0 + 128], g_.ap[:, d * 256 + p * 128:d * 256 + p * 128 + 128], tri.ap[:, d, :], True, True,
                       [g_.b(), tri.b()], [PB(pbk)])
            ACT(eb_.ap, ps[pbk][:], AF.Exp, [PB(pbk)], [eb_.b()], scale=-1.0 / 16)
            ACT(ei_.ap, ps[pbk][:], AF.Exp, [PB(pbk)], [ei_.b()], scale=1.0 / 16)
            if os.environ.get("P2CUT") == "2":
                S.barrier()
                return finish([("f", f.ap, [])])

            for d in range(2):
                for p in range(2):
                    c0 = (d * 2 + p) * 128
                    for hh in range(2):
                        q0 = hh * 64
                        STT("dve", qd_.ap[q0:q0 + 64, d, 2 * p + hh, :], gqk.ap[q0:q0 + 64, p, t * 128:(t + 1) * 128], 0.125,
                            eb_.ap[q0:q0 + 64, c0:c0 + 128], ALU.mult, ALU.mult, [gqk.b(), eb_.b()], [qd_.b()])
                    TT("dve", ki_.ap[:, d, p, :], gqk.ap[:, 2 + p, t * 128:(t + 1) * 128], ei_.ap[:, c0:c0 + 128], ALU.mult,
                       [gqk.b(), ei_.b()], [ki_.b()])
            if os.environ.get("P2CUT") == "3":
                S.barrier()
                return finish([("f", f.ap, [])])

            for d in range(2):
                psd = ps0 if d == 0 else ps1
                for h in range(4):
                    r0 = (h % 2) * 64
                    MM(ps[psd][:, h * 128:(h + 1) * 128], ki_.ap[:, d, h // 2, :], qd_.ap[:, d, h, :],
                       True, True, [ki_.b(), qd_.b()], [PB(psd)])
                if os.environ.get("P2CUT") == "41":
                    S.barrier()
                    return finish([("f", f.ap, [])])
                for h in range(4):
                    TT("dve", sm.ap[:, d, h, :], ps[psd][:, h * 128:(h + 1) * 128], tri.ap[:, d, :], ALU.mult,
                       [PB(psd), tri.b()], [sm.b()])
                if os.environ.get("P2CUT") == "42":
                    S.barrier()
                    return finish([("f", f.ap, [])])
            if os.environ.get("P2CUT") == "4":
                S.barrier()
                return finish([("f", f.ap, [])])

            for h in range(4):
                r0 = (h % 2) * 64
                o_ = ps[po][:, h * 128:(h + 1) * 128]
                MM(o_, sm.ap[:, 0, h, :], gv.ap[:, t, h * 128:(h + 1) * 128], True, False, [sm.b(), gv.b()], [PB(po)])
                MM(o_, sm.ap[:, 1, h, :], gv.ap[:, t, h * 128:(h + 1) * 128], False, True, [sm.b(), gv.b()], [PB(po)])
            if os.environ.get("P2CUT") == "5":
                S.barrier()
                return finish([("f", f.ap, [])])

            for h in range(4):
                r0 = (h % 2) * 64
                o_ = ps[pz][:, h * 128:(h + 1) * 128]
                MM(o_, qd_.ap[:, 0, h, :], Sb.ap[:, 0 + h // 2, t - 2, :], True, False, [qd_.b(), Sb.b()], [PB(pz)])
                MM(o_, qd_.ap[:, 1, h, :], Sb.ap[:, 2 + h // 2, t - 2, :], False, True, [qd_.b(), Sb.b()], [PB(pz)])
            osb = oq
            ACT(osb.ap.rearrange("p a b -> p (a b)"), ps[pz][:], AF.Identity, [PB(pz)], [oq.b()])
            TT("dve", osb.ap.rearrange("p a b -> p (a b)"), ps[po][:], osb.ap.rearrange("p a b -> p (a b)"), ALU.add,
               [PB(po), oq.b()], [oq.b()])
            if os.environ.get("P2CUT") == "6":
                S.barrier()
                return finish([("f", f.ap, [])])

            o3 = osb.ap
            sq_ = eb_.ap.rearrange("p (a b) -> p a b", a=4, b=128)
            MSET("pool", f.ap[:, 0:4], 0.0, [f.b()])
            for h in range(4):
                ACT(sq_[:, h, :], o3[:, h, :], AF.Square, [oq.b()], [eb_.b(), f.b()], accum=f.ap[:, h:h + 1])
            ACT(f.ap[:, 4:8], f.ap[:, 0:4], AF.Sqrt, [f.b(), epsb.b()], [f.b()], bias=epsb.ap, scale=1.0 / 128)
            RECIP(f.ap[:, 4:8], f.ap[:, 4:8], [f.b()], [f.b()])
            for h in range(4):
                STT("dve", mix.ap[:, t - 2, 512 + h * 128:512 + (h + 1) * 128], o3[:, h, :], f.ap[:, 4 + h:5 + h], glangb.ap,
                    ALU.mult, ALU.mult, [oq.b(), f.b(), glangb.b()], [mix.b(t - 2)])
            if os.environ.get("P2CUT") == "7":
                S.barrier()
                return finish([("f", f.ap, [])])

        S.barrier()
        if stop == 6:
            return finish([("mix", mix.ap, [])])
        A.top = att_mark

        wb = [A.alloc("wbuf%d" % i, [128, 8, 512], BF16) for i in range(1)]
        sg = [A.alloc("sg%d" % i, [128, 512], F32) for i in range(2)]
        w = wb[0]
        DMA("pool", w.ap, win_d[:, :, 2560:3072], (), [w.b()], "wbuf0")
        for t in range(TL):
            pb = t % 2
            s_ = sg[t % 2]
            for kc in range(8):
                MM(ps[pb][:], hT.ap[:, kc, (t + 2) * 128:(t + 3) * 128], w.ap[:, kc, :], kc == 0, kc == 7,
                   [w.b(), hT.b(t + 2)], [PB(pb)])
            ACT(s_.ap, ps[pb][:], AF.Silu, [PB(pb)], [s_.b()])
            TT("dve" if t % 2 == 0 else "pool", mix.ap[:, t, 512:1024], mix.ap[:, t, 512:1024], s_.ap, ALU.mult,
               [mix.b(t), s_.b()], [mix.b(t)])
        S.barrier()
        if stop == 7:
            return finish([("mix", mix.ap, [])])
        A.top = att_mark

        wo = A.alloc("wo", [128, 8, D], BF16)
        gt1b = A.alloc("gt1b", [128, D], F32)
        DMA("pool", wo.ap, wout_d, (), [wo.b()], "wo")
        DMA("sp", gt1b.ap, mods_d[b, 16:24, :].rearrange("(o a) b -> o (a b)", o=1).partition_broadcast(128), ["mods"], [gt1b.b()], "gt1b")
        mixT = [A.alloc("mixT%d" % i, [128, 8, 128], BF16) for i in range(2)]
        xt = [A.alloc("xt%d" % i, [128, D], F32) for i in range(2)]
        x1 = [A.alloc("x1_%d" % i, [128, D], F32) for i in range(2)]
        ytmp = A.alloc("ytmp", [128, D], F32)
        xs2 = A.alloc("xs2", [128, D], F32)
        h32 = A.alloc("h32", [128, 8, 128], F32)
        junk = A.alloc("junk", [128, D], BF16)
        st = [A.alloc("st%d" % i, [128, 2], F32) for i in range(2)]
        rt = [A.alloc("rt%d" % i, [128, 160], F32) for i in range(2)]
        h2T = hT
        for t in range(TL):
            k = t % 2
            mT, x_, x1_, s_, r_ = mixT[k], xt[k], x1[k], st[k], rt[k]
            DMA("sp", x_.ap, xin[b, (t + 2) * 128:(t + 3) * 128, :], (), [x_.b()], "xt%d" % k)
            pT = ps[0][:].bitcast(BF16)
            for kc in range(8):
                TR(pT[:, kc * 128:(kc + 1) * 128], mix.ap[:, t, kc * 128:(kc + 1) * 128], identb.ap, [mix.b(t), identb.b()], [PB(0)])
            CP("act", mT.ap, pT.rearrange("p (a b) -> p a b", a=8, b=128), [PB(0)], [mT.b()])
            for half in range(2):
                pb = 1 + half
                for kc in range(8):
                    MM(ps[pb][:], mT.ap[:, kc, :], wo.ap[:, kc, half * 512:(half + 1) * 512], kc == 0, kc == 7,
                       [mT.b(), wo.b()], [PB(pb)])
                sl = slice(half * 512, (half + 1) * 512)
                TT("dve", ytmp.ap[:, sl], ps[pb][:], gt1b.ap[:, sl], ALU.mult, [PB(pb), gt1b.b()], [ytmp.b(half)])
                TT("pool", x1_.ap[:, sl], ytmp.ap[:, sl], x_.ap[:, sl], ALU.add, [ytmp.b(half), x_.b()], [x1_.b()])
            DMA("sp", x1s_d[b, t * 128:(t + 1) * 128, :], x1_.ap, [x1_.b()], ["x1s"], "x1_%d" % k)
            rms_stats(x1_.ap, [x1_.b()], junk, s_, D)
            S.op("dve", (lambda o, i, s: lambda e: e.tensor_scalar_mul(out=o, in0=i, scalar1=s))(xs2.ap, x1_.ap, s_.ap[:, 1:2]),
                 [x1_.b(), s_.b()], [xs2.b()])
            for kc in range(8):
                pb = 3 + kc // 4
                TR(ps[pb][:, (kc % 4) * 128:(kc % 4 + 1) * 128], xs2.ap[:, kc * 128:(kc + 1) * 128], identf.ap,
                   [xs2.b(), identf.b()], [PB(pb)])
            for kc in range(8):
                pb = 3 + kc // 4
                i_ = ps[pb][:, (kc % 4) * 128:(kc % 4 + 1) * 128]
                if kc % 2 == 0:
                    ACT(h32.ap[:, kc, :], i_, AF.Identity, [PB(pb), A2.b(), modT.b()], [h32.b()],
                        bias=modT.ap[:, 24 + kc, b:b + 1], scale=A2.ap[:, b, kc:kc + 1])
                else:
                    TS("dve", h32.ap[:, kc, :], i_, A2.ap[:, b, kc:kc + 1], modT.ap[:, 24 + kc, b:b + 1], ALU.mult, ALU.add,
                       [PB(pb), A2.b(), modT.b()], [h32.b()])
            CP("pool", h2T.ap[:, :, t * 128:(t + 1) * 128], h32.ap, [h32.b()], [h2T.b("m%d" % t)])
            for kc in range(8):
                MM(ps[5][:, 0:36], h32.ap[:, kc, :], wr32.ap[:, kc, :], kc == 0, kc == 7, [h32.b(), wr32.b()], [PB(5)])
            R = r_.ap
            rb = [r_.b()]
            lg, els, k1, k2, tmp = R[:, 0:36], R[:, 36:68], R[:, 68:100], R[:, 100:132], R[:, 132:140]
            sc = R[:, 140:160]
            TT("dve", lg, ps[5][:, 0:36], brb.ap, ALU.add, [PB(5), brb.b()], rb)
            RMAX(sc[:, 0:1], lg[:, 0:4], rb, rb)
            TS("dve", tmp[:, 0:4], lg[:, 0:4], sc[:, 0:1], None, ALU.is_equal, None, rb, rb)
            TS("dve", sc[:, 1:2], sc[:, 0:1], -1.0, None, ALU.mult, None, rb, rb)
            MSET("dve", sc[:, 2:3], 0.0, rb)
            ACT(tmp[:, 4:8], lg[:, 0:4], AF.Exp, rb, rb, bias=sc[:, 1:2], accum=sc[:, 2:3])
            RECIP(sc[:, 3:4], sc[:, 2:3], rb, rb)
            TS("dve", tmp[:, 0:4], tmp[:, 0:4], 1e30, -1e30, ALU.mult, ALU.add, rb, rb)
            for g in range(4):
                TS("dve", els[:, g * 8:(g + 1) * 8], lg[:, 4 + g * 8:12 + g * 8], tmp[:, g:g + 1], None, ALU.add, None, rb, rb)
            RMAX(sc[:, 4:5], els, rb, rb)
            TS("dve", k1, els, sc[:, 4:5], None, ALU.is_equal, None, rb, rb)
            STT("dve", els, k1, -1e30, els, ALU.mult, ALU.add, rb, rb)
            RMAX(sc[:, 5:6], els, rb, rb)
            TS("dve", k2, els, sc[:, 5:6], None, ALU.is_equal, None, rb, rb)
            TT("dve", sc[:, 6:7], sc[:, 5:6], sc[:, 4:5], ALU.subtract, rb, rb)
            ACT(sc[:, 7:8], sc[:, 6:7], AF.Exp, rb, rb)
            TS("dve", sc[:, 8:9], sc[:, 7:8], 1.0, None, ALU.add, None, rb, rb)
            RECIP(sc[:, 8:9], sc[:, 8:9], rb, rb)
            TT("dve", sc[:, 9:10], sc[:, 8:9], sc[:, 3:4], ALU.mult, rb, rb)
            TT("dve", sc[:, 10:11], sc[:, 9:10], sc[:, 7:8], ALU.mult, rb, rb)
            TS("dve", k1, k1, sc[:, 9:10], None, ALU.mult, None, rb, rb)
            STT("dve", Wd.ap[:, t, :], k2, sc[:, 10:11], k1, ALU.mult, ALU.add, rb, [Wd.b()])
        S.barrier()
        if stop == 8:
            return finish([("Wd", Wd.ap, []), ("h2T", hT.ap, [])])
        A.top = mixer_mark

        yacc = A.alloc("yacc", [128, TL, D], F32)
        gt2b = A.alloc("gt2b", [128, D], F32)
        rg = [A.alloc("rg%d" % i, [128, 8, 512], BF16) for i in range(2)]
        ru = [A.alloc("ru%d" % i, [128, 8, 512], BF16) for i in range(2)]
        rd = [A.alloc("rd%d" % i, [128, 4, D], BF16) for i in range(2)]
        sgt = [A.alloc("sgt%d" % i, [128, 512], F32) for i in range(2)]
        hTm = [A.alloc("hTm%d" % i, [128, 4, 512], BF16) for i in range(2)]
        DMA("sp", gt2b.ap, mods_d[b, 40:48, :].rearrange("(o a) b -> o (a b)", o=1).partition_broadcast(128), ["mods"], [gt2b.b()], "gt2b")
        for t in range(TL):
            DMA("sp", yacc.ap[:, t, :], x1s_d[b, t * 128:(t + 1) * 128, :], ["x1s"], [yacc.b(t)], "yacc%d" % (t % 4))
        h2all = [h2T.b("m%d" % t) for t in range(TL)]
        units = [(e, tb) for e in range(NE) for tb in range(4)]
        cnts = [0, 0]

        def emit_GU(u):
            e, tb = units[u]
            k = e % 2
            if tb == 0:
                DMA("pool", rg[k].ap, ewg_d[e], (), [rg[k].b()], "rg%d" % k)
                DMA("pool", ru[k].ap, ewu_d[e], (), [ru[k].b()], "ru%d" % k)
                DMA("pool", rd[k].ap, ewd_d[e], (), [rd[k].b()], "rd%d" % k)
                for hc in range(4):
                    TT("pool", rd[k].ap[:, hc, :], rd[k].ap[:, hc, :], gt2b.ap, ALU.mult, [rd[k].b(), gt2b.b()], [rd[k].b()])
            hm = hTm[u % 2]
            hr = h2all[tb * 4:tb * 4 + 4]
            for hc in range(4):
                pg, pu = cnts[0] % 2, 2 + cnts[0] % 2
                s_ = sgt[cnts[0] % 2]
                cnts[0] += 1
                for kc in range(8):
                    MM(ps[pg][:], rg[k].ap[:, kc, hc * 128:(hc + 1) * 128], h2T.ap[:, kc, tb * 512:(tb + 1) * 512],
                       kc == 0, kc == 7, [rg[k].b()] + hr, [PB(pg)])
                for kc in range(8):
                    MM(ps[pu][:], ru[k].ap[:, kc, hc * 128:(hc + 1) * 128], h2T.ap[:, kc, tb * 512:(tb + 1) * 512],
                       kc == 0, kc == 7, [ru[k].b()] + hr, [PB(pu)])
                ACT(s_.ap, ps[pg][:], AF.Silu, [PB(pg)], [s_.b()])
                TT("dve", hm.ap[:, hc, :], ps[pu][:], s_.ap, ALU.mult, [PB(pu), s_.b()], [hm.b()])

        def emit_DOWN(u):
            e, tb = units[u]
            k = e % 2
            hm = hTm[u % 2]
            for ti in range(4):
                t = tb * 4 + ti
                for half in range(2):
                    py = 4 + cnts[1] % 4
                    cnts[1] += 1
                    for hc in range(4):
                        MM(ps[py][:], hm.ap[:, hc, ti * 128:(ti + 1) * 128], rd[k].ap[:, hc, half * 512:(half + 1) * 512],
                           hc == 0, hc == 3, [hm.b(), rd[k].b()], [PB(py)])
                    ya = yacc.ap[:, t, half * 512:(half + 1) * 512]
                    STT("dve", ya, ps[py][:], Wd.ap[:, t, e:e + 1], ya, ALU.mult, ALU.add,
                        [PB(py), Wd.b(), yacc.b(t)], [yacc.b(t)])

        emit_GU(0)
        for u in range(len(units)):
            if u + 1 < len(units):
                emit_GU(u + 1)
            emit_DOWN(u)
        if stop == 9:
            S.barrier()
            return finish([("yacc", yacc.ap, [])])
        fngb = A.alloc("fngb", [128, D], F32)
        DMA("sp", fngb.ap, fng_d.partition_broadcast(128), (), [fngb.b()], "fngb")
        ot = [A.alloc("ot%d" % i, [128, D], F32) for i in range(2)]
        junk = A.alloc("junk", [128, D], BF16)
        st = [A.alloc("st%d" % i, [128, 2], F32) for i in range(2)]
        for t in range(TL):
            o_, s_ = ot[t % 2], st[t % 2]
            rms_stats(yacc.ap[:, t, :], [yacc.b(t)], junk, s_, D)
            STT("dve", o_.ap, yacc.ap[:, t, :], s_.ap[:, 1:2], fngb.ap, ALU.mult, ALU.mult,
                [yacc.b(t), s_.b(), fngb.b()], [o_.b()])
            out_stores.append(DMA("sp", out_d[b, t * 128:(t + 1) * 128, :], o_.ap, [o_.b()], (), "ot%d" % (t % 2)))
        S.barrier()

    S.op("sp", None, extra_deps=out_stores)
    S.emit()
    return nc


def _pk(w, kc):
    K, N = w.shape
    return np.ascontiguousarray(w.reshape(kc, 128, N).transpose(1, 0, 2))


def _rope_tables():
    f = np.arange(16, dtype=np.float32)
    inv = np.power(np.float32(10000.0), -f / np.float32(16)).astype(np.float32)
    t = np.arange(2048)
    pos = np.stack([(t // 64).astype(np.float32), (t % 64).astype(np.float32)], 0)
    cos = np.zeros((128, 2048), np.float32)
    sin = np.zeros((128, 2048), np.float32)
    perm = np.zeros((128, 128), np.float32)
    for p in range(128):
        d = p % 64
        axis, half, fi = d // 32, (d % 32) // 16, d % 16
        ang = (pos[axis] * inv[fi]).astype(np.float32)
        cos[p] = np.cos(ang)
        sin[p] = np.sin(ang) * (-1.0 if half == 0 else 1.0)
        partner = p + 16 if half == 0 else p - 16
        perm[partner, p] = 1.0
    return cos, sin, perm


def _tri():
    j = np.arange(128)[:, None]
    i = np.arange(128)[None, :]
    tri = np.stack([(j <= i), (j >= i), (j > i), (j < i)], 1).astype(np.float32)
    return np.ascontiguousarray(tri)


_NC_CACHE = {}


def kernel(x, c, ctx, c_ctx, w_ada, b_ada, norm_mix_g, norm_ffn_g, w_in, da_lambda, da_subln_g, gla_gate_w2,
           gla_gate_b, gla_norm_g, w_out, router_group_w, router_group_b, router_expert_w, router_expert_b,
           expert_w_gate, expert_w_up, expert_w_down, final_norm_g):
    f = lambda a: np.asarray(a, dtype=np.float32)
    x, c, ctx, c_ctx = f(x), f(c), f(ctx), f(c_ctx)
    n_cores = 8
    cos, sin, perm = _rope_tables()
    w2 = np.zeros((32, 512), np.float32)
    gw2 = f(gla_gate_w2)[0]
    w2[0:16, 0:256] = gw2[0]
    w2[16:32, 256:512] = gw2[1]
    shared = {
        "w_ada": _pk(f(w_ada)[0], 8),
        "b_ada": np.ascontiguousarray(f(b_ada)[0].reshape(48, 128).T),
        "g1": np.ascontiguousarray(f(norm_mix_g)[0].reshape(8, 128).T),
        "g2": np.ascontiguousarray(f(norm_ffn_g)[0].reshape(8, 128).T),
        "w_in": _pk(f(w_in)[0], 8),
        "da_lambda": f(da_lambda)[0].reshape(1, 256),
        "subln_g": f(da_subln_g)[0].reshape(1, 128),
        "gla_norm_g": f(gla_norm_g)[0].reshape(1, 128),
        "final_g": f(final_norm_g).reshape(1, D),
        "gate_w2": w2,
        "gate_b": f(gla_gate_b)[0].reshape(1, 512),
        "w_out": _pk(f(w_out)[0], 8),
        "w_router": _pk(np.concatenate([f(router_group_w)[0], f(router_expert_w)[0]], 1), 8),
        "b_router": np.concatenate([f(router_group_b)[0], f(router_expert_b)[0]]).reshape(1, 36),
        "ewg": np.ascontiguousarray(f(expert_w_gate)[0].reshape(NE, 8, 128, 512).transpose(0, 2, 1, 3)),
        "ewu": np.ascontiguousarray(f(expert_w_up)[0].reshape(NE, 8, 128, 512).transpose(0, 2, 1, 3)),
        "ewd": np.ascontiguousarray(f(expert_w_down)[0].reshape(NE, 4, 128, D).transpose(0, 2, 1, 3)),
        "rope_cos": cos, "rope_sin": sin, "rope_perm": perm, "tri": _tri(),
    }
    in_maps = []
    for i in range(n_cores):
        bs = [NB * i + k for k in range(NB)]
        xin = np.stack([np.concatenate([ctx[bb], x[bb]], 0) for bb in bs], 0)
        cvec = np.stack([c[bs[0]], c[bs[1]], c_ctx], 1)
        m = dict(shared)
        m["xin"] = np.ascontiguousarray(xin)
        m["cT"] = np.ascontiguousarray(cvec.reshape(8, 128, 3).transpose(1, 0, 2))
        in_maps.append(m)
    if "nc" not in _NC_CACHE:
        _NC_CACHE["nc"] = build_program()
    res = run_bass_kernel_spmd(_NC_CACHE["nc"], in_maps, core_ids=list(range(n_cores)))
    out = np.concatenate([np.asarray(r["out"]) for r in res.results], 0)
    return out.astype(np.float32)
```
